# Optimizing a Trainium2 kernel written in Bass

```python
import jax, jax.numpy as jnp
from jax import lax
import numpy as np

D_MODEL = 1024
BATCH = 2
SEQ = 16384
DEPTH = 1

A_GROUPS = 4
A_GROUP_DIM = 128
A_WIDTH = A_GROUPS * A_GROUP_DIM
CHUNK = 128
N_HEADS = 4
HEAD_DIM = 128
B_WIDTH = N_HEADS * HEAD_DIM
KV_LATENT = 256
ROT_DIM = HEAD_DIM // 4
IDX_HEADS = 4
IDX_DIM = 64
IDX_ROT = IDX_DIM // 4
TOPK_MAX = 256
Q_BLOCK = 128
ROPE_THETA = 500000.0
N_BRANCHES = 2
PEER_HEADS = 8
PEER_KEY_DIM = 256
PEER_HALF = PEER_KEY_DIM // 2
N_KEYS = 128
N_EXPERTS = N_KEYS * N_KEYS
PEER_TOPK = 16
PEER_BLOCK = 128
EPS = 1e-6

SPLITS = (A_WIDTH, A_WIDTH, B_WIDTH, KV_LATENT, IDX_HEADS * IDX_DIM, IDX_DIM, IDX_HEADS, N_BRANCHES * D_MODEL)
IN_WIDTH = sum(SPLITS)
SPLIT_OFFSETS = tuple(int(o) for o in np.cumsum(SPLITS)[:-1])

kernel_name = "hybrid_gmlp_dsa_peer_block"


def rms_norm(x, g):
    xf = x.astype(jnp.float32)
    y = xf * lax.rsqrt(jnp.mean(xf * xf, axis=-1, keepdims=True) + EPS)
    return (y * g.astype(jnp.float32)).astype(x.dtype)


def layer_norm(x, g, b):
    xf = x.astype(jnp.float32)
    mu = jnp.mean(xf, axis=-1, keepdims=True)
    xc = xf - mu
    y = xc * lax.rsqrt(jnp.mean(xc * xc, axis=-1, keepdims=True) + EPS)
    return (y * g.astype(jnp.float32) + b.astype(jnp.float32)).astype(x.dtype)


def partial_rope(x, pos, rot_dim):
    half = rot_dim // 2
    inv = jnp.power(jnp.float32(ROPE_THETA), -jnp.arange(half, dtype=jnp.float32) * 2.0 / rot_dim)
    ang = pos.astype(jnp.float32)[..., None] * inv
    cos = jnp.cos(ang)[:, :, None, :]
    sin = jnp.sin(ang)[:, :, None, :]
    xr = x[..., :rot_dim].astype(jnp.float32)
    x1, x2 = xr[..., :half], xr[..., half:]
    rot = jnp.concatenate([x1 * cos - x2 * sin, x2 * cos + x1 * sin], axis=-1)
    return jnp.concatenate([rot.astype(x.dtype), x[..., rot_dim:]], axis=-1)


def chunked_spatial_gating(u, v, v_g, v_b, w_s, b_s):
    bsz, s, _ = u.shape
    u = jax.nn.gelu(u)
    v = layer_norm(jax.nn.gelu(v), v_g, v_b)
    v = v.reshape(bsz, s // CHUNK, CHUNK, A_GROUPS, A_GROUP_DIM)
    causal = jnp.tril(jnp.ones((CHUNK, CHUNK), dtype=bool))
    w = jnp.where(causal[None], w_s, jnp.zeros_like(w_s))
    z = jnp.einsum('gts,bcsgd->bctgd', w, v) + b_s.T[None, None, :, :, None]
    return u * z.reshape(bsz, s, A_WIDTH)


def dsa_attention(q, k, v, q_idx, k_idx, w_idx):
    bsz, s = q.shape[:2]
    n_sel = min(TOPK_MAX, s // 4)
    nblk = s // Q_BLOCK
    key_pos = jnp.arange(s)

    def to_blocks(a):
        return a.reshape(bsz, nblk, Q_BLOCK, *a.shape[2:]).swapaxes(0, 1)

    def gather_rows(table, idx):
        return table[idx]

    def block_fn(args):
        blk, qb, qib, wib = args
        q_pos = blk * Q_BLOCK + jnp.arange(Q_BLOCK)
        causal = key_pos[None, :] <= q_pos[:, None]
        logits = jnp.einsum('bqhd,bsd->bqhs', qib, k_idx, preferred_element_type=jnp.float32) * (IDX_DIM ** -0.5)
        scores = jnp.einsum('bqh,bqhs->bqs', wib.astype(jnp.float32), jax.nn.relu(logits))
        scores = jnp.where(causal[None], scores, -jnp.inf)
        _, idx = lax.top_k(scores, n_sel)
        valid = idx <= q_pos[None, :, None]
        k_sel = jax.vmap(gather_rows)(k, idx)
        v_sel = jax.vmap(gather_rows)(v, idx)
        att = jnp.einsum('bqhd,bqnd->bqhn', qb, k_sel, preferred_element_type=jnp.float32) * (HEAD_DIM ** -0.5)
        att = jnp.where(valid[:, :, None, :], att, jnp.float32(-1e30))
        p = jax.nn.softmax(att, axis=-1).astype(v.dtype)
        return jnp.einsum('bqhn,bqnd->bqhd', p, v_sel)

    out = lax.map(block_fn, (jnp.arange(nblk), to_blocks(q), to_blocks(q_idx), to_blocks(w_idx)))
    return out.swapaxes(0, 1).reshape(bsz, s, B_WIDTH)


def peer(xn, wq, subkeys, u_tab, v_tab):
    bsz, s, d = xn.shape
    q = (xn @ wq).reshape(bsz, s, PEER_HEADS, 2, PEER_HALF)
    sub = jnp.einsum('bshpd,hpnd->bshpn', q, subkeys, preferred_element_type=jnp.float32)
    s_top, i_top = lax.top_k(sub, PEER_TOPK)
    cand = (s_top[..., 0, :, None] + s_top[..., 1, None, :]).reshape(bsz, s, PEER_HEADS, PEER_TOPK * PEER_TOPK)
    cand_idx = (i_top[..., 0, :, None] * N_KEYS + i_top[..., 1, None, :]).reshape(bsz, s, PEER_HEADS, PEER_TOPK * PEER_TOPK)
    best, pos = lax.top_k(cand, PEER_TOPK)
    expert = jnp.take_along_axis(cand_idx, pos, axis=-1)
    gate = jax.nn.softmax(best, axis=-1)
    nb = (bsz * s) // PEER_BLOCK
    hk = PEER_HEADS * PEER_TOPK
    xt = xn.reshape(nb, PEER_BLOCK, d)
    et = expert.reshape(nb, PEER_BLOCK, hk)
    gt = gate.reshape(nb, PEER_BLOCK, hk)

    def blk(args):
        xb, eb, gb = args
        ub = u_tab[eb]
        vb = v_tab[eb]
        a = jax.nn.gelu(jnp.einsum('td,ted->te', xb, ub, preferred_element_type=jnp.float32))
        return jnp.einsum('te,ted->td', (gb * a).astype(vb.dtype), vb)

    y = lax.map(blk, (xt, et, gt))
    return y.reshape(bsz, s, d)


def setup_inputs(seed: int = 0) -> dict:
    key = jax.random.key(seed)
    ks = jax.random.split(key, 24)
    f32 = jnp.float32
    L = DEPTH
    nrm = lambda k, shape, scale: jax.random.normal(k, shape, f32) * scale
    x = jax.random.normal(ks[0], (BATCH, SEQ, D_MODEL), f32)
    positions = jnp.broadcast_to(jnp.arange(SEQ, dtype=jnp.int32)[None, :], (BATCH, SEQ))
    return {
        "x": x,
        "positions": positions,
        "norm1_g": 1.0 + nrm(ks[1], (L, D_MODEL), 0.02),
        "w_in": nrm(ks[2], (L, D_MODEL, IN_WIDTH), D_MODEL ** -0.5),
        "v_norm_g": 1.0 + nrm(ks[3], (L, A_WIDTH), 0.02),
        "v_norm_b": nrm(ks[4], (L, A_WIDTH), 0.02),
        "spatial_w": nrm(ks[5], (L, A_GROUPS, CHUNK, CHUNK), CHUNK ** -0.5),
        "spatial_b": 1.0 + nrm(ks[6], (L, A_GROUPS, CHUNK), 0.1),
        "kv_norm_g": 1.0 + nrm(ks[7], (L, KV_LATENT), 0.02),
        "w_uk": nrm(ks[8], (L, KV_LATENT, HEAD_DIM), KV_LATENT ** -0.5),
        "w_uv": nrm(ks[9], (L, KV_LATENT, HEAD_DIM), KV_LATENT ** -0.5),
        "q_norm_g": 1.0 + nrm(ks[10], (L, HEAD_DIM), 0.02),
        "k_norm_g": 1.0 + nrm(ks[11], (L, HEAD_DIM), 0.02),
        "w_a_out": nrm(ks[12], (L, A_WIDTH, D_MODEL), A_WIDTH ** -0.5),
        "w_b_out": nrm(ks[13], (L, B_WIDTH, D_MODEL), B_WIDTH ** -0.5),
        "w_o": nrm(ks[14], (L, D_MODEL, D_MODEL), D_MODEL ** -0.5),
        "norm2_g": 1.0 + nrm(ks[15], (L, D_MODEL), 0.02),
        "peer_wq": nrm(ks[16], (L, D_MODEL, PEER_HEADS * PEER_KEY_DIM), D_MODEL ** -0.5),
        "peer_subkeys": nrm(ks[17], (L, PEER_HEADS, 2, N_KEYS, PEER_HALF), PEER_HALF ** -0.5),
        "peer_u": nrm(ks[18], (L, N_EXPERTS, D_MODEL), D_MODEL ** -0.5),
        "peer_v": nrm(ks[19], (L, N_EXPERTS, D_MODEL), 0.5 * PEER_HEADS ** -0.5),
    }


def reference(x, positions, norm1_g, w_in, v_norm_g, v_norm_b, spatial_w, spatial_b, kv_norm_g, w_uk, w_uv,
              q_norm_g, k_norm_g, w_a_out, w_b_out, w_o, norm2_g, peer_wq, peer_subkeys, peer_u, peer_v):
    bsz, s, d = x.shape
    h = x
    for l in range(DEPTH):
        xn = rms_norm(h, norm1_g[l])
        proj = xn @ w_in[l]
        u, v, q, c_kv, q_idx, k_idx, w_idx, gates = jnp.split(proj, SPLIT_OFFSETS, axis=-1)

        y_a = chunked_spatial_gating(u, v, v_norm_g[l], v_norm_b[l], spatial_w[l], spatial_b[l])

        c_n = rms_norm(c_kv, kv_norm_g[l])
        k = rms_norm(c_n @ w_uk[l], k_norm_g[l])
        vv = c_n @ w_uv[l]
        qh = rms_norm(q.reshape(bsz, s, N_HEADS, HEAD_DIM), q_norm_g[l])
        qh = partial_rope(qh, positions, ROT_DIM)
        k = partial_rope(k[:, :, None, :], positions, ROT_DIM)[:, :, 0, :]
        qi = partial_rope(q_idx.reshape(bsz, s, IDX_HEADS, IDX_DIM), positions, IDX_ROT)
        ki = partial_rope(k_idx[:, :, None, :], positions, IDX_ROT)[:, :, 0, :]
        wi = w_idx * (IDX_HEADS ** -0.5)
        y_b = dsa_attention(qh, k, vv, qi, ki, wi)

        g = jax.nn.sigmoid(gates.reshape(bsz, s, N_BRANCHES, d))
        merged = g[:, :, 0, :] * (y_a @ w_a_out[l]) + g[:, :, 1, :] * (y_b @ w_b_out[l])
        h = h + merged @ w_o[l]

        hn = rms_norm(h, norm2_g[l])
        h = h + peer(hn, peer_wq[l], peer_subkeys[l], peer_u[l], peer_v[l])
    return h
```

```python
import numpy as np
import ml_dtypes
from contextlib import ExitStack
import concourse.bass as bass
import concourse.mybir as mybir
from concourse.bass_utils import run_bass_kernel_spmd

F32 = mybir.dt.float32
BF16 = mybir.dt.bfloat16
I32 = mybir.dt.int32
U32 = mybir.dt.uint32
ALU = mybir.AluOpType
AF = mybir.ActivationFunctionType
AX = mybir.AxisListType

D = 1024
INW = 4164
EPS = 1e-6
TOPK = 256
NEXP = 16384
PI = float(np.pi)
ENGS = ("pe", "act", "dve", "pool", "sp")
KDMA = 8
BIS_ITERS = 26
KT0 = 0
NDUMMY = 0
KT1 = None


def _k(x):
    if isinstance(x, tuple):
        return x
    return (x, (x.name,))


class Prog:
    def __init__(self, nc):
        self.nc = nc
        self.ops = {e: [] for e in ENGS}
        self.cnt = {e: 0 for e in ENGS}
        self.dcnt = {e: 0 for e in ENGS}
        self.waited = {e: {} for e in ENGS}
        self.res_w = {}
        self.res_r = {}

    def op(self, eng, fn, reads=(), writes=(), dma=False):
        waits = {}

        def addw(t):
            sk, v = t
            if waits.get(sk, 0) < v:
                waits[sk] = v

        for r in reads:
            if r in self.res_w:
                addw(self.res_w[r])
        for w in writes:
            if w in self.res_w:
                addw(self.res_w[w])
            for sk, v in self.res_r.get(w, {}).items():
                addw((sk, v))
        if dma:
            k = self.dcnt[eng]
            self.dcnt[eng] += 1
            slot = k % KDMA
            semkey = ("dma", eng, slot)
            val = 16 * (k // KDMA + 1)
            inc = 16
            if k >= KDMA:
                addw((semkey, val - 16))
        else:
            self.cnt[eng] += 1
            semkey = eng
            val = self.cnt[eng]
            inc = 1
        wl = []
        for sk, v in waits.items():
            if eng == "pe" and sk == "pe":
                continue
            if self.waited[eng].get(sk, 0) >= v:
                continue
            self.waited[eng][sk] = v
            wl.append((sk, v))
        self.ops[eng].append((wl, fn, semkey, inc))
        tok = (semkey, val)
        for r in reads:
            d = self.res_r.setdefault(r, {})
            if d.get(semkey, 0) < val:
                d[semkey] = val
        for w in writes:
            self.res_w[w] = tok
            self.res_r[w] = {}
        return tok

    def _rw(self, outs, ins):
        wk = []
        rk = []
        for o in outs:
            wk += list(_k(o)[1])
        for i in ins:
            if i is None or isinstance(i, (int, float)):
                continue
            rk += list(_k(i)[1])
        return rk, wk

    @staticmethod
    def _a(x):
        if x is None or isinstance(x, (int, float)):
            return x
        return _k(x)[0]

    def dot(self, junk, a, b, acc):
        rk, wk = self._rw([junk, acc], [a, b])
        j, x, y, c = self._a(junk), self._a(a), self._a(b), self._a(acc)
        self.op("dve", lambda e: e.scalar_tensor_tensor(j, x, 1.0, y, ALU.mult, ALU.mult, accum_out=c), rk, wk)

    def matmul(self, out, lhsT, rhs, start=True, stop=True):
        rk, wk = self._rw([out], [lhsT, rhs])
        o, l, r = self._a(out), self._a(lhsT), self._a(rhs)
        self.op("pe", lambda e: e.matmul(o, l, r, start=start, stop=stop), rk, wk)

    def transpose(self, out, in_, ident):
        rk, wk = self._rw([out], [in_])
        o, i, d = self._a(out), self._a(in_), self._a(ident)
        self.op("pe", lambda e: e.transpose(o, i, d), rk, wk)

    def act(self, out, in_, func, bias=None, scale=None, accum=None, eng="act"):
        outs = [out] + ([accum] if accum is not None else [])
        rk, wk = self._rw(outs, [in_, bias, scale])
        o, i, b, s, a = self._a(out), self._a(in_), self._a(bias), self._a(scale), self._a(accum)
        kw = {}
        if b is not None:
            kw["bias"] = b
        if s is not None:
            kw["scale"] = s
        if a is not None:
            kw["accum_out"] = a
        self.op("act", lambda e: e.activation(o, i, func, **kw), rk, wk)

    def ts(self, out, in0, s1, s2, op0, op1=None, accum=None, eng="dve"):
        outs = [out] + ([accum] if accum is not None else [])
        rk, wk = self._rw(outs, [in0, s1, s2])
        o, i, a1, a2, ac = self._a(out), self._a(in0), self._a(s1), self._a(s2), self._a(accum)
        kw = {}
        if op1 is not None:
            kw["op1"] = op1
        if ac is not None:
            kw["accum_out"] = ac
        self.op(eng, lambda e: e.tensor_scalar(o, i, a1, a2, op0, **kw), rk, wk)

    def tt(self, out, in0, in1, op, eng="dve"):
        rk, wk = self._rw([out], [in0, in1])
        o, i0, i1 = self._a(out), self._a(in0), self._a(in1)
        self.op(eng, lambda e: e.tensor_tensor(o, i0, i1, op), rk, wk)

    def stt(self, out, in0, scalar, in1, op0, op1, eng="dve"):
        rk, wk = self._rw([out], [in0, scalar, in1])
        o, i0, s, i1 = self._a(out), self._a(in0), self._a(scalar), self._a(in1)
        self.op(eng, lambda e: e.scalar_tensor_tensor(o, i0, s, i1, op0, op1), rk, wk)

    def copy(self, out, in_, eng="dve"):
        rk, wk = self._rw([out], [in_])
        o, i = self._a(out), self._a(in_)
        if eng == "act":
            self.op("act", lambda e: e.activation(o, i, AF.Copy), rk, wk)
        else:
            self.op(eng, lambda e: e.tensor_copy(o, i), rk, wk)

    def reduce(self, out, in_, op, eng="dve"):
        rk, wk = self._rw([out], [in_])
        o, i = self._a(out), self._a(in_)
        self.op(eng, lambda e: e.tensor_reduce(o, i, AX.X, op), rk, wk)

    def memset(self, out, val, eng="dve"):
        rk, wk = self._rw([out], [])
        o = self._a(out)
        self.op(eng, lambda e: e.memset(o, val), rk, wk)

    def dma(self, out, in_, eng="sp"):
        rk, wk = self._rw([out], [in_])
        o, i = self._a(out), self._a(in_)
        self.op(eng, lambda e: e.dma_start(out=o, in_=i), rk, wk, dma=True)

    def gather(self, out, table, idx):
        rk, wk = self._rw([out], [idx])
        o, t, i = self._a(out), self._a(table), self._a(idx)
        self.op(
            "pool",
            lambda e: e.indirect_dma_start(
                out=o, out_offset=None, in_=t, in_offset=bass.IndirectOffsetOnAxis(ap=i, axis=0)
            ),
            rk,
            wk,
            dma=True,
        )

    def emit(self, es):
        nc = self.nc
        sems = {}
        for e in ENGS:
            sems[e] = es.enter_context(nc.semaphore("s_" + e))
            for s in range(KDMA):
                sems[("dma", e, s)] = es.enter_context(nc.semaphore("d_%s_%d" % (e, s)))
        block = es.enter_context(nc.Block())

        def run(engobj, name):
            for wl, fn, semkey, inc in self.ops[name]:
                for sk, v in wl:
                    engobj.wait_ge(sems[sk], v)
                fn(engobj).then_inc(sems[semkey], inc)
            k = self.dcnt[name]
            for s in range(min(k, KDMA)):
                n = (k - 1 - s) // KDMA + 1
                engobj.wait_ge(sems[("dma", name, s)], 16 * n)

        @block.tensor
        def _(e):
            run(e, "pe")

        @block.scalar
        def _(e):
            run(e, "act")

        @block.vector
        def _(e):
            run(e, "dve")

        @block.gpsimd
        def _(e):
            run(e, "pool")

        @block.sync
        def _(e):
            run(e, "sp")


def build_program(S, NOWN, dbg=None):
    NKT = S // 128
    nc = bass.Bass("TRN2", target_bir_lowering=False)
    es = ExitStack()
    P = Prog(nc)

    def din(name, shape, dt=F32):
        return nc.dram_tensor(name, list(shape), dt, kind="ExternalInput")

    NXP = 4
    TPP = NKT // NXP
    x_seq = [din("x_seq%d" % i, [S // NXP, D]) for i in range(NXP)]
    x_own = din("x_own", [NOWN * 128, D])
    pos_seq = din("pos_seq", [128, NKT], I32)
    pos_own = din("pos_own", [128, NOWN], I32)
    w_in = din("w_in", [D, INW])
    g1_d = din("g1", [128, 8])
    g2_d = din("g2", [128, 8])
    g2b_d = din("g2b", [128, D])
    kvg_d = din("kvg", [128, 2])
    vng_d = din("vng", [128, 512])
    vnb_d = din("vnb", [128, 512])
    spw_d = din("spw", [4, 128, 128])
    spb_d = din("spb", [128, 4])
    wuk_d = din("wuk", [256, 128])
    wuv_d = din("wuv", [256, 128])
    qg_d = din("qgb", [128, 128])
    kg_d = din("kgb", [128, 128])
    wa_d = din("wa", [512, D])
    wb_d = din("wb", [512, D])
    wo_d = din("wo", [D, D])
    wq_d = din("wq", [D, 2048])
    sk_d = din("subk", [16, 128, 128])
    if not dbg:
        u_tab = din("peer_u", [NEXP, D])
        v_tab = din("peer_v", [NEXP, D])
    ident_d = din("ident", [128, 128], BF16)
    tril_d = din("tril", [128, 128])
    maskb_d = din("maskb", [128, 512])
    inv_d = din("invf", [128, 24])
    iota_d = din("iota16", [128, 16])
    out_d = nc.dram_tensor("out", [NOWN * 128, D], F32, kind="ExternalOutput")
    okind = "ExternalOutput" if dbg else "Internal"
    winbf = nc.dram_tensor("winbf", [D, INW], BF16, kind="Internal")
    wabf = nc.dram_tensor("wabf", [512, D], BF16, kind="Internal")
    wbbf = nc.dram_tensor("wbbf", [512, D], BF16, kind="Internal")
    wobf = nc.dram_tensor("wobf", [D, D], BF16, kind="Internal")
    wqbf = nc.dram_tensor("wqbf", [D, 2048], BF16, kind="Internal")
    kiT_d = nc.dram_tensor("kiT_d", [NKT, 64, 128], BF16, kind=okind)
    kT_d = nc.dram_tensor("kT_d", [NKT, 128, 128], BF16, kind=okind)
    vv_d = nc.dram_tensor("vv_d", [S, 128], BF16, kind=okind)

    if dbg == "H":
        d_sc = nc.dram_tensor("d_sc", [128, 512], F32, kind="ExternalOutput")
        d_st = nc.dram_tensor("d_st", [128, 64], F32, kind="ExternalOutput")
        d_yb = nc.dram_tensor("d_yb", [128, 512], BF16, kind="ExternalOutput")
        d_ma = nc.dram_tensor("d_ma", [128, D], F32, kind="ExternalOutput")
        d_gb = nc.dram_tensor("d_gb", [128, D], F32, kind="ExternalOutput")
        d_q = nc.dram_tensor("d_q", [128, 512], BF16, kind="ExternalOutput")
        d_qi = nc.dram_tensor("d_qi", [128, 512], BF16, kind="ExternalOutput")
        d_mT = nc.dram_tensor("d_mT", [128, 512], BF16, kind="ExternalOutput")
        d_e1 = nc.dram_tensor("d_e1", [128, 512], BF16, kind="ExternalOutput")
        d_e2 = nc.dram_tensor("d_e2", [128, 512], BF16, kind="ExternalOutput")
        d_rz = nc.dram_tensor("d_rz", [128, 512], F32, kind="ExternalOutput")
        d_num = nc.dram_tensor("d_num", [128, 512], F32, kind="ExternalOutput")

    def T(name, shape, dt=F32):
        return es.enter_context(nc.sbuf_tensor(name, list(shape), dt))

    AW = 16384
    arena = T("arena", [128, AW])

    def av(lo, hi, dt=F32):
        keys = tuple(("sc", c) for c in range(lo // 512, (hi - 1) // 512 + 1))
        ap = arena[:, lo:hi]
        if dt is not F32:
            ap = ap.bitcast(dt)
        return (ap, keys)

    ident = T("identb", [128, 128], BF16)
    ones = T("onesb", [128, 128], BF16)
    negpi = T("negpi", [128, 1])
    epsb = T("epsb", [128, 1])
    g1 = T("g1s", [128, 8])
    g2 = T("g2s", [128, 8])
    g2b = T("g2bs", [128, D])
    kvg = T("kvgs", [128, 2])
    vng = T("vngs", [128, 512])
    vnb = T("vnbs", [128, 512])
    spb = T("spbs", [128, 4])
    qgb = T("qgbs", [128, 128])
    kgb = T("kgbs", [128, 128])
    trilm = T("trilm", [128, 128])
    maskb = T("maskbs", [128, 512])
    invf = T("invfs", [128, 24])
    iota16 = T("iota16s", [128, 16])
    posk_i = T("posk_i", [128, NKT], I32)
    posk = T("posk", [128, NKT])
    poso_i = T("poso_i", [128, NOWN], I32)
    poso = T("poso", [128, NOWN])
    wk = T("wk", [128, 8, 320], BF16)
    wkv = T("wkv", [128, 2, 256], BF16)
    skT = T("skT", [128, 16, 128], BF16)
    wsT = T("wsT", [128, 4, 128], BF16)

    ps = [es.enter_context(nc.psum_tensor("ps%d" % i, [128, 512], F32)) for i in range(8)]

    def psb(i):
        return ps[i][:].bitcast(BF16)

    for dst, src in ((ident, ident_d), (g1, g1_d), (g2, g2_d), (g2b, g2b_d), (kvg, kvg_d), (vng, vng_d),
                     (vnb, vnb_d), (spb, spb_d), (qgb, qg_d), (kgb, kg_d), (trilm, tril_d), (maskb, maskb_d),
                     (invf, inv_d), (iota16, iota_d), (posk_i, pos_seq), (poso_i, pos_own)):
        P.dma(dst[:], src.ap())
    P.memset(ones[:], 1.0)
    P.memset(negpi[:], -PI)
    P.memset(epsb[:], EPS)
    P.copy(posk[:], posk_i[:])
    P.copy(poso[:], poso_i[:])

    if dbg == "C0":
        P.dma(out_d[0:128, :], g2b[:])
        P.emit(es)
        es.close()
        return nc
    stg = av(0, INW)
    stgb = av(4608, 4608 + INW // 2, BF16)
    for c in range(8):
        P.dma(stg, w_in[c * 128:(c + 1) * 128, :])
        P.act(stgb, stg, AF.Copy, scale=g1[:, c:c + 1])
        P.dma(winbf[c * 128:(c + 1) * 128, :], stgb)
        P.copy(wk[:, c, 0:256], (stgb[0][:, 1536:1792], stgb[1]))
        P.copy(wk[:, c, 256:320], (stgb[0][:, 2048:2112], stgb[1]))
    s2 = av(8192, 8192 + 2048)
    s2b = av(12288, 12288 + 1024, BF16)
    for c in range(8):
        P.dma(s2, wq_d[c * 128:(c + 1) * 128, :])
        P.act(s2b, s2, AF.Copy, scale=g2[:, c:c + 1])
        P.dma(wqbf[c * 128:(c + 1) * 128, :], s2b)
    s3 = av(10240, 10240 + 1024)
    s3b = av(13312, 13312 + 512, BF16)
    for c in range(4):
        P.dma(s3, wa_d[c * 128:(c + 1) * 128, :])
        P.copy(s3b, s3)
        P.dma(wabf[c * 128:(c + 1) * 128, :], s3b)
        P.dma(s3, wb_d[c * 128:(c + 1) * 128, :])
        P.copy(s3b, s3, eng="act")
        P.dma(wbbf[c * 128:(c + 1) * 128, :], s3b)
    for c in range(8):
        P.dma(s3, wo_d[c * 128:(c + 1) * 128, :])
        P.copy(s3b, s3)
        P.dma(wobf[c * 128:(c + 1) * 128, :], s3b)
    s4 = av(11264, 11264 + 128)
    for c in range(2):
        P.dma(s4, wuk_d[c * 128:(c + 1) * 128, :])
        P.act(wkv[:, c, 0:128], s4, AF.Copy, scale=kvg[:, c:c + 1])
        P.dma(s4, wuv_d[c * 128:(c + 1) * 128, :])
        P.act(wkv[:, c, 128:256], s4, AF.Copy, scale=kvg[:, c:c + 1])
    s5 = av(11776, 11776 + 64, BF16)
    for g in range(16):
        P.dma(s4, sk_d[g])
        P.copy(s5, s4)
        P.transpose(psb(7)[:, 0:128], s5, ident[:])
        P.copy(skT[:, g, :], psb(7)[:, 0:128])
    for g in range(4):
        P.dma(s4, spw_d[g])
        P.tt(s5, s4, trilm[:], ALU.mult)
        P.transpose(psb(7)[:, 0:128], s5, ident[:])
        P.copy(wsT[:, g, :], psb(7)[:, 0:128])

    if dbg == "C1":
        P.dma(out_d[0:128, :], g2b[:])
        P.emit(es)
        es.close()
        return nc
    xt = [T("xt%d" % i, [128, D]) for i in range(2)]
    xsb = T("xsb", [128, D], BF16)
    xsT = T("xsT", [128, 8, 128], BF16)
    st = T("stats", [128, 64])
    ang = T("ang", [128, 24])
    a12 = T("a12", [128, 48])
    a12f = T("a12f", [128, 48])
    a12i = T("a12i", [128, 48], I32)
    sc = T("sincos", [128, 48])
    rtmp = T("rtmp", [128, 4, 64])
    rtmp2 = T("rtmp2", [128, 4, 64])
    junkb = T("junkb", [128, 2048], BF16)
    junkf32 = T("junkf32", [128, 1024])
    junkf32b = T("junkf32b", [128, 256])
    cnb = T("cnb", [128, 256], BF16)
    cnT = T("cnT", [128, 2, 128], BF16)
    kn = T("kn", [128, 128])
    kbf = T("kbf", [128, 128], BF16)
    kif = T("kif", [128, 64])
    kibf = T("kibf", [128, 128], BF16)
    P.memset(kibf[:], 0.0)
    vvb = T("vvb", [128, 128], BF16)
    kTs = T("kTs", [128, 128], BF16)
    kiTs = T("kiTs", [64, 128], BF16)
    pooldummy = T("pooldummy", [128, 8], BF16)

    def sumsq(src, n, acc):
        if n <= 256:
            P.copy(junkf32b[:, 0:n], src)
            src = junkf32b[:, 0:n]
        P.dot(junkf32[:, 0:n], src, src, acc)

    def rstd_from_ss(dst, ss, n):
        P.act(dst, ss, AF.Sqrt, bias=epsb[:, 0:1], scale=1.0 / n)
        rk, wk_ = P._rw([dst], [dst])
        P.op("dve", lambda e, o=_k(dst)[0]: e.reciprocal(o, o), rk, wk_)

    def sincos(pos_col):
        P.ts(a12[:, 0:24], invf[:], pos_col, None, ALU.mult)
        P.ts(a12[:, 24:48], invf[:], pos_col, 0.5 * PI, ALU.mult, ALU.add)
        P.ts(a12f[:], a12[:], 1.0 / (2 * PI), None, ALU.mult)
        P.copy(a12i[:], a12f[:])
        P.copy(a12f[:], a12i[:])
        P.stt(a12[:], a12f[:], -2 * PI, a12[:], ALU.mult, ALU.add)
        P.ts(a12f[:], a12[:], PI, None, ALU.is_gt)
        P.stt(a12[:], a12f[:], -2 * PI, a12[:], ALU.mult, ALU.add)
        P.ts(a12f[:], a12[:], -PI, None, ALU.is_lt)
        P.stt(a12[:], a12f[:], 2 * PI, a12[:], ALU.mult, ALU.add)
        P.act(sc[:], a12[:], AF.Sin)

    def rope(dst, src, nh, dim, half, off, scv=None):
        if scv is None:
            scv = sc[:]
        sca, sck = _k(scv)
        sinb = (sca[:, off:off + half].unsqueeze(1).to_broadcast([128, nh, half]), sck)
        cosb = (sca[:, 24 + off:24 + off + half].unsqueeze(1).to_broadcast([128, nh, half]), sck)
        sa, sk_ = _k(src)
        da, dk_ = _k(dst)
        x1 = (sa[:, :, 0:half], sk_)
        x2 = (sa[:, :, half:2 * half], sk_)
        t1 = rtmp[:, 0:nh, 0:half]
        t2 = rtmp2[:, 0:nh, 0:half]
        P.tt(t1, x1, cosb, ALU.mult)
        P.tt(t2, x2, sinb, ALU.mult)
        P.tt((da[:, :, 0:half], dk_), t1, t2, ALU.subtract)
        t3 = rtmp[:, 0:nh, 16:16 + half]
        t4 = rtmp2[:, 0:nh, 16:16 + half]
        P.tt(t3, x2, cosb, ALU.mult)
        P.tt(t4, x1, sinb, ALU.mult)
        P.tt((da[:, :, half:2 * half], dk_), t3, t4, ALU.add)
        P.copy((da[:, :, 2 * half:dim], dk_), (sa[:, :, 2 * half:dim], sk_), eng="act")

    def front(xtile, bank):
        sumsq(xtile, D, st[:, 0:1])
        rstd_from_ss(st[:, 1:2], st[:, 0:1], D)
        P.act(xsb[:], xtile, AF.Copy, scale=st[:, 1:2])
        for c in range(8):
            P.transpose(psb(bank)[:, c * 128:(c + 1) * 128], xsb[:, c * 128:(c + 1) * 128], ident[:])
        P.copy(xsT[:].rearrange("p c t -> p (c t)"), psb(bank))

    def sincos_batch(dst, pos_all, t0, t1, A, Af, Ai):
        n = t1 - t0
        Aa, Ak = A
        A3 = Aa.rearrange("p (t c) -> p t c", c=48)
        invb = invf[:].unsqueeze(1).to_broadcast([128, n, 24])
        posb = pos_all[:, t0:t1].unsqueeze(2).to_broadcast([128, n, 24])
        P.tt((A3[:, :, 0:24], Ak), invb, posb, ALU.mult)
        P.ts((A3[:, :, 24:48], Ak), (A3[:, :, 0:24], Ak), 0.5 * PI, None, ALU.add)
        P.ts(Af, A, 1.0 / (2 * PI), None, ALU.mult)
        P.copy(Ai, Af)
        P.copy(Af, Ai)
        P.stt(A, Af, -2 * PI, A, ALU.mult, ALU.add)
        P.ts(Af, A, PI, None, ALU.is_gt)
        P.stt(A, Af, -2 * PI, A, ALU.mult, ALU.add)
        P.ts(Af, A, -PI, None, ALU.is_lt)
        P.stt(A, Af, 2 * PI, A, ALU.mult, ALU.add)
        P.act(dst, A, AF.Sin)


    def ktile(kt):
        xtile = xt[kt % 2]
        P.dma(xtile[:], x_seq[kt // TPP][(kt % TPP) * 128:(kt % TPP + 1) * 128, :])
        front(xtile[:], 7)
        for c in range(8):
            P.matmul(ps[0][:, 0:320], xsT[:, c, :], wk[:, c, :], start=(c == 0), stop=(c == 7))
        sincos(posk[:, kt:kt + 1])
        sckt = None
        for _d in range(NDUMMY):
            P.act(junkb[:], junkb[:], AF.Copy)
        sumsq(ps[0][:, 0:256], 256, st[:, 2:3])
        rstd_from_ss(st[:, 3:4], st[:, 2:3], 256)
        P.act(cnb[:], ps[0][:, 0:256], AF.Copy, scale=st[:, 3:4])
        P.copy(kif[:], ps[0][:, 256:320])
        rope(kibf[:, 0:64].rearrange("p (h d) -> p h d", h=1), kif[:].rearrange("p (h d) -> p h d", h=1), 1, 64, 8, 16, sckt)
        for c in range(2):
            P.transpose(psb(6)[:, c * 128:(c + 1) * 128], cnb[:, c * 128:(c + 1) * 128], ident[:])
        P.copy(cnT[:].rearrange("p c t -> p (c t)"), psb(6)[:, 0:256])
        for c in range(2):
            P.matmul(ps[1][:, 0:256], cnT[:, c, :], wkv[:, c, :], start=(c == 0), stop=(c == 1))
        if dbg == "K3":
            return
        sumsq(ps[1][:, 0:128], 128, st[:, 4:5])
        rstd_from_ss(st[:, 5:6], st[:, 4:5], 128)
        P.stt(kn[:], ps[1][:, 0:128], st[:, 5:6], kgb[:], ALU.mult, ALU.mult)
        rope(kbf[:].rearrange("p (h d) -> p h d", h=1), kn[:].rearrange("p (h d) -> p h d", h=1), 1, 128, 16, 0, sckt)
        P.copy(vvb[:], ps[1][:, 128:256], eng="act")
        P.transpose(psb(5)[:, 0:128], kbf[:], ident[:])
        P.transpose(psb(5)[:, 128:256], kibf[:], ident[:])
        P.copy(kTs[:], psb(5)[:, 0:128], eng="act")
        P.copy(kiTs[:], psb(5)[0:64, 128:256], eng="act")
        if dbg == "K4":
            return
        if dbg != "K6":
            P.dma(vv_d[kt * 128:(kt + 1) * 128, :], vvb[:])
        if dbg == "K5":
            return
        P.dma(kT_d[kt], kTs[:])
        if dbg == "K6":
            return
        P.dma(kiT_d[kt], kiTs[:])


    if dbg and dbg[0] == "K":
        for kt in range(KT0, NKT if KT1 is None else KT1):
            ktile(kt)

    if dbg and dbg[0] == "K":
        P.dma(out_d[0:128, :], xt[0][:])
        P.emit(es)
        es.close()
        return nc

    wbuf = [T("wbuf%d" % i, [128, 2048], BF16) for i in range(3)]
    wctr = [0]

    def wstream(src, a, b):
        buf = wbuf[wctr[0] % 3]
        wctr[0] += 1
        view = buf[:, 0:a * b].rearrange("p (a b) -> p a b", a=a)
        P.dma(view, src.rearrange("(c p) n -> p c n", p=128))
        return view

    ug = T("ug", [128, 512])
    vg = T("vg", [128, 512])
    gt = T("gt", [128, 1024])
    vnbf = T("vnbf", [128, 512], BF16)
    yab = T("yab", [128, 512], BF16)
    yaT = T("yaT", [128, 4, 128], BF16)
    sg = T("sg", [128, 2048])
    merged = T("merged", [128, D])
    mbf = T("mbf", [128, D], BF16)
    mT = T("mT", [128, 8, 128], BF16)
    qn = T("qn", [128, 4, 128])
    qbf = T("qbf", [128, 4, 128], BF16)
    qT = T("qT", [128, 4, 128], BF16)
    qir = T("qir", [128, 4, 64])
    qibf = T("qibf", [128, 4, 128], BF16)
    qiT = T("qiT", [128, 4, 128], BF16)
    P.memset(qibf[:], 0.0)
    kich = [T("kich%d" % i, [64, 512], BF16) for i in range(2)]
    kch = [T("kch%d" % i, [128, 512], BF16) for i in range(2)]
    vch = [T("vch%d" % i, [128, 4, 128], BF16) for i in range(2)]
    rl = [T("rl%d" % i, [128, 512]) for i in range(4)]
    maskc = T("maskc", [128, 512], BF16)
    maskT = T("maskT", [128, 4, 128], BF16)
    eT = [T("eT%d" % i, [128, 512], BF16) for i in range(2)]
    eTm = [T("eTm%d" % i, [128, 512], BF16) for i in range(2)]
    rz = T("rz", [128, 512])
    ybT = T("ybT", [128, 4, 128], BF16)
    hres = T("hres", [128, D])
    hsb = T("hsb", [128, D], BF16)
    hsT = T("hsT", [128, 8, 128], BF16)
    qpT = T("qpT", [128, 16, 128], BF16)
    vals = T("vals", [128, 16, 16])
    idxu = T("idxu", [128, 16, 16], U32)
    idxf = T("idxf", [128, 16, 16])
    best = T("best", [128, 8, 16])
    posu = T("posu", [128, 8, 16], U32)
    pa_i = T("pa_i", [128, 8, 16], U32)
    pb_i = T("pb_i", [128, 8, 16], U32)
    pa_f = T("pa_f", [128, 8, 16])
    pb_f = T("pb_f", [128, 8, 16])
    sel1 = T("sel1", [128, 8, 16])
    sel2 = T("sel2", [128, 8, 16])
    eidf = T("eidf", [128, 128])
    eid = T("eid", [128, 128], I32)
    gate = T("gate", [128, 8, 16])
    adot = T("adot", [128, 128])
    cw = T("cw", [128, 128])
    gtmp = T("gtmp", [128, 128])

    def gelu(dst, src, tmp, n):
        P.tt(tmp, src, src, ALU.mult)
        P.ts(tmp, tmp, 0.044715, 1.0, ALU.mult, ALU.add)
        P.tt(tmp, tmp, src, ALU.mult)
        P.act(tmp, tmp, AF.Sigmoid, scale=1.5957691216057308)
        P.tt(dst, src, tmp, ALU.mult)

    def top16(vout, iout, src, tmp):
        va, vk = _k(vout)
        ia, ik = _k(iout)
        rk, wk_ = P._rw([vout], [src])
        P.op("dve", lambda e, o=va[:, 0:8], i=_k(src)[0]: e.max(o, i), rk, wk_)
        rk, wk_ = P._rw([iout], [vout, src])
        P.op("dve", lambda e, o=ia[:, 0:8], m=va[:, 0:8], i=_k(src)[0]: e.max_index(o, m, i), rk, wk_)
        rk, wk_ = P._rw([tmp], [vout, src])
        P.op("dve", lambda e, o=_k(tmp)[0], m=va[:, 0:8], i=_k(src)[0]: e.match_replace(o, m, i, -1e30), rk, wk_)
        rk, wk_ = P._rw([vout], [tmp])
        P.op("dve", lambda e, o=va[:, 8:16], i=_k(tmp)[0]: e.max(o, i), rk, wk_)
        rk, wk_ = P._rw([iout], [vout, tmp])
        P.op("dve", lambda e, o=ia[:, 8:16], m=va[:, 8:16], i=_k(tmp)[0]: e.max_index(o, m, i), rk, wk_)

    proj = av(0, INW)
    pj = proj[0]
    pjk = proj[1]

    for j in range(NOWN):
        nch = (4 * j + 4) * 128 // 512
        for kt in range(4 * j, 4 * j + 4):
            ktile(kt)
        xres = xt[j % 2]
        P.dma(xres[:], x_own[j * 128:(j + 1) * 128, :])
        front(xres[:], 7)
        sincos(poso[:, j:j + 1])
        ncc = (INW + 255) // 256
        for cc in range(ncc):
            c0 = cc * 256
            w = min(256, INW - c0)
            wbf = wstream(winbf[:, c0:c0 + w], 8, w)
            bank = cc % 2
            for c in range(8):
                P.matmul(ps[bank][:, 0:w], xsT[:, c, :], wbf[:, c, :], start=(c == 0), stop=(c == 7))
            P.copy((pj[:, c0:c0 + w], pjk), ps[bank][:, 0:w], eng=("act" if cc % 2 else "dve"))
        gelu(ug[:], (pj[:, 0:512], pjk), gt[:, 0:512], 512)
        gelu(vg[:], (pj[:, 512:1024], pjk), gt[:, 512:1024], 512)
        P.reduce(st[:, 8:9], vg[:], ALU.add)
        P.dot(junkf32[:, 0:512], vg[:], vg[:], st[:, 9:10])
        P.ts(st[:, 10:11], st[:, 8:9], 1.0 / 512, None, ALU.mult)
        P.tt(st[:, 11:12], st[:, 10:11], st[:, 10:11], ALU.mult)
        P.stt(st[:, 12:13], st[:, 9:10], 1.0 / 512, st[:, 11:12], ALU.mult, ALU.subtract)
        rstd_from_ss(st[:, 12:13], st[:, 12:13], 1)
        P.ts(vg[:], vg[:], st[:, 10:11], st[:, 12:13], ALU.subtract, ALU.mult)
        P.tt(vg[:], vg[:], vng[:], ALU.mult)
        P.tt(vnbf[:], vg[:], vnb[:], ALU.add)
        for g in range(4):
            P.matmul(ps[2][:, g * 128:(g + 1) * 128], wsT[:, g, :], vnbf[:, g * 128:(g + 1) * 128])
        for g in range(4):
            P.stt(yab[:, g * 128:(g + 1) * 128], ps[2][:, g * 128:(g + 1) * 128], spb[:, g:g + 1],
                  ug[:, g * 128:(g + 1) * 128], ALU.add, ALU.mult)
        for c in range(4):
            P.transpose(psb(3)[:, c * 128:(c + 1) * 128], yab[:, c * 128:(c + 1) * 128], ident[:])
        P.copy(yaT[:].rearrange("p c t -> p (c t)"), psb(3)[:, 0:512])
        for hf in range(2):
            wv = wstream(wabf[:, hf * 512:(hf + 1) * 512], 4, 512)
            for c in range(4):
                P.matmul(ps[4 + hf][:], yaT[:, c, :], wv[:, c, :], start=(c == 0), stop=(c == 3))
        P.act(sg[:], (pj[:, 2116:4164], pjk), AF.Sigmoid)
        for hf in range(2):
            P.tt(merged[:, hf * 512:(hf + 1) * 512], sg[:, hf * 512:(hf + 1) * 512], ps[4 + hf][:], ALU.mult)
        P.tt(junkf32[:, 0:512], (pj[:, 1024:1536], pjk), (pj[:, 1024:1536], pjk), ALU.mult)
        P.reduce(st[:, 16:20], junkf32[:, 0:512].rearrange("p (h d) -> p h d", h=4), ALU.add)
        rstd_from_ss(st[:, 20:24], st[:, 16:20], 128)
        for h in range(4):
            P.stt(qn[:, h, :], (pj[:, 1024 + h * 128:1024 + (h + 1) * 128], pjk), st[:, 20 + h:21 + h], qgb[:], ALU.mult, ALU.mult)
        rope(qbf[:], qn[:], 4, 128, 16, 0)
        for h in range(4):
            P.transpose(psb(6)[:, h * 128:(h + 1) * 128], qbf[:, h, :], ident[:])
        P.copy(qT[:].rearrange("p h t -> p (h t)"), psb(6)[:, 0:512])
        wid = (pj[:, 2112:2116], pjk)
        P.ts(st[:, 28:32], wid, 0.0, 2.0, ALU.is_gt, ALU.mult)
        P.ts(st[:, 28:32], st[:, 28:32], -1.0, None, ALU.add)
        P.stt(st[:, 24:28], wid, 0.0625, st[:, 28:32], ALU.mult, ALU.mult)
        rope(qir[:], (pj[:, 1792:2048].rearrange("p (h d) -> p h d", h=4), pjk), 4, 64, 8, 16)
        P.tt(qibf[:, :, 0:64], qir[:], st[:, 24:28].unsqueeze(2).to_broadcast([128, 4, 64]), ALU.mult)
        for h in range(4):
            P.transpose(psb(7)[:, h * 128:(h + 1) * 128], qibf[:, h, :], ident[:])
        P.copy(qiT[:].rearrange("p h t -> p (h t)"), psb(7)[:, 0:512])
        for c in range(nch):
            kb = kich[c % 2]
            P.dma(kb[:].rearrange("p (k t) -> p k t", k=4), kiT_d[c * 4:(c + 1) * 4].rearrange("k p t -> p k t"))
            scv = av(c * 512, (c + 1) * 512)
            for h in range(4):
                P.matmul(ps[h][:], qiT[0:64, h, :], kb[:])
            for h in range(4):
                P.act(rl[h][:], ps[h][:], AF.Relu)
            P.ts(scv, rl[0][:], st[:, 28:29], None, ALU.mult)
            for h in range(1, 4):
                P.stt(scv, rl[h][:], st[:, 28 + h:29 + h], scv, ALU.mult, ALU.add)
            if c == nch - 1:
                P.tt(scv, scv, maskb[:], ALU.add)
        n = nch * 512
        lo, hi, mid, cnt, ge, dd = (st[:, 32:33], st[:, 33:34], st[:, 34:35], st[:, 35:36], st[:, 36:37], st[:, 37:38])
        cnts = st[:, 40:48]
        P.memset(mid, 0.0)
        pieces = [(p0, min(p0 + 2048, n)) for p0 in range(0, n, 2048)]
        hstep = 64.0
        ktop = min(TOPK, S // 4) - 0.5
        for it in range(BIS_ITERS):
            for pi_, (p0, p1) in enumerate(pieces):
                P.ts(junkb[:, 0:p1 - p0], av(p0, p1), mid, None, ALU.is_ge, ALU.add, accum=cnts[:, pi_:pi_ + 1])
            if len(pieces) > 1:
                P.reduce(cnt, cnts[:, 0:len(pieces)], ALU.add)
                cc_ = cnt
            else:
                cc_ = cnts[:, 0:1]
            P.ts(dd, cc_, ktop, hstep, ALU.is_ge, ALU.mult)
            P.stt(mid, dd, -0.5 * hstep, mid, ALU.add, ALU.add)
            hstep *= 0.5
        P.ts(lo, mid, -hstep, None, ALU.add)
        nkb = nch * 4
        for c in range(nch):
            kc = kch[c % 2]
            vc = vch[c % 2]
            P.dma(kc[:].rearrange("p (k t) -> p k t", k=4), kT_d[c * 4:(c + 1) * 4].rearrange("k p t -> p k t"))
            P.dma(vc[:], vv_d[c * 512:(c + 1) * 512, :].rearrange("(kb p) d -> p kb d", p=128))
            P.ts(maskc[:], av(c * 512, (c + 1) * 512), lo, None, ALU.is_ge)
            for b in range(4):
                P.transpose(psb(0)[:, b * 128:(b + 1) * 128], maskc[:, b * 128:(b + 1) * 128], ident[:])
            P.copy(maskT[:].rearrange("p b q -> p (b q)"), psb(0)[:, 0:512])
            for b in range(4):
                gkb = c * 4 + b
                bank = 1 + (gkb % 2)
                P.matmul(ps[bank][:], kc[:, b * 128:(b + 1) * 128], qT[:].rearrange("p h t -> p (h t)"))
                e1 = eT[gkb % 2]
                e2 = eTm[gkb % 2]
                P.act(e1[:], ps[bank][:], AF.Exp, scale=float(128 ** -0.5))
                P.tt(e2[:].rearrange("p (h q) -> p h q", h=4), e1[:].rearrange("p (h q) -> p h q", h=4),
                     maskT[:, b, :].unsqueeze(1).to_broadcast([128, 4, 128]), ALU.mult)
                P.matmul(ps[4][:], vc[:, b, :], e2[:], start=(gkb == 0), stop=(gkb == nkb - 1))
                P.matmul(ps[5][:], ones[:], e2[:], start=(gkb == 0), stop=(gkb == nkb - 1))
        if dbg == "H" and j == NOWN - 1:
            P.dma(d_mT.ap(), maskT[:].rearrange("p b q -> p (b q)"))
            P.dma(d_e1.ap(), eT[(nkb - 1) % 2][:])
            P.dma(d_e2.ap(), eTm[(nkb - 1) % 2][:])
            P.copy(gt[:, 0:512], ps[4][:])
            P.dma(d_num.ap(), gt[:, 0:512])
        P.op("dve", lambda e: e.reciprocal(rz[:], ps[5][:]), ["ps5"], ["rz"])
        if dbg == "H" and j == NOWN - 1:
            P.dma(d_rz.ap(), rz[:])
        P.copy(gt[:, 512:1024], ps[4][:], eng="act")
        P.tt(ybT[:].rearrange("p h t -> p (h t)"), gt[:, 512:1024], rz[:], ALU.mult)
        for hf in range(2):
            wv = wstream(wbbf[:, hf * 512:(hf + 1) * 512], 4, 512)
            for h in range(4):
                P.matmul(ps[6 + hf][:], ybT[:, h, :], wv[:, h, :], start=(h == 0), stop=(h == 3))
        for hf in range(2):
            P.tt(gt[:, hf * 512:(hf + 1) * 512], sg[:, 1024 + hf * 512:1024 + (hf + 1) * 512], ps[6 + hf][:], ALU.mult)
        if dbg == "H" and j == NOWN - 1:
            P.dma(d_yb.ap(), ybT[:].rearrange("p h t -> p (h t)"))
            P.dma(d_gb.ap(), gt[:])
        P.tt(mbf[:], merged[:], gt[:], ALU.add)
        for c in range(8):
            P.transpose(psb(0)[:, c * 128:(c + 1) * 128], mbf[:, c * 128:(c + 1) * 128], ident[:])
        P.copy(mT[:].rearrange("p c t -> p (c t)"), psb(0))
        for qd in range(4):
            wv = wstream(wobf[:, qd * 256:(qd + 1) * 256], 8, 256)
            for c in range(8):
                P.matmul(ps[1 + qd // 2][:, (qd % 2) * 256:(qd % 2 + 1) * 256], mT[:, c, :], wv[:, c, :],
                         start=(c == 0), stop=(c == 7))
        for hf in range(2):
            P.tt(hres[:, hf * 512:(hf + 1) * 512], xres[:, hf * 512:(hf + 1) * 512], ps[1 + hf][:], ALU.add)
        if dbg == "H":
            P.dma(out_d[j * 128:(j + 1) * 128, :], hres[:])
            continue
        hn = av(0, 1024)
        sub = av(1024, 3072)
        sub2 = av(3072, 5120)
        cand = av(5120, 7168)
        oh = av(7168, 9216)
        junkf = av(9216, 10240)
        gbuf = [av(10240 + r * 1024, 10240 + (r + 1) * 1024) for r in range(6)]
        sumsq(hres[:], D, st[:, 48:49])
        rstd_from_ss(st[:, 49:50], st[:, 48:49], D)
        P.act(hsb[:], hres[:], AF.Copy, scale=st[:, 49:50])
        P.stt(hn, hres[:], st[:, 49:50], g2b[:], ALU.mult, ALU.mult)
        for c in range(8):
            P.transpose(psb(2)[:, c * 128:(c + 1) * 128], hsb[:, c * 128:(c + 1) * 128], ident[:])
        P.copy(hsT[:].rearrange("p c t -> p (c t)"), psb(2))
        for gp in range(8):
            wv = wstream(wqbf[:, gp * 256:(gp + 1) * 256], 8, 256)
            for g2_ in range(2):
                g = gp * 2 + g2_
                bank = 3 + g // 4
                for c in range(8):
                    P.matmul(ps[bank][:, (g % 4) * 128:(g % 4 + 1) * 128], wv[:, c, g2_ * 128:(g2_ + 1) * 128], hsT[:, c, :],
                             start=(c == 0), stop=(c == 7))
        for b in range(4):
            P.copy(qpT[:, b * 4:(b + 1) * 4, :].rearrange("p g t -> p (g t)"), ps[3 + b][:], eng=("act" if b % 2 else "dve"))
        sbanks = [7, 0, 1, 2]
        for g in range(16):
            P.matmul(ps[sbanks[g // 4]][:, (g % 4) * 128:(g % 4 + 1) * 128], qpT[:, g, :], skT[:, g, :])
        for b in range(4):
            P.copy((sub[0][:, b * 512:(b + 1) * 512], sub[1]), ps[sbanks[b]][:], eng=("act" if b % 2 else "dve"))
        for g in range(16):
            top16(vals[:, g, :], idxu[:, g, :], (sub[0][:, g * 128:(g + 1) * 128], sub[1]),
                  (sub2[0][:, g * 128:(g + 1) * 128], sub2[1]))
        v4 = vals[:].rearrange("p (h t) k -> p h t k", t=2)
        c4 = (cand[0].rearrange("p (h a b) -> p h a b", h=8, a=16), cand[1])
        P.tt(c4, v4[:, :, 0, :].unsqueeze(3).to_broadcast([128, 8, 16, 16]),
             v4[:, :, 1, :].unsqueeze(2).to_broadcast([128, 8, 16, 16]), ALU.add)
        for h in range(8):
            top16(best[:, h, :], posu[:, h, :], (cand[0][:, h * 256:(h + 1) * 256], cand[1]),
                  (sub2[0][:, h * 256:(h + 1) * 256], sub2[1]))
        P.op("dve", lambda e: e.tensor_single_scalar(pa_i[:], posu[:], 4, ALU.logical_shift_right), ["posu"], ["pa_i"])
        P.op("dve", lambda e: e.tensor_single_scalar(pb_i[:], posu[:], 15, ALU.bitwise_and), ["posu"], ["pb_i"])
        P.copy(pa_f[:], pa_i[:])
        P.copy(pb_f[:], pb_i[:])
        P.copy(idxf[:], idxu[:])
        i4 = idxf[:].rearrange("p (h t) k -> p h t k", t=2)
        o4 = (oh[0].rearrange("p (h k a) -> p h k a", h=8, k=16), oh[1])
        iob = iota16[:].unsqueeze(1).unsqueeze(1).to_broadcast([128, 8, 16, 16])
        for (pf, t_, so) in ((pa_f, 0, sel1), (pb_f, 1, sel2)):
            P.tt(o4, iob, pf[:].unsqueeze(3).to_broadcast([128, 8, 16, 16]), ALU.is_equal)
            P.tt(o4, o4, i4[:, :, t_, :].unsqueeze(2).to_broadcast([128, 8, 16, 16]), ALU.mult)
            P.reduce(so[:], o4, ALU.add)
        P.stt(eidf[:].rearrange("p (h k) -> p h k", h=8), sel1[:], 128.0, sel2[:], ALU.mult, ALU.add)
        P.copy(eid[:], eidf[:])
        P.reduce(st[:, 50:58], best[:], ALU.max)
        P.tt(gate[:], best[:], st[:, 50:58].unsqueeze(2).to_broadcast([128, 8, 16]), ALU.subtract)
        P.act(gate[:], gate[:], AF.Exp)
        P.reduce(rz[:, 0:8], gate[:], ALU.add)
        P.op("dve", lambda e: e.reciprocal(rz[:, 8:16], rz[:, 0:8]), ["rz"], ["rz"])
        P.tt(gate[:], gate[:], rz[:, 8:16].unsqueeze(2).to_broadcast([128, 8, 16]), ALU.mult)
        for s_ in range(128):
            gb = gbuf[s_ % 6]
            P.gather(gb, u_tab.ap(), eid[:, s_:s_ + 1])
            P.dot(junkf, gb, hn, adot[:, s_:s_ + 1])
        gelu(cw[:], adot[:], gtmp[:], 128)
        P.tt(cw[:], cw[:], gate[:].rearrange("p h k -> p (h k)"), ALU.mult)
        for s_ in range(128):
            gb = gbuf[s_ % 6]
            P.gather(gb, v_tab.ap(), eid[:, s_:s_ + 1])
            P.stt(hres[:], gb, cw[:, s_:s_ + 1], hres[:], ALU.mult, ALU.add)
        P.dma(out_d[j * 128:(j + 1) * 128, :], hres[:])

    P.emit(es)
    es.close()
    return nc


def _host_inputs(inp, S, NOWN):
    f32 = np.float32
    B = inp["x"].shape[0]
    x = np.asarray(inp["x"], f32)
    pos = np.asarray(inp["positions"], np.int32)
    NKT = S // 128
    rep = lambda v, n=128: np.ascontiguousarray(np.broadcast_to(np.asarray(v, f32).reshape(1, -1), (n, np.asarray(v).size)))
    pc = lambda v, c: np.ascontiguousarray(np.asarray(v, f32).reshape(c, 128).T)
    half_q = np.arange(16, dtype=f32)
    half_i = np.arange(8, dtype=f32)
    inv_q = np.power(f32(500000.0), -half_q * f32(2.0) / f32(32)).astype(f32)
    inv_i = np.power(f32(500000.0), -half_i * f32(2.0) / f32(16)).astype(f32)
    invf = rep(np.concatenate([inv_q, inv_i]))
    common = {
        "w_in": np.ascontiguousarray(inp["w_in"][0], f32),
        "g1": pc(inp["norm1_g"][0], 8),
        "g2": pc(inp["norm2_g"][0], 8),
        "g2b": rep(inp["norm2_g"][0]),
        "kvg": pc(inp["kv_norm_g"][0], 2),
        "vng": rep(inp["v_norm_g"][0]),
        "vnb": rep(inp["v_norm_b"][0]),
        "spw": np.ascontiguousarray(inp["spatial_w"][0], f32),
        "spb": np.ascontiguousarray(np.asarray(inp["spatial_b"][0], f32).T),
        "wuk": np.ascontiguousarray(inp["w_uk"][0], f32),
        "wuv": np.ascontiguousarray(inp["w_uv"][0], f32),
        "qgb": rep(inp["q_norm_g"][0]),
        "kgb": rep(inp["k_norm_g"][0]),
        "wa": np.ascontiguousarray(inp["w_a_out"][0], f32),
        "wb": np.ascontiguousarray(inp["w_b_out"][0], f32),
        "wo": np.ascontiguousarray(inp["w_o"][0], f32),
        "wq": np.ascontiguousarray(inp["peer_wq"][0], f32),
        "subk": np.ascontiguousarray(np.asarray(inp["peer_subkeys"][0], f32).reshape(16, 128, 128)),
        "peer_u": np.ascontiguousarray(inp["peer_u"][0], f32),
        "peer_v": np.ascontiguousarray(inp["peer_v"][0], f32),
        "ident": np.eye(128, dtype=f32).astype(ml_dtypes.bfloat16),
        "tril": np.tril(np.ones((128, 128), f32)),
        "invf": invf,
        "iota16": rep(np.arange(16, dtype=f32)),
    }
    maps = []
    lanes = 8 // B
    for core in range(8):
        b, c = core // lanes, core % lanes
        tiles = [lanes * j + c for j in range(NOWN)]
        rows = np.concatenate([np.arange(t * 128, (t + 1) * 128) for t in tiles])
        mb = np.zeros((128, 512), f32)
        for r in range(4):
            blk = mb[:, r * 128:(r + 1) * 128]
            if r > c:
                blk[:] = -1e30
            elif r == c:
                blk[:] = np.where(np.arange(128)[None, :] <= np.arange(128)[:, None], 0.0, -1e30)
        m = dict(common)
        for i in range(4):
            m["x_seq%d" % i] = np.ascontiguousarray(x[b][i * (S // 4):(i + 1) * (S // 4)])
        m["x_own"] = np.ascontiguousarray(x[b][rows])
        m["pos_seq"] = np.ascontiguousarray(pos[b].reshape(NKT, 128).T)
        m["pos_own"] = np.ascontiguousarray(pos[b][rows].reshape(NOWN, 128).T)
        m["maskb"] = mb
        maps.append((m, b, rows))
    return maps


_NC_CACHE = {}


def kernel(**inputs):
    x = np.asarray(inputs["x"])
    B, S, _ = x.shape
    lanes = 8 // B
    NOWN = S // 128 // lanes
    key = (S, NOWN)
    if key not in _NC_CACHE:
        _NC_CACHE[key] = build_program(S, NOWN)
    nc = _NC_CACHE[key]
    maps = _host_inputs(inputs, S, NOWN)
    res = run_bass_kernel_spmd(nc, [m for m, _, _ in maps], core_ids=list(range(8)))
    out = np.empty((B, S, D), np.float32)
    for (m, b, rows), r in zip(maps, res.results):
        out[b, rows] = r["out"]
    return out
```

```python
import numpy as np
import ml_dtypes
from contextlib import ExitStack
import concourse.bass as bass
import concourse.mybir as mybir
from concourse.bass_utils import run_bass_kernel_spmd

F32 = mybir.dt.float32
BF16 = mybir.dt.bfloat16
I32 = mybir.dt.int32
U32 = mybir.dt.uint32
ALU = mybir.AluOpType
AF = mybir.ActivationFunctionType
AX = mybir.AxisListType

D = 1024
INW = 4164
EPS = 1e-6
TOPK = 256
NEXP = 16384
PI = float(np.pi)
ENGS = ("pe", "act", "dve", "pool", "sp")
KDMA = 8
BIS_ITERS = 25
KT0 = 0
NDUMMY = 0
KT1 = None


def _k(x):
    if isinstance(x, tuple):
        return x
    return (x, (x.name,))


class Prog:
    def __init__(self, nc):
        self.nc = nc
        self.ops = {e: [] for e in ENGS}
        self.cnt = {e: 0 for e in ENGS}
        self.dcnt = {e: 0 for e in ENGS}
        self.waited = {e: {} for e in ENGS}
        self.res_w = {}
        self.res_r = {}

    def op(self, eng, fn, reads=(), writes=(), dma=False):
        waits = {}

        def addw(t):
            sk, v = t
            if waits.get(sk, 0) < v:
                waits[sk] = v

        for r in reads:
            if r in self.res_w:
                addw(self.res_w[r])
        for w in writes:
            if w in self.res_w:
                addw(self.res_w[w])
            for sk, v in self.res_r.get(w, {}).items():
                addw((sk, v))
        if dma:
            k = self.dcnt[eng]
            self.dcnt[eng] += 1
            slot = k % KDMA
            semkey = ("dma", eng, slot)
            val = 16 * (k // KDMA + 1)
            inc = 16
            if k >= KDMA:
                addw((semkey, val - 16))
        else:
            self.cnt[eng] += 1
            semkey = eng
            val = self.cnt[eng]
            inc = 1
        wl = []
        for sk, v in waits.items():
            if eng == "pe" and sk == "pe":
                continue
            if self.waited[eng].get(sk, 0) >= v:
                continue
            self.waited[eng][sk] = v
            wl.append((sk, v))
        self.ops[eng].append((wl, fn, semkey, inc))
        tok = (semkey, val)
        for r in reads:
            d = self.res_r.setdefault(r, {})
            if d.get(semkey, 0) < val:
                d[semkey] = val
        for w in writes:
            self.res_w[w] = tok
            self.res_r[w] = {}
        return tok

    def _rw(self, outs, ins):
        wk = []
        rk = []
        for o in outs:
            wk += list(_k(o)[1])
        for i in ins:
            if i is None or isinstance(i, (int, float)):
                continue
            rk += list(_k(i)[1])
        return rk, wk

    @staticmethod
    def _a(x):
        if x is None or isinstance(x, (int, float)):
            return x
        return _k(x)[0]

    def dot(self, junk, a, b, acc):
        rk, wk = self._rw([junk, acc], [a, b])
        j, x, y, c = self._a(junk), self._a(a), self._a(b), self._a(acc)
        self.op("dve", lambda e: e.scalar_tensor_tensor(j, x, 1.0, y, ALU.mult, ALU.mult, accum_out=c), rk, wk)

    def matmul(self, out, lhsT, rhs, start=True, stop=True):
        rk, wk = self._rw([out], [lhsT, rhs])
        o, l, r = self._a(out), self._a(lhsT), self._a(rhs)
        self.op("pe", lambda e: e.matmul(o, l, r, start=start, stop=stop), rk, wk)

    def transpose(self, out, in_, ident):
        rk, wk = self._rw([out], [in_])
        o, i, d = self._a(out), self._a(in_), self._a(ident)
        self.op("pe", lambda e: e.transpose(o, i, d), rk, wk)

    def act(self, out, in_, func, bias=None, scale=None, accum=None, eng="act"):
        outs = [out] + ([accum] if accum is not None else [])
        rk, wk = self._rw(outs, [in_, bias, scale])
        o, i, b, s, a = self._a(out), self._a(in_), self._a(bias), self._a(scale), self._a(accum)
        kw = {}
        if b is not None:
            kw["bias"] = b
        if s is not None:
            kw["scale"] = s
        if a is not None:
            kw["accum_out"] = a
        self.op("act", lambda e: e.activation(o, i, func, **kw), rk, wk)

    def ts(self, out, in0, s1, s2, op0, op1=None, accum=None, eng="dve"):
        outs = [out] + ([accum] if accum is not None else [])
        rk, wk = self._rw(outs, [in0, s1, s2])
        o, i, a1, a2, ac = self._a(out), self._a(in0), self._a(s1), self._a(s2), self._a(accum)
        kw = {}
        if op1 is not None:
            kw["op1"] = op1
        if ac is not None:
            kw["accum_out"] = ac
        self.op(eng, lambda e: e.tensor_scalar(o, i, a1, a2, op0, **kw), rk, wk)

    def tt(self, out, in0, in1, op, eng="dve"):
        rk, wk = self._rw([out], [in0, in1])
        o, i0, i1 = self._a(out), self._a(in0), self._a(in1)
        self.op(eng, lambda e: e.tensor_tensor(o, i0, i1, op), rk, wk)

    def stt(self, out, in0, scalar, in1, op0, op1, eng="dve"):
        rk, wk = self._rw([out], [in0, scalar, in1])
        o, i0, s, i1 = self._a(out), self._a(in0), self._a(scalar), self._a(in1)
        self.op(eng, lambda e: e.scalar_tensor_tensor(o, i0, s, i1, op0, op1), rk, wk)

    def copy(self, out, in_, eng="dve"):
        rk, wk = self._rw([out], [in_])
        o, i = self._a(out), self._a(in_)
        if eng == "act":
            self.op("act", lambda e: e.activation(o, i, AF.Copy), rk, wk)
        else:
            self.op(eng, lambda e: e.tensor_copy(o, i), rk, wk)

    def reduce(self, out, in_, op, eng="dve"):
        rk, wk = self._rw([out], [in_])
        o, i = self._a(out), self._a(in_)
        self.op(eng, lambda e: e.tensor_reduce(o, i, AX.X, op), rk, wk)

    def memset(self, out, val, eng="dve"):
        rk, wk = self._rw([out], [])
        o = self._a(out)
        self.op(eng, lambda e: e.memset(o, val), rk, wk)

    def dma(self, out, in_, eng="sp"):
        rk, wk = self._rw([out], [in_])
        o, i = self._a(out), self._a(in_)
        self.op(eng, lambda e: e.dma_start(out=o, in_=i), rk, wk, dma=True)

    def gather(self, out, table, idx):
        rk, wk = self._rw([out], [idx])
        o, t, i = self._a(out), self._a(table), self._a(idx)
        self.op(
            "pool",
            lambda e: e.indirect_dma_start(
                out=o, out_offset=None, in_=t, in_offset=bass.IndirectOffsetOnAxis(ap=i, axis=0)
            ),
            rk,
            wk,
            dma=True,
        )

    def emit(self, es):
        nc = self.nc
        sems = {}
        for e in ENGS:
            sems[e] = es.enter_context(nc.semaphore("s_" + e))
            for s in range(KDMA):
                sems[("dma", e, s)] = es.enter_context(nc.semaphore("d_%s_%d" % (e, s)))
        block = es.enter_context(nc.Block())

        def run(engobj, name):
            for wl, fn, semkey, inc in self.ops[name]:
                for sk, v in wl:
                    engobj.wait_ge(sems[sk], v)
                fn(engobj).then_inc(sems[semkey], inc)
            k = self.dcnt[name]
            for s in range(min(k, KDMA)):
                n = (k - 1 - s) // KDMA + 1
                engobj.wait_ge(sems[("dma", name, s)], 16 * n)

        @block.tensor
        def _(e):
            run(e, "pe")

        @block.scalar
        def _(e):
            run(e, "act")

        @block.vector
        def _(e):
            run(e, "dve")

        @block.gpsimd
        def _(e):
            run(e, "pool")

        @block.sync
        def _(e):
            run(e, "sp")


def build_program(S, NOWN, dbg=None):
    NKT = S // 128
    nc = bass.Bass("TRN2", target_bir_lowering=False)
    es = ExitStack()
    P = Prog(nc)

    def din(name, shape, dt=F32):
        return nc.dram_tensor(name, list(shape), dt, kind="ExternalInput")

    NXP = 4
    TPP = NKT // NXP
    x_seq = [din("x_seq%d" % i, [S // NXP, D]) for i in range(NXP)]
    x_own = din("x_own", [NOWN * 128, D])
    pos_seq = din("pos_seq", [128, NKT], I32)
    pos_own = din("pos_own", [128, NOWN], I32)
    w_in = din("w_in", [D, INW])
    g1_d = din("g1", [128, 8])
    g2_d = din("g2", [128, 8])
    g2b_d = din("g2b", [128, D])
    kvg_d = din("kvg", [128, 2])
    vng_d = din("vng", [128, 512])
    vnb_d = din("vnb", [128, 512])
    spw_d = din("spw", [4, 128, 128])
    spb_d = din("spb", [128, 4])
    wuk_d = din("wuk", [256, 128])
    wuv_d = din("wuv", [256, 128])
    qg_d = din("qgb", [128, 128])
    kg_d = din("kgb", [128, 128])
    wa_d = din("wa", [512, D])
    wb_d = din("wb", [512, D])
    wo_d = din("wo", [D, D])
    wq_d = din("wq", [D, 2048])
    sk_d = din("subk", [16, 128, 128])
    if not dbg:
        u_tab = din("peer_u", [NEXP, D])
        v_tab = din("peer_v", [NEXP, D])
    ident_d = din("ident", [128, 128], BF16)
    tril_d = din("tril", [128, 128])
    maskb_d = din("maskb", [128, 512])
    inv_d = din("invf", [128, 24])
    iota_d = din("iota16", [128, 16])
    out_d = nc.dram_tensor("out", [NOWN * 128, D], F32, kind="ExternalOutput")
    okind = "ExternalOutput" if dbg else "Internal"
    winbf = nc.dram_tensor("winbf", [D, INW], BF16, kind="Internal")
    wabf = nc.dram_tensor("wabf", [512, D], BF16, kind="Internal")
    wbbf = nc.dram_tensor("wbbf", [512, D], BF16, kind="Internal")
    wobf = nc.dram_tensor("wobf", [D, D], BF16, kind="Internal")
    wqbf = nc.dram_tensor("wqbf", [D, 2048], BF16, kind="Internal")
    kiT_d = nc.dram_tensor("kiT_d", [NKT, 64, 128], BF16, kind=okind)
    kT_d = nc.dram_tensor("kT_d", [NKT, 128, 128], BF16, kind=okind)
    vv_d = nc.dram_tensor("vv_d", [S, 128], BF16, kind=okind)

    if dbg == "H":
        d_sc = nc.dram_tensor("d_sc", [128, 512], F32, kind="ExternalOutput")
        d_st = nc.dram_tensor("d_st", [128, 64], F32, kind="ExternalOutput")
        d_yb = nc.dram_tensor("d_yb", [128, 512], BF16, kind="ExternalOutput")
        d_ma = nc.dram_tensor("d_ma", [128, D], F32, kind="ExternalOutput")
        d_gb = nc.dram_tensor("d_gb", [128, D], F32, kind="ExternalOutput")
        d_q = nc.dram_tensor("d_q", [128, 512], BF16, kind="ExternalOutput")
        d_qi = nc.dram_tensor("d_qi", [128, 512], BF16, kind="ExternalOutput")
        d_mT = nc.dram_tensor("d_mT", [128, 512], BF16, kind="ExternalOutput")
        d_e1 = nc.dram_tensor("d_e1", [128, 512], BF16, kind="ExternalOutput")
        d_e2 = nc.dram_tensor("d_e2", [128, 512], BF16, kind="ExternalOutput")
        d_rz = nc.dram_tensor("d_rz", [128, 512], F32, kind="ExternalOutput")
        d_num = nc.dram_tensor("d_num", [128, 512], F32, kind="ExternalOutput")

    def T(name, shape, dt=F32):
        return es.enter_context(nc.sbuf_tensor(name, list(shape), dt))

    AW = 16384
    arena = T("arena", [128, AW])

    def av(lo, hi, dt=F32):
        keys = tuple(("sc", c) for c in range(lo // 512, (hi - 1) // 512 + 1))
        ap = arena[:, lo:hi]
        if dt is not F32:
            ap = ap.bitcast(dt)
        return (ap, keys)

    ident = T("identb", [128, 128], BF16)
    ones = T("onesb", [128, 128], BF16)
    negpi = T("negpi", [128, 1])
    epsb = T("epsb", [128, 1])
    g1 = T("g1s", [128, 8])
    g2 = T("g2s", [128, 8])
    g2b = T("g2bs", [128, D])
    kvg = T("kvgs", [128, 2])
    vng = T("vngs", [128, 512])
    vnb = T("vnbs", [128, 512])
    spb = T("spbs", [128, 4])
    qgb = T("qgbs", [128, 128])
    kgb = T("kgbs", [128, 128])
    trilm = T("trilm", [128, 128])
    maskb = T("maskbs", [128, 512])
    invf = T("invfs", [128, 24])
    iota16 = T("iota16s", [128, 16])
    posk_i = T("posk_i", [128, NKT], I32)
    posk = T("posk", [128, NKT])
    poso_i = T("poso_i", [128, NOWN], I32)
    poso = T("poso", [128, NOWN])
    wk = T("wk", [128, 8, 320], BF16)
    wkv = T("wkv", [128, 2, 256], BF16)
    skT = T("skT", [128, 16, 128], BF16)
    wsT = T("wsT", [128, 4, 128], BF16)

    ps = [es.enter_context(nc.psum_tensor("ps%d" % i, [128, 512], F32)) for i in range(8)]

    def psb(i):
        return ps[i][:].bitcast(BF16)

    for dst, src in ((ident, ident_d), (g1, g1_d), (g2, g2_d), (g2b, g2b_d), (kvg, kvg_d), (vng, vng_d),
                     (vnb, vnb_d), (spb, spb_d), (qgb, qg_d), (kgb, kg_d), (trilm, tril_d), (maskb, maskb_d),
                     (invf, inv_d), (iota16, iota_d), (posk_i, pos_seq), (poso_i, pos_own)):
        P.dma(dst[:], src.ap())
    P.memset(ones[:], 1.0)
    P.memset(negpi[:], -PI)
    P.memset(epsb[:], EPS)
    P.copy(posk[:], posk_i[:])
    P.copy(poso[:], poso_i[:])

    if dbg == "C0":
        P.dma(out_d[0:128, :], g2b[:])
        P.emit(es)
        es.close()
        return nc
    stg = av(0, INW)
    stgb = av(4608, 4608 + INW // 2, BF16)
    for c in range(8):
        P.dma(stg, w_in[c * 128:(c + 1) * 128, :])
        P.act(stgb, stg, AF.Copy, scale=g1[:, c:c + 1])
        P.dma(winbf[c * 128:(c + 1) * 128, :], stgb)
        P.copy(wk[:, c, 0:256], (stgb[0][:, 1536:1792], stgb[1]))
        P.copy(wk[:, c, 256:320], (stgb[0][:, 2048:2112], stgb[1]))
    s2 = av(8192, 8192 + 2048)
    s2b = av(12288, 12288 + 1024, BF16)
    for c in range(8):
        P.dma(s2, wq_d[c * 128:(c + 1) * 128, :])
        P.act(s2b, s2, AF.Copy, scale=g2[:, c:c + 1])
        P.dma(wqbf[c * 128:(c + 1) * 128, :], s2b)
    s3 = av(10240, 10240 + 1024)
    s3b = av(13312, 13312 + 512, BF16)
    for c in range(4):
        P.dma(s3, wa_d[c * 128:(c + 1) * 128, :])
        P.copy(s3b, s3)
        P.dma(wabf[c * 128:(c + 1) * 128, :], s3b)
        P.dma(s3, wb_d[c * 128:(c + 1) * 128, :])
        P.copy(s3b, s3, eng="act")
        P.dma(wbbf[c * 128:(c + 1) * 128, :], s3b)
    for c in range(8):
        P.dma(s3, wo_d[c * 128:(c + 1) * 128, :])
        P.copy(s3b, s3)
        P.dma(wobf[c * 128:(c + 1) * 128, :], s3b)
    s4 = av(11264, 11264 + 128)
    for c in range(2):
        P.dma(s4, wuk_d[c * 128:(c + 1) * 128, :])
        P.act(wkv[:, c, 0:128], s4, AF.Copy, scale=kvg[:, c:c + 1])
        P.dma(s4, wuv_d[c * 128:(c + 1) * 128, :])
        P.act(wkv[:, c, 128:256], s4, AF.Copy, scale=kvg[:, c:c + 1])
    s5 = av(11776, 11776 + 64, BF16)
    for g in range(16):
        P.dma(s4, sk_d[g])
        P.copy(s5, s4)
        P.transpose(psb(7)[:, 0:128], s5, ident[:])
        P.copy(skT[:, g, :], psb(7)[:, 0:128])
    for g in range(4):
        P.dma(s4, spw_d[g])
        P.tt(s5, s4, trilm[:], ALU.mult)
        P.transpose(psb(7)[:, 0:128], s5, ident[:])
        P.copy(wsT[:, g, :], psb(7)[:, 0:128])

    if dbg == "C1":
        P.dma(out_d[0:128, :], g2b[:])
        P.emit(es)
        es.close()
        return nc
    xt = [T("xt%d" % i, [128, D]) for i in range(2)]
    xsb = T("xsb", [128, D], BF16)
    xsT = T("xsT", [128, 8, 128], BF16)
    st = T("stats", [128, 64])
    ang = T("ang", [128, 24])
    a12 = T("a12", [128, 48])
    a12f = T("a12f", [128, 48])
    a12i = T("a12i", [128, 48], I32)
    sc = T("sincos", [128, 48])
    rtmp = T("rtmp", [128, 4, 64])
    rtmp2 = T("rtmp2", [128, 4, 64])
    junkb = T("junkb", [128, 2048], BF16)
    junkf32 = T("junkf32", [128, 1024])
    junkf32b = T("junkf32b", [128, 256])
    cnb = T("cnb", [128, 256], BF16)
    cnT = T("cnT", [128, 2, 128], BF16)
    kn = T("kn", [128, 128])
    kbf = T("kbf", [128, 128], BF16)
    kif = T("kif", [128, 64])
    kibf = T("kibf", [128, 128], BF16)
    P.memset(kibf[:], 0.0)
    vvb = T("vvb", [128, 128], BF16)
    kTs = T("kTs", [128, 128], BF16)
    kiTs = T("kiTs", [64, 128], BF16)
    pooldummy = T("pooldummy", [128, 8], BF16)

    def sumsq(src, n, acc):
        if n <= 256:
            P.copy(junkf32b[:, 0:n], src)
            src = junkf32b[:, 0:n]
        P.dot(junkf32[:, 0:n], src, src, acc)

    def rstd_from_ss(dst, ss, n):
        P.act(dst, ss, AF.Sqrt, bias=epsb[:, 0:1], scale=1.0 / n)
        rk, wk_ = P._rw([dst], [dst])
        P.op("dve", lambda e, o=_k(dst)[0]: e.reciprocal(o, o), rk, wk_)

    def sincos(pos_col):
        P.ts(a12[:, 0:24], invf[:], pos_col, None, ALU.mult)
        P.ts(a12[:, 24:48], invf[:], pos_col, 0.5 * PI, ALU.mult, ALU.add)
        P.ts(a12f[:], a12[:], 1.0 / (2 * PI), None, ALU.mult)
        P.copy(a12i[:], a12f[:])
        P.copy(a12f[:], a12i[:])
        P.stt(a12[:], a12f[:], -2 * PI, a12[:], ALU.mult, ALU.add)
        P.ts(a12f[:], a12[:], PI, None, ALU.is_gt)
        P.stt(a12[:], a12f[:], -2 * PI, a12[:], ALU.mult, ALU.add)
        P.ts(a12f[:], a12[:], -PI, None, ALU.is_lt)
        P.stt(a12[:], a12f[:], 2 * PI, a12[:], ALU.mult, ALU.add)
        P.act(sc[:], a12[:], AF.Sin)

    def rope(dst, src, nh, dim, half, off, scv=None):
        if scv is None:
            scv = sc[:]
        sca, sck = _k(scv)
        sinb = (sca[:, off:off + half].unsqueeze(1).to_broadcast([128, nh, half]), sck)
        cosb = (sca[:, 24 + off:24 + off + half].unsqueeze(1).to_broadcast([128, nh, half]), sck)
        sa, sk_ = _k(src)
        da, dk_ = _k(dst)
        x1 = (sa[:, :, 0:half], sk_)
        x2 = (sa[:, :, half:2 * half], sk_)
        t1 = rtmp[:, 0:nh, 0:half]
        t2 = rtmp2[:, 0:nh, 0:half]
        P.tt(t1, x1, cosb, ALU.mult)
        P.tt(t2, x2, sinb, ALU.mult)
        P.tt((da[:, :, 0:half], dk_), t1, t2, ALU.subtract)
        t3 = rtmp[:, 0:nh, 16:16 + half]
        t4 = rtmp2[:, 0:nh, 16:16 + half]
        P.tt(t3, x2, cosb, ALU.mult)
        P.tt(t4, x1, sinb, ALU.mult)
        P.tt((da[:, :, half:2 * half], dk_), t3, t4, ALU.add)
        P.copy((da[:, :, 2 * half:dim], dk_), (sa[:, :, 2 * half:dim], sk_), eng="act")

    def front(xtile, bank):
        sumsq(xtile, D, st[:, 0:1])
        rstd_from_ss(st[:, 1:2], st[:, 0:1], D)
        P.act(xsb[:], xtile, AF.Copy, scale=st[:, 1:2])
        for c in range(8):
            P.transpose(psb(bank)[:, c * 128:(c + 1) * 128], xsb[:, c * 128:(c + 1) * 128], ident[:])
        P.copy(xsT[:].rearrange("p c t -> p (c t)"), psb(bank))

    def sincos_batch(dst, pos_all, t0, t1, A, Af, Ai):
        n = t1 - t0
        Aa, Ak = A
        A3 = Aa.rearrange("p (t c) -> p t c", c=48)
        invb = invf[:].unsqueeze(1).to_broadcast([128, n, 24])
        posb = pos_all[:, t0:t1].unsqueeze(2).to_broadcast([128, n, 24])
        P.tt((A3[:, :, 0:24], Ak), invb, posb, ALU.mult)
        P.ts((A3[:, :, 24:48], Ak), (A3[:, :, 0:24], Ak), 0.5 * PI, None, ALU.add)
        P.ts(Af, A, 1.0 / (2 * PI), None, ALU.mult)
        P.copy(Ai, Af)
        P.copy(Af, Ai)
        P.stt(A, Af, -2 * PI, A, ALU.mult, ALU.add)
        P.ts(Af, A, PI, None, ALU.is_gt)
        P.stt(A, Af, -2 * PI, A, ALU.mult, ALU.add)
        P.ts(Af, A, -PI, None, ALU.is_lt)
        P.stt(A, Af, 2 * PI, A, ALU.mult, ALU.add)
        P.act(dst, A, AF.Sin)


    SC5 = [None]

    def ktile(kt):
        xtile = xt[kt % 2]
        P.dma(xtile[:], x_seq[kt // TPP][(kt % TPP) * 128:(kt % TPP + 1) * 128, :])
        front(xtile[:], 7)
        for c in range(8):
            P.matmul(ps[0][:, 0:320], xsT[:, c, :], wk[:, c, :], start=(c == 0), stop=(c == 7))
        if SC5[0] is not None:
            sckt = SC5[0][:, (kt % 4) * 48:(kt % 4 + 1) * 48]
        else:
            sincos(posk[:, kt:kt + 1])
            sckt = None
        for _d in range(NDUMMY):
            P.act(junkb[:], junkb[:], AF.Copy)
        sumsq(ps[0][:, 0:256], 256, st[:, 2:3])
        rstd_from_ss(st[:, 3:4], st[:, 2:3], 256)
        P.act(cnb[:], ps[0][:, 0:256], AF.Copy, scale=st[:, 3:4])
        P.copy(kif[:], ps[0][:, 256:320])
        rope(kibf[:, 0:64].rearrange("p (h d) -> p h d", h=1), kif[:].rearrange("p (h d) -> p h d", h=1), 1, 64, 8, 16, sckt)
        for c in range(2):
            P.transpose(psb(6)[:, c * 128:(c + 1) * 128], cnb[:, c * 128:(c + 1) * 128], ident[:])
        P.copy(cnT[:].rearrange("p c t -> p (c t)"), psb(6)[:, 0:256])
        for c in range(2):
            P.matmul(ps[1][:, 0:256], cnT[:, c, :], wkv[:, c, :], start=(c == 0), stop=(c == 1))
        if dbg == "K3":
            return
        sumsq(ps[1][:, 0:128], 128, st[:, 4:5])
        rstd_from_ss(st[:, 5:6], st[:, 4:5], 128)
        P.stt(kn[:], ps[1][:, 0:128], st[:, 5:6], kgb[:], ALU.mult, ALU.mult)
        rope(kbf[:].rearrange("p (h d) -> p h d", h=1), kn[:].rearrange("p (h d) -> p h d", h=1), 1, 128, 16, 0, sckt)
        P.copy(vvb[:], ps[1][:, 128:256], eng="act")
        P.transpose(psb(5)[:, 0:128], kbf[:], ident[:])
        P.transpose(psb(5)[:, 128:256], kibf[:], ident[:])
        P.copy(kTs[:], psb(5)[:, 0:128], eng="act")
        P.copy(kiTs[:], psb(5)[0:64, 128:256], eng="act")
        if dbg == "K4":
            return
        if dbg != "K6":
            P.dma(vv_d[kt * 128:(kt + 1) * 128, :], vvb[:])
        if dbg == "K5":
            return
        P.dma(kT_d[kt], kTs[:])
        if dbg == "K6":
            return
        P.dma(kiT_d[kt], kiTs[:])


    if dbg and dbg[0] == "K":
        for kt in range(KT0, NKT if KT1 is None else KT1):
            ktile(kt)

    if dbg and dbg[0] == "K":
        P.dma(out_d[0:128, :], xt[0][:])
        P.emit(es)
        es.close()
        return nc

    wbuf = [T("wbuf%d" % i, [128, 2048], BF16) for i in range(3)]
    wctr = [0]

    def wstream(src, a, b):
        buf = wbuf[wctr[0] % 3]
        wctr[0] += 1
        view = buf[:, 0:a * b].rearrange("p (a b) -> p a b", a=a)
        P.dma(view, src.rearrange("(c p) n -> p c n", p=128))
        return view

    ug = T("ug", [128, 512])
    vg = T("vg", [128, 512])
    gt = T("gt", [128, 1024])
    vnbf = T("vnbf", [128, 512], BF16)
    yab = T("yab", [128, 512], BF16)
    yaT = T("yaT", [128, 4, 128], BF16)
    sg = T("sg", [128, 2048])
    merged = T("merged", [128, D])
    mbf = T("mbf", [128, D], BF16)
    mT = T("mT", [128, 8, 128], BF16)
    qn = T("qn", [128, 4, 128])
    qbf = T("qbf", [128, 4, 128], BF16)
    qT = T("qT", [128, 4, 128], BF16)
    qir = T("qir", [128, 4, 64])
    qibf = T("qibf", [128, 4, 128], BF16)
    qiT = T("qiT", [128, 4, 128], BF16)
    P.memset(qibf[:], 0.0)
    kich = [T("kich%d" % i, [64, 512], BF16) for i in range(2)]
    kch = [T("kch%d" % i, [128, 512], BF16) for i in range(2)]
    vch = [T("vch%d" % i, [128, 4, 128], BF16) for i in range(2)]
    rl = [T("rl%d" % i, [128, 512]) for i in range(4)]
    maskc = T("maskc", [128, 512], BF16)
    maskT = T("maskT", [128, 4, 128], BF16)
    eT = [T("eT%d" % i, [128, 512], BF16) for i in range(2)]
    eTm = [T("eTm%d" % i, [128, 512], BF16) for i in range(2)]
    rz = T("rz", [128, 512])
    ybT = T("ybT", [128, 4, 128], BF16)
    hres = T("hres", [128, D])
    sc5 = T("sc5", [128, 240])

    def sincos5(j):
        A = junkf32[:, 0:240]
        Af = junkf32[:, 256:496]
        Ai = junkf32[:, 512:752].bitcast(I32)
        A3 = A.rearrange("p (t c) -> p t c", c=48)
        P.tt(A3[:, 0:4, 0:24], invf[:].unsqueeze(1).to_broadcast([128, 4, 24]),
             posk[:, 4 * j:4 * j + 4].unsqueeze(2).to_broadcast([128, 4, 24]), ALU.mult)
        P.ts(A3[:, 4, 0:24], invf[:], poso[:, j:j + 1], None, ALU.mult)
        P.ts(A3[:, :, 24:48], A3[:, :, 0:24], 0.5 * PI, None, ALU.add)
        P.ts(Af, A, 1.0 / (2 * PI), None, ALU.mult)
        P.copy(Ai, Af)
        P.copy(Af, Ai)
        P.stt(A, Af, -2 * PI, A, ALU.mult, ALU.add)
        P.ts(Af, A, PI, None, ALU.is_gt)
        P.stt(A, Af, -2 * PI, A, ALU.mult, ALU.add)
        P.ts(Af, A, -PI, None, ALU.is_lt)
        P.stt(A, Af, 2 * PI, A, ALU.mult, ALU.add)
        P.act(sc5[:], A, AF.Sin)

    hsb = T("hsb", [128, D], BF16)
    hsT = T("hsT", [128, 8, 128], BF16)
    qpT = T("qpT", [128, 16, 128], BF16)
    vals = T("vals", [128, 16, 16])
    idxu = T("idxu", [128, 16, 16], U32)
    idxf = T("idxf", [128, 16, 16])
    best = T("best", [128, 8, 16])
    posu = T("posu", [128, 8, 16], U32)
    pa_i = T("pa_i", [128, 8, 16], U32)
    pb_i = T("pb_i", [128, 8, 16], U32)
    pa_f = T("pa_f", [128, 8, 16])
    pb_f = T("pb_f", [128, 8, 16])
    sel1 = T("sel1", [128, 8, 16])
    sel2 = T("sel2", [128, 8, 16])
    eidf = T("eidf", [128, 128])
    eid = T("eid", [128, 128], I32)
    gate = T("gate", [128, 8, 16])
    adot = T("adot", [128, 128])
    cw = T("cw", [128, 128])
    gtmp = T("gtmp", [128, 128])

    def gelu(dst, src, tmp, n):
        P.tt(tmp, src, src, ALU.mult)
        P.ts(tmp, tmp, 0.044715, 1.0, ALU.mult, ALU.add)
        P.tt(tmp, tmp, src, ALU.mult)
        P.act(tmp, tmp, AF.Sigmoid, scale=1.5957691216057308)
        P.tt(dst, src, tmp, ALU.mult)

    def top16(vout, iout, src, tmp):
        va, vk = _k(vout)
        ia, ik = _k(iout)
        rk, wk_ = P._rw([vout], [src])
        P.op("dve", lambda e, o=va[:, 0:8], i=_k(src)[0]: e.max(o, i), rk, wk_)
        rk, wk_ = P._rw([iout], [vout, src])
        P.op("dve", lambda e, o=ia[:, 0:8], m=va[:, 0:8], i=_k(src)[0]: e.max_index(o, m, i), rk, wk_)
        rk, wk_ = P._rw([tmp], [vout, src])
        P.op("dve", lambda e, o=_k(tmp)[0], m=va[:, 0:8], i=_k(src)[0]: e.match_replace(o, m, i, -1e30), rk, wk_)
        rk, wk_ = P._rw([vout], [tmp])
        P.op("dve", lambda e, o=va[:, 8:16], i=_k(tmp)[0]: e.max(o, i), rk, wk_)
        rk, wk_ = P._rw([iout], [vout, tmp])
        P.op("dve", lambda e, o=ia[:, 8:16], m=va[:, 8:16], i=_k(tmp)[0]: e.max_index(o, m, i), rk, wk_)

    proj = av(0, INW)
    pj = proj[0]
    pjk = proj[1]

    for j in range(NOWN):
        nch = (4 * j + 4) * 128 // 512
        sincos5(j)
        SC5[0] = sc5
        for kt in range(4 * j, 4 * j + 4):
            ktile(kt)
        xres = xt[j % 2]
        P.dma(xres[:], x_own[j * 128:(j + 1) * 128, :])
        front(xres[:], 7)
        ncc = (INW + 255) // 256
        for cc in range(ncc):
            c0 = cc * 256
            w = min(256, INW - c0)
            wbf = wstream(winbf[:, c0:c0 + w], 8, w)
            bank = cc % 2
            for c in range(8):
                P.matmul(ps[bank][:, 0:w], xsT[:, c, :], wbf[:, c, :], start=(c == 0), stop=(c == 7))
            P.copy((pj[:, c0:c0 + w], pjk), ps[bank][:, 0:w], eng=("act" if cc % 2 else "dve"))
        gelu(ug[:], (pj[:, 0:512], pjk), gt[:, 0:512], 512)
        gelu(vg[:], (pj[:, 512:1024], pjk), gt[:, 512:1024], 512)
        P.reduce(st[:, 8:9], vg[:], ALU.add)
        P.dot(junkf32[:, 0:512], vg[:], vg[:], st[:, 9:10])
        P.ts(st[:, 10:11], st[:, 8:9], 1.0 / 512, None, ALU.mult)
        P.tt(st[:, 11:12], st[:, 10:11], st[:, 10:11], ALU.mult)
        P.stt(st[:, 12:13], st[:, 9:10], 1.0 / 512, st[:, 11:12], ALU.mult, ALU.subtract)
        rstd_from_ss(st[:, 12:13], st[:, 12:13], 1)
        P.ts(vg[:], vg[:], st[:, 10:11], st[:, 12:13], ALU.subtract, ALU.mult)
        P.tt(vg[:], vg[:], vng[:], ALU.mult)
        P.tt(vnbf[:], vg[:], vnb[:], ALU.add)
        for g in range(4):
            P.matmul(ps[2][:, g * 128:(g + 1) * 128], wsT[:, g, :], vnbf[:, g * 128:(g + 1) * 128])
        for g in range(4):
            P.stt(yab[:, g * 128:(g + 1) * 128], ps[2][:, g * 128:(g + 1) * 128], spb[:, g:g + 1],
                  ug[:, g * 128:(g + 1) * 128], ALU.add, ALU.mult)
        for c in range(4):
            P.transpose(psb(3)[:, c * 128:(c + 1) * 128], yab[:, c * 128:(c + 1) * 128], ident[:])
        P.copy(yaT[:].rearrange("p c t -> p (c t)"), psb(3)[:, 0:512])
        for hf in range(2):
            wv = wstream(wabf[:, hf * 512:(hf + 1) * 512], 4, 512)
            for c in range(4):
                P.matmul(ps[4 + hf][:], yaT[:, c, :], wv[:, c, :], start=(c == 0), stop=(c == 3))
        P.act(sg[:], (pj[:, 2116:4164], pjk), AF.Sigmoid)
        for hf in range(2):
            P.tt(merged[:, hf * 512:(hf + 1) * 512], sg[:, hf * 512:(hf + 1) * 512], ps[4 + hf][:], ALU.mult)
        P.tt(junkf32[:, 0:512], (pj[:, 1024:1536], pjk), (pj[:, 1024:1536], pjk), ALU.mult)
        P.reduce(st[:, 16:20], junkf32[:, 0:512].rearrange("p (h d) -> p h d", h=4), ALU.add)
        rstd_from_ss(st[:, 20:24], st[:, 16:20], 128)
        for h in range(4):
            P.stt(qn[:, h, :], (pj[:, 1024 + h * 128:1024 + (h + 1) * 128], pjk), st[:, 20 + h:21 + h], qgb[:], ALU.mult, ALU.mult)
        rope(qbf[:], qn[:], 4, 128, 16, 0, sc5[:, 192:240])
        for h in range(4):
            P.transpose(psb(6)[:, h * 128:(h + 1) * 128], qbf[:, h, :], ident[:])
        P.copy(qT[:].rearrange("p h t -> p (h t)"), psb(6)[:, 0:512])
        wid = (pj[:, 2112:2116], pjk)
        P.ts(st[:, 28:32], wid, 0.0, 2.0, ALU.is_gt, ALU.mult)
        P.ts(st[:, 28:32], st[:, 28:32], -1.0, None, ALU.add)
        P.stt(st[:, 24:28], wid, 0.0625, st[:, 28:32], ALU.mult, ALU.mult)
        rope(qir[:], (pj[:, 1792:2048].rearrange("p (h d) -> p h d", h=4), pjk), 4, 64, 8, 16, sc5[:, 192:240])
        P.tt(qibf[:, :, 0:64], qir[:], st[:, 24:28].unsqueeze(2).to_broadcast([128, 4, 64]), ALU.mult)
        for h in range(4):
            P.transpose(psb(7)[:, h * 128:(h + 1) * 128], qibf[:, h, :], ident[:])
        P.copy(qiT[:].rearrange("p h t -> p (h t)"), psb(7)[:, 0:512])
        for c in range(nch):
            kb = kich[c % 2]
            P.dma(kb[:].rearrange("p (k t) -> p k t", k=4), kiT_d[c * 4:(c + 1) * 4].rearrange("k p t -> p k t"))
            scv = av(c * 512, (c + 1) * 512)
            for h in range(4):
                P.matmul(ps[h][:], qiT[0:64, h, :], kb[:])
            for h in range(4):
                P.act(rl[h][:], ps[h][:], AF.Relu)
            P.ts(scv, rl[0][:], st[:, 28:29], None, ALU.mult)
            for h in range(1, 4):
                P.stt(scv, rl[h][:], st[:, 28 + h:29 + h], scv, ALU.mult, ALU.add)
            if c == nch - 1:
                P.tt(scv, scv, maskb[:], ALU.add)
        n = nch * 512
        lo, hi, mid, cnt, ge, dd = (st[:, 32:33], st[:, 33:34], st[:, 34:35], st[:, 35:36], st[:, 36:37], st[:, 37:38])
        cnts = st[:, 40:48]
        P.memset(mid, 0.0)
        pieces = [(p0, min(p0 + 2048, n)) for p0 in range(0, n, 2048)]
        hstep = 32.0
        ktop = min(TOPK, S // 4) - 0.5
        for it in range(BIS_ITERS):
            for pi_, (p0, p1) in enumerate(pieces):
                P.ts(junkb[:, 0:p1 - p0], av(p0, p1), mid, None, ALU.is_ge, ALU.add, accum=cnts[:, pi_:pi_ + 1])
            if len(pieces) > 1:
                P.reduce(cnt, cnts[:, 0:len(pieces)], ALU.add)
                cc_ = cnt
            else:
                cc_ = cnts[:, 0:1]
            P.ts(dd, cc_, ktop, hstep, ALU.is_ge, ALU.mult)
            P.stt(mid, dd, -0.5 * hstep, mid, ALU.add, ALU.add)
            hstep *= 0.5
        P.ts(lo, mid, -hstep, None, ALU.add)
        nkb = nch * 4
        for c in range(nch):
            kc = kch[c % 2]
            vc = vch[c % 2]
            P.dma(kc[:].rearrange("p (k t) -> p k t", k=4), kT_d[c * 4:(c + 1) * 4].rearrange("k p t -> p k t"))
            P.dma(vc[:], vv_d[c * 512:(c + 1) * 512, :].rearrange("(kb p) d -> p kb d", p=128))
            P.ts(maskc[:], av(c * 512, (c + 1) * 512), lo, None, ALU.is_ge)
            for b in range(4):
                P.transpose(psb(0)[:, b * 128:(b + 1) * 128], maskc[:, b * 128:(b + 1) * 128], ident[:])
            P.copy(maskT[:].rearrange("p b q -> p (b q)"), psb(0)[:, 0:512])
            for b in range(4):
                gkb = c * 4 + b
                bank = 1 + (gkb % 2)
                P.matmul(ps[bank][:], kc[:, b * 128:(b + 1) * 128], qT[:].rearrange("p h t -> p (h t)"))
                e1 = eT[gkb % 2]
                e2 = eTm[gkb % 2]
                P.act(e1[:], ps[bank][:], AF.Exp, scale=float(128 ** -0.5))
                P.tt(e2[:].rearrange("p (h q) -> p h q", h=4), e1[:].rearrange("p (h q) -> p h q", h=4),
                     maskT[:, b, :].unsqueeze(1).to_broadcast([128, 4, 128]), ALU.mult)
                P.matmul(ps[4][:], vc[:, b, :], e2[:], start=(gkb == 0), stop=(gkb == nkb - 1))
                P.matmul(ps[5][:], ones[:], e2[:], start=(gkb == 0), stop=(gkb == nkb - 1))
        if dbg == "H" and j == NOWN - 1:
            P.dma(d_mT.ap(), maskT[:].rearrange("p b q -> p (b q)"))
            P.dma(d_e1.ap(), eT[(nkb - 1) % 2][:])
            P.dma(d_e2.ap(), eTm[(nkb - 1) % 2][:])
            P.copy(gt[:, 0:512], ps[4][:])
            P.dma(d_num.ap(), gt[:, 0:512])
        P.op("dve", lambda e: e.reciprocal(rz[:], ps[5][:]), ["ps5"], ["rz"])
        if dbg == "H" and j == NOWN - 1:
            P.dma(d_rz.ap(), rz[:])
        P.copy(gt[:, 512:1024], ps[4][:], eng="act")
        P.tt(ybT[:].rearrange("p h t -> p (h t)"), gt[:, 512:1024], rz[:], ALU.mult)
        for hf in range(2):
            wv = wstream(wbbf[:, hf * 512:(hf + 1) * 512], 4, 512)
            for h in range(4):
                P.matmul(ps[6 + hf][:], ybT[:, h, :], wv[:, h, :], start=(h == 0), stop=(h == 3))
        for hf in range(2):
            P.tt(gt[:, hf * 512:(hf + 1) * 512], sg[:, 1024 + hf * 512:1024 + (hf + 1) * 512], ps[6 + hf][:], ALU.mult)
        if dbg == "H" and j == NOWN - 1:
            P.dma(d_yb.ap(), ybT[:].rearrange("p h t -> p (h t)"))
            P.dma(d_gb.ap(), gt[:])
        P.tt(mbf[:], merged[:], gt[:], ALU.add)
        for c in range(8):
            P.transpose(psb(0)[:, c * 128:(c + 1) * 128], mbf[:, c * 128:(c + 1) * 128], ident[:])
        P.copy(mT[:].rearrange("p c t -> p (c t)"), psb(0))
        for qd in range(4):
            wv = wstream(wobf[:, qd * 256:(qd + 1) * 256], 8, 256)
            for c in range(8):
                P.matmul(ps[1 + qd // 2][:, (qd % 2) * 256:(qd % 2 + 1) * 256], mT[:, c, :], wv[:, c, :],
                         start=(c == 0), stop=(c == 7))
        for hf in range(2):
            P.tt(hres[:, hf * 512:(hf + 1) * 512], xres[:, hf * 512:(hf + 1) * 512], ps[1 + hf][:], ALU.add)
        if dbg == "H":
            P.dma(out_d[j * 128:(j + 1) * 128, :], hres[:])
            continue
        hn = av(0, 1024)
        sub = av(1024, 3072)
        sub2 = av(3072, 5120)
        cand = av(5120, 7168)
        oh = av(7168, 9216)
        junkf = av(9216, 10240)
        gbuf = [av(10240 + r * 1024, 10240 + (r + 1) * 1024) for r in range(6)]
        sumsq(hres[:], D, st[:, 48:49])
        rstd_from_ss(st[:, 49:50], st[:, 48:49], D)
        P.act(hsb[:], hres[:], AF.Copy, scale=st[:, 49:50])
        P.stt(hn, hres[:], st[:, 49:50], g2b[:], ALU.mult, ALU.mult)
        for c in range(8):
            P.transpose(psb(2)[:, c * 128:(c + 1) * 128], hsb[:, c * 128:(c + 1) * 128], ident[:])
        P.copy(hsT[:].rearrange("p c t -> p (c t)"), psb(2))
        for gp in range(8):
            wv = wstream(wqbf[:, gp * 256:(gp + 1) * 256], 8, 256)
            for g2_ in range(2):
                g = gp * 2 + g2_
                bank = 3 + g // 4
                for c in range(8):
                    P.matmul(ps[bank][:, (g % 4) * 128:(g % 4 + 1) * 128], wv[:, c, g2_ * 128:(g2_ + 1) * 128], hsT[:, c, :],
                             start=(c == 0), stop=(c == 7))
        for b in range(4):
            P.copy(qpT[:, b * 4:(b + 1) * 4, :].rearrange("p g t -> p (g t)"), ps[3 + b][:], eng=("act" if b % 2 else "dve"))
        sbanks = [7, 0, 1, 2]
        for g in range(16):
            P.matmul(ps[sbanks[g // 4]][:, (g % 4) * 128:(g % 4 + 1) * 128], qpT[:, g, :], skT[:, g, :])
        for b in range(4):
            P.copy((sub[0][:, b * 512:(b + 1) * 512], sub[1]), ps[sbanks[b]][:], eng=("act" if b % 2 else "dve"))
        for g in range(16):
            top16(vals[:, g, :], idxu[:, g, :], (sub[0][:, g * 128:(g + 1) * 128], sub[1]),
                  (sub2[0][:, g * 128:(g + 1) * 128], sub2[1]))
        v4 = vals[:].rearrange("p (h t) k -> p h t k", t=2)
        c4 = (cand[0].rearrange("p (h a b) -> p h a b", h=8, a=16), cand[1])
        P.tt(c4, v4[:, :, 0, :].unsqueeze(3).to_broadcast([128, 8, 16, 16]),
             v4[:, :, 1, :].unsqueeze(2).to_broadcast([128, 8, 16, 16]), ALU.add)
        for h in range(8):
            top16(best[:, h, :], posu[:, h, :], (cand[0][:, h * 256:(h + 1) * 256], cand[1]),
                  (sub2[0][:, h * 256:(h + 1) * 256], sub2[1]))
        P.op("dve", lambda e: e.tensor_single_scalar(pa_i[:], posu[:], 4, ALU.logical_shift_right), ["posu"], ["pa_i"])
        P.op("dve", lambda e: e.tensor_single_scalar(pb_i[:], posu[:], 15, ALU.bitwise_and), ["posu"], ["pb_i"])
        P.copy(pa_f[:], pa_i[:])
        P.copy(pb_f[:], pb_i[:])
        P.copy(idxf[:], idxu[:])
        i4 = idxf[:].rearrange("p (h t) k -> p h t k", t=2)
        o4 = (oh[0].rearrange("p (h k a) -> p h k a", h=8, k=16), oh[1])
        iob = iota16[:].unsqueeze(1).unsqueeze(1).to_broadcast([128, 8, 16, 16])
        for (pf, t_, so) in ((pa_f, 0, sel1), (pb_f, 1, sel2)):
            P.tt(o4, iob, pf[:].unsqueeze(3).to_broadcast([128, 8, 16, 16]), ALU.is_equal)
            P.tt(o4, o4, i4[:, :, t_, :].unsqueeze(2).to_broadcast([128, 8, 16, 16]), ALU.mult)
            P.reduce(so[:], o4, ALU.add)
        P.stt(eidf[:].rearrange("p (h k) -> p h k", h=8), sel1[:], 128.0, sel2[:], ALU.mult, ALU.add)
        P.copy(eid[:], eidf[:])
        P.reduce(st[:, 50:58], best[:], ALU.max)
        P.tt(gate[:], best[:], st[:, 50:58].unsqueeze(2).to_broadcast([128, 8, 16]), ALU.subtract)
        P.act(gate[:], gate[:], AF.Exp)
        P.reduce(rz[:, 0:8], gate[:], ALU.add)
        P.op("dve", lambda e: e.reciprocal(rz[:, 8:16], rz[:, 0:8]), ["rz"], ["rz"])
        P.tt(gate[:], gate[:], rz[:, 8:16].unsqueeze(2).to_broadcast([128, 8, 16]), ALU.mult)
        for s_ in range(128):
            gb = gbuf[s_ % 6]
            P.gather(gb, u_tab.ap(), eid[:, s_:s_ + 1])
            P.dot(junkf, gb, hn, adot[:, s_:s_ + 1])
        gelu(cw[:], adot[:], gtmp[:], 128)
        P.tt(cw[:], cw[:], gate[:].rearrange("p h k -> p (h k)"), ALU.mult)
        for s_ in range(128):
            gb = gbuf[s_ % 6]
            P.gather(gb, v_tab.ap(), eid[:, s_:s_ + 1])
            P.stt(hres[:], gb, cw[:, s_:s_ + 1], hres[:], ALU.mult, ALU.add)
        P.dma(out_d[j * 128:(j + 1) * 128, :], hres[:])

    P.emit(es)
    es.close()
    return nc


def _host_inputs(inp, S, NOWN):
    f32 = np.float32
    B = inp["x"].shape[0]
    x = np.asarray(inp["x"], f32)
    pos = np.asarray(inp["positions"], np.int32)
    NKT = S // 128
    rep = lambda v, n=128: np.ascontiguousarray(np.broadcast_to(np.asarray(v, f32).reshape(1, -1), (n, np.asarray(v).size)))
    pc = lambda v, c: np.ascontiguousarray(np.asarray(v, f32).reshape(c, 128).T)
    half_q = np.arange(16, dtype=f32)
    half_i = np.arange(8, dtype=f32)
    inv_q = np.power(f32(500000.0), -half_q * f32(2.0) / f32(32)).astype(f32)
    inv_i = np.power(f32(500000.0), -half_i * f32(2.0) / f32(16)).astype(f32)
    invf = rep(np.concatenate([inv_q, inv_i]))
    common = {
        "w_in": np.ascontiguousarray(inp["w_in"][0], f32),
        "g1": pc(inp["norm1_g"][0], 8),
        "g2": pc(inp["norm2_g"][0], 8),
        "g2b": rep(inp["norm2_g"][0]),
        "kvg": pc(inp["kv_norm_g"][0], 2),
        "vng": rep(inp["v_norm_g"][0]),
        "vnb": rep(inp["v_norm_b"][0]),
        "spw": np.ascontiguousarray(inp["spatial_w"][0], f32),
        "spb": np.ascontiguousarray(np.asarray(inp["spatial_b"][0], f32).T),
        "wuk": np.ascontiguousarray(inp["w_uk"][0], f32),
        "wuv": np.ascontiguousarray(inp["w_uv"][0], f32),
        "qgb": rep(inp["q_norm_g"][0]),
        "kgb": rep(inp["k_norm_g"][0]),
        "wa": np.ascontiguousarray(inp["w_a_out"][0], f32),
        "wb": np.ascontiguousarray(inp["w_b_out"][0], f32),
        "wo": np.ascontiguousarray(inp["w_o"][0], f32),
        "wq": np.ascontiguousarray(inp["peer_wq"][0], f32),
        "subk": np.ascontiguousarray(np.asarray(inp["peer_subkeys"][0], f32).reshape(16, 128, 128)),
        "peer_u": np.ascontiguousarray(inp["peer_u"][0], f32),
        "peer_v": np.ascontiguousarray(inp["peer_v"][0], f32),
        "ident": np.eye(128, dtype=f32).astype(ml_dtypes.bfloat16),
        "tril": np.tril(np.ones((128, 128), f32)),
        "invf": invf,
        "iota16": rep(np.arange(16, dtype=f32)),
    }
    maps = []
    lanes = 8 // B
    for core in range(8):
        b, c = core // lanes, core % lanes
        tiles = [lanes * j + c for j in range(NOWN)]
        rows = np.concatenate([np.arange(t * 128, (t + 1) * 128) for t in tiles])
        mb = np.zeros((128, 512), f32)
        for r in range(4):
            blk = mb[:, r * 128:(r + 1) * 128]
            if r > c:
                blk[:] = -1e30
            elif r == c:
                blk[:] = np.where(np.arange(128)[None, :] <= np.arange(128)[:, None], 0.0, -1e30)
        m = dict(common)
        for i in range(4):
            m["x_seq%d" % i] = np.ascontiguousarray(x[b][i * (S // 4):(i + 1) * (S // 4)])
        m["x_own"] = np.ascontiguousarray(x[b][rows])
        m["pos_seq"] = np.ascontiguousarray(pos[b].reshape(NKT, 128).T)
        m["pos_own"] = np.ascontiguousarray(pos[b][rows].reshape(NOWN, 128).T)
        m["maskb"] = mb
        maps.append((m, b, rows))
    return maps


_NC_CACHE = {}


def kernel(**inputs):
    x = np.asarray(inputs["x"])
    B, S, _ = x.shape
    lanes = 8 // B
    NOWN = S // 128 // lanes
    key = (S, NOWN)
    if key not in _NC_CACHE:
        _NC_CACHE[key] = build_program(S, NOWN)
    nc = _NC_CACHE[key]
    maps = _host_inputs(inputs, S, NOWN)
    res = run_bass_kernel_spmd(nc, [m for m, _, _ in maps], core_ids=list(range(8)))
    out = np.empty((B, S, D), np.float32)
    for (m, b, rows), r in zip(maps, res.results):
        out[b, rows] = r["out"]
    return out
```

```python
import numpy as np
import ml_dtypes
from contextlib import ExitStack
import concourse.bass as bass
import concourse.mybir as mybir
from concourse.bass_utils import run_bass_kernel_spmd

F32 = mybir.dt.float32
BF16 = mybir.dt.bfloat16
I32 = mybir.dt.int32
U32 = mybir.dt.uint32
ALU = mybir.AluOpType
AF = mybir.ActivationFunctionType
AX = mybir.AxisListType

D = 1024
INW = 4164
EPS = 1e-6
TOPK = 256
NEXP = 16384
PI = float(np.pi)
ENGS = ("pe", "act", "dve", "pool", "sp")
KDMA = 8
BIS_ITERS = 24
KT0 = 0
NDUMMY = 0
KT1 = None


def _k(x):
    if isinstance(x, tuple):
        return x
    return (x, (x.name,))


class Prog:
    def __init__(self, nc):
        self.nc = nc
        self.ops = {e: [] for e in ENGS}
        self.cnt = {e: 0 for e in ENGS}
        self.dcnt = {e: 0 for e in ENGS}
        self.waited = {e: {} for e in ENGS}
        self.res_w = {}
        self.res_r = {}

    def op(self, eng, fn, reads=(), writes=(), dma=False):
        waits = {}

        def addw(t):
            sk, v = t
            if waits.get(sk, 0) < v:
                waits[sk] = v

        for r in reads:
            if r in self.res_w:
                addw(self.res_w[r])
        for w in writes:
            if w in self.res_w:
                addw(self.res_w[w])
            for sk, v in self.res_r.get(w, {}).items():
                addw((sk, v))
        if dma:
            k = self.dcnt[eng]
            self.dcnt[eng] += 1
            slot = k % KDMA
            semkey = ("dma", eng, slot)
            val = 16 * (k // KDMA + 1)
            inc = 16
            if k >= KDMA:
                addw((semkey, val - 16))
        else:
            self.cnt[eng] += 1
            semkey = eng
            val = self.cnt[eng]
            inc = 1
        wl = []
        for sk, v in waits.items():
            if eng == "pe" and sk == "pe":
                continue
            if self.waited[eng].get(sk, 0) >= v:
                continue
            self.waited[eng][sk] = v
            wl.append((sk, v))
        self.ops[eng].append((wl, fn, semkey, inc))
        tok = (semkey, val)
        for r in reads:
            d = self.res_r.setdefault(r, {})
            if d.get(semkey, 0) < val:
                d[semkey] = val
        for w in writes:
            self.res_w[w] = tok
            self.res_r[w] = {}
        return tok

    def _rw(self, outs, ins):
        wk = []
        rk = []
        for o in outs:
            wk += list(_k(o)[1])
        for i in ins:
            if i is None or isinstance(i, (int, float)):
                continue
            rk += list(_k(i)[1])
        return rk, wk

    @staticmethod
    def _a(x):
        if x is None or isinstance(x, (int, float)):
            return x
        return _k(x)[0]

    def dot(self, junk, a, b, acc):
        rk, wk = self._rw([junk, acc], [a, b])
        j, x, y, c = self._a(junk), self._a(a), self._a(b), self._a(acc)
        self.op("dve", lambda e: e.scalar_tensor_tensor(j, x, 1.0, y, ALU.mult, ALU.mult, accum_out=c), rk, wk)

    def matmul(self, out, lhsT, rhs, start=True, stop=True):
        rk, wk = self._rw([out], [lhsT, rhs])
        o, l, r = self._a(out), self._a(lhsT), self._a(rhs)
        self.op("pe", lambda e: e.matmul(o, l, r, start=start, stop=stop), rk, wk)

    def transpose(self, out, in_, ident):
        rk, wk = self._rw([out], [in_])
        o, i, d = self._a(out), self._a(in_), self._a(ident)
        self.op("pe", lambda e: e.transpose(o, i, d), rk, wk)

    def act(self, out, in_, func, bias=None, scale=None, accum=None, eng="act"):
        outs = [out] + ([accum] if accum is not None else [])
        rk, wk = self._rw(outs, [in_, bias, scale])
        o, i, b, s, a = self._a(out), self._a(in_), self._a(bias), self._a(scale), self._a(accum)
        kw = {}
        if b is not None:
            kw["bias"] = b
        if s is not None:
            kw["scale"] = s
        if a is not None:
            kw["accum_out"] = a
        self.op("act", lambda e: e.activation(o, i, func, **kw), rk, wk)

    def ts(self, out, in0, s1, s2, op0, op1=None, accum=None, eng="dve"):
        outs = [out] + ([accum] if accum is not None else [])
        rk, wk = self._rw(outs, [in0, s1, s2])
        o, i, a1, a2, ac = self._a(out), self._a(in0), self._a(s1), self._a(s2), self._a(accum)
        kw = {}
        if op1 is not None:
            kw["op1"] = op1
        if ac is not None:
            kw["accum_out"] = ac
        self.op(eng, lambda e: e.tensor_scalar(o, i, a1, a2, op0, **kw), rk, wk)

    def tt(self, out, in0, in1, op, eng="dve"):
        rk, wk = self._rw([out], [in0, in1])
        o, i0, i1 = self._a(out), self._a(in0), self._a(in1)
        self.op(eng, lambda e: e.tensor_tensor(o, i0, i1, op), rk, wk)

    def stt(self, out, in0, scalar, in1, op0, op1, eng="dve"):
        rk, wk = self._rw([out], [in0, scalar, in1])
        o, i0, s, i1 = self._a(out), self._a(in0), self._a(scalar), self._a(in1)
        self.op(eng, lambda e: e.scalar_tensor_tensor(o, i0, s, i1, op0, op1), rk, wk)

    def copy(self, out, in_, eng="dve"):
        rk, wk = self._rw([out], [in_])
        o, i = self._a(out), self._a(in_)
        if eng == "act":
            self.op("act", lambda e: e.activation(o, i, AF.Copy), rk, wk)
        else:
            self.op(eng, lambda e: e.tensor_copy(o, i), rk, wk)

    def reduce(self, out, in_, op, eng="dve"):
        rk, wk = self._rw([out], [in_])
        o, i = self._a(out), self._a(in_)
        self.op(eng, lambda e: e.tensor_reduce(o, i, AX.X, op), rk, wk)

    def memset(self, out, val, eng="dve"):
        rk, wk = self._rw([out], [])
        o = self._a(out)
        self.op(eng, lambda e: e.memset(o, val), rk, wk)

    def dma(self, out, in_, eng="sp"):
        rk, wk = self._rw([out], [in_])
        o, i = self._a(out), self._a(in_)
        self.op(eng, lambda e: e.dma_start(out=o, in_=i), rk, wk, dma=True)

    def gather(self, out, table, idx):
        rk, wk = self._rw([out], [idx])
        o, t, i = self._a(out), self._a(table), self._a(idx)
        self.op(
            "pool",
            lambda e: e.indirect_dma_start(
                out=o, out_offset=None, in_=t, in_offset=bass.IndirectOffsetOnAxis(ap=i, axis=0)
            ),
            rk,
            wk,
            dma=True,
        )

    def emit(self, es):
        nc = self.nc
        sems = {}
        for e in ENGS:
            sems[e] = es.enter_context(nc.semaphore("s_" + e))
            for s in range(KDMA):
                sems[("dma", e, s)] = es.enter_context(nc.semaphore("d_%s_%d" % (e, s)))
        block = es.enter_context(nc.Block())

        def run(engobj, name):
            for wl, fn, semkey, inc in self.ops[name]:
                for sk, v in wl:
                    engobj.wait_ge(sems[sk], v)
                fn(engobj).then_inc(sems[semkey], inc)
            k = self.dcnt[name]
            for s in range(min(k, KDMA)):
                n = (k - 1 - s) // KDMA + 1
                engobj.wait_ge(sems[("dma", name, s)], 16 * n)

        @block.tensor
        def _(e):
            run(e, "pe")

        @block.scalar
        def _(e):
            run(e, "act")

        @block.vector
        def _(e):
            run(e, "dve")

        @block.gpsimd
        def _(e):
            run(e, "pool")

        @block.sync
        def _(e):
            run(e, "sp")


def build_program(S, NOWN, dbg=None):
    NKT = S // 128
    nc = bass.Bass("TRN2", target_bir_lowering=False)
    es = ExitStack()
    P = Prog(nc)

    def din(name, shape, dt=F32):
        return nc.dram_tensor(name, list(shape), dt, kind="ExternalInput")

    NXP = 4
    TPP = NKT // NXP
    x_seq = [din("x_seq%d" % i, [S // NXP, D]) for i in range(NXP)]
    x_own = din("x_own", [NOWN * 128, D])
    pos_seq = din("pos_seq", [128, NKT], I32)
    pos_own = din("pos_own", [128, NOWN], I32)
    w_in = din("w_in", [D, INW])
    g1_d = din("g1", [128, 8])
    g2_d = din("g2", [128, 8])
    g2b_d = din("g2b", [128, D])
    kvg_d = din("kvg", [128, 2])
    vng_d = din("vng", [128, 512])
    vnb_d = din("vnb", [128, 512])
    spw_d = din("spw", [4, 128, 128])
    spb_d = din("spb", [128, 4])
    wuk_d = din("wuk", [256, 128])
    wuv_d = din("wuv", [256, 128])
    qg_d = din("qgb", [128, 128])
    kg_d = din("kgb", [128, 128])
    wa_d = din("wa", [512, D])
    wb_d = din("wb", [512, D])
    wo_d = din("wo", [D, D])
    wq_d = din("wq", [D, 2048])
    sk_d = din("subk", [16, 128, 128])
    if not dbg:
        u_tab = din("peer_u", [NEXP, D])
        v_tab = din("peer_v", [NEXP, D])
    ident_d = din("ident", [128, 128], BF16)
    tril_d = din("tril", [128, 128])
    maskb_d = din("maskb", [128, 512])
    inv_d = din("invf", [128, 24])
    iota_d = din("iota16", [128, 16])
    out_d = nc.dram_tensor("out", [NOWN * 128, D], F32, kind="ExternalOutput")
    okind = "ExternalOutput" if dbg else "Internal"
    winbf = nc.dram_tensor("winbf", [D, INW], BF16, kind="Internal")
    wabf = nc.dram_tensor("wabf", [512, D], BF16, kind="Internal")
    wbbf = nc.dram_tensor("wbbf", [512, D], BF16, kind="Internal")
    wobf = nc.dram_tensor("wobf", [D, D], BF16, kind="Internal")
    wqbf = nc.dram_tensor("wqbf", [D, 2048], BF16, kind="Internal")
    kiT_d = nc.dram_tensor("kiT_d", [NKT, 64, 128], BF16, kind=okind)
    kT_d = nc.dram_tensor("kT_d", [NKT, 128, 128], BF16, kind=okind)
    vv_d = nc.dram_tensor("vv_d", [S, 128], BF16, kind=okind)

    if dbg == "H":
        d_sc = nc.dram_tensor("d_sc", [128, 512], F32, kind="ExternalOutput")
        d_st = nc.dram_tensor("d_st", [128, 64], F32, kind="ExternalOutput")
        d_yb = nc.dram_tensor("d_yb", [128, 512], BF16, kind="ExternalOutput")
        d_ma = nc.dram_tensor("d_ma", [128, D], F32, kind="ExternalOutput")
        d_gb = nc.dram_tensor("d_gb", [128, D], F32, kind="ExternalOutput")
        d_q = nc.dram_tensor("d_q", [128, 512], BF16, kind="ExternalOutput")
        d_qi = nc.dram_tensor("d_qi", [128, 512], BF16, kind="ExternalOutput")
        d_mT = nc.dram_tensor("d_mT", [128, 512], BF16, kind="ExternalOutput")
        d_e1 = nc.dram_tensor("d_e1", [128, 512], BF16, kind="ExternalOutput")
        d_e2 = nc.dram_tensor("d_e2", [128, 512], BF16, kind="ExternalOutput")
        d_rz = nc.dram_tensor("d_rz", [128, 512], F32, kind="ExternalOutput")
        d_num = nc.dram_tensor("d_num", [128, 512], F32, kind="ExternalOutput")

    def T(name, shape, dt=F32):
        return es.enter_context(nc.sbuf_tensor(name, list(shape), dt))

    AW = 16384
    arena = T("arena", [128, AW])

    def av(lo, hi, dt=F32):
        keys = tuple(("sc", c) for c in range(lo // 512, (hi - 1) // 512 + 1))
        ap = arena[:, lo:hi]
        if dt is not F32:
            ap = ap.bitcast(dt)
        return (ap, keys)

    ident = T("identb", [128, 128], BF16)
    ones = T("onesb", [128, 128], BF16)
    negpi = T("negpi", [128, 1])
    epsb = T("epsb", [128, 1])
    g1 = T("g1s", [128, 8])
    g2 = T("g2s", [128, 8])
    g2b = T("g2bs", [128, D])
    kvg = T("kvgs", [128, 2])
    vng = T("vngs", [128, 512])
    vnb = T("vnbs", [128, 512])
    spb = T("spbs", [128, 4])
    qgb = T("qgbs", [128, 128])
    kgb = T("kgbs", [128, 128])
    trilm = T("trilm", [128, 128])
    maskb = T("maskbs", [128, 512])
    invf = T("invfs", [128, 24])
    iota16 = T("iota16s", [128, 16])
    posk_i = T("posk_i", [128, NKT], I32)
    posk = T("posk", [128, NKT])
    poso_i = T("poso_i", [128, NOWN], I32)
    poso = T("poso", [128, NOWN])
    wk = T("wk", [128, 8, 320], BF16)
    wkv = T("wkv", [128, 2, 256], BF16)
    skT = T("skT", [128, 16, 128], BF16)
    wsT = T("wsT", [128, 4, 128], BF16)

    ps = [es.enter_context(nc.psum_tensor("ps%d" % i, [128, 512], F32)) for i in range(8)]

    def psb(i):
        return ps[i][:].bitcast(BF16)

    for dst, src in ((ident, ident_d), (g1, g1_d), (g2, g2_d), (g2b, g2b_d), (kvg, kvg_d), (vng, vng_d),
                     (vnb, vnb_d), (spb, spb_d), (qgb, qg_d), (kgb, kg_d), (trilm, tril_d), (maskb, maskb_d),
                     (invf, inv_d), (iota16, iota_d), (posk_i, pos_seq), (poso_i, pos_own)):
        P.dma(dst[:], src.ap())
    P.memset(ones[:], 1.0)
    P.memset(negpi[:], -PI)
    P.memset(epsb[:], EPS)
    P.copy(posk[:], posk_i[:])
    P.copy(poso[:], poso_i[:])

    if dbg == "C0":
        P.dma(out_d[0:128, :], g2b[:])
        P.emit(es)
        es.close()
        return nc
    stg = av(0, INW)
    stgb = av(4608, 4608 + INW // 2, BF16)
    for c in range(8):
        P.dma(stg, w_in[c * 128:(c + 1) * 128, :])
        P.act(stgb, stg, AF.Copy, scale=g1[:, c:c + 1])
        P.dma(winbf[c * 128:(c + 1) * 128, :], stgb)
        P.copy(wk[:, c, 0:256], (stgb[0][:, 1536:1792], stgb[1]))
        P.copy(wk[:, c, 256:320], (stgb[0][:, 2048:2112], stgb[1]))
    s2 = av(8192, 8192 + 2048)
    s2b = av(12288, 12288 + 1024, BF16)
    for c in range(8):
        P.dma(s2, wq_d[c * 128:(c + 1) * 128, :])
        P.act(s2b, s2, AF.Copy, scale=g2[:, c:c + 1])
        P.dma(wqbf[c * 128:(c + 1) * 128, :], s2b)
    s3 = av(10240, 10240 + 1024)
    s3b = av(13312, 13312 + 512, BF16)
    for c in range(4):
        P.dma(s3, wa_d[c * 128:(c + 1) * 128, :])
        P.copy(s3b, s3)
        P.dma(wabf[c * 128:(c + 1) * 128, :], s3b)
        P.dma(s3, wb_d[c * 128:(c + 1) * 128, :])
        P.copy(s3b, s3, eng="act")
        P.dma(wbbf[c * 128:(c + 1) * 128, :], s3b)
    for c in range(8):
        P.dma(s3, wo_d[c * 128:(c + 1) * 128, :])
        P.copy(s3b, s3)
        P.dma(wobf[c * 128:(c + 1) * 128, :], s3b)
    s4 = av(11264, 11264 + 128)
    for c in range(2):
        P.dma(s4, wuk_d[c * 128:(c + 1) * 128, :])
        P.act(wkv[:, c, 0:128], s4, AF.Copy, scale=kvg[:, c:c + 1])
        P.dma(s4, wuv_d[c * 128:(c + 1) * 128, :])
        P.act(wkv[:, c, 128:256], s4, AF.Copy, scale=kvg[:, c:c + 1])
    s5 = av(11776, 11776 + 64, BF16)
    for g in range(16):
        P.dma(s4, sk_d[g])
        P.copy(s5, s4)
        P.transpose(psb(7)[:, 0:128], s5, ident[:])
        P.copy(skT[:, g, :], psb(7)[:, 0:128])
    for g in range(4):
        P.dma(s4, spw_d[g])
        P.tt(s5, s4, trilm[:], ALU.mult)
        P.transpose(psb(7)[:, 0:128], s5, ident[:])
        P.copy(wsT[:, g, :], psb(7)[:, 0:128])

    if dbg == "C1":
        P.dma(out_d[0:128, :], g2b[:])
        P.emit(es)
        es.close()
        return nc
    xt = [T("xt%d" % i, [128, D]) for i in range(2)]
    xsb = T("xsb", [128, D], BF16)
    xsT = T("xsT", [128, 8, 128], BF16)
    st = T("stats", [128, 64])
    ang = T("ang", [128, 24])
    a12 = T("a12", [128, 48])
    a12f = T("a12f", [128, 48])
    a12i = T("a12i", [128, 48], I32)
    sc = T("sincos", [128, 48])
    rtmp = T("rtmp", [128, 4, 64])
    rtmp2 = T("rtmp2", [128, 4, 64])
    junkb = T("junkb", [128, 2048], BF16)
    junkf32 = T("junkf32", [128, 1024])
    junkf32b = T("junkf32b", [128, 256])
    cnb = T("cnb", [128, 256], BF16)
    cnT = T("cnT", [128, 2, 128], BF16)
    kn = T("kn", [128, 128])
    kbf = T("kbf", [128, 128], BF16)
    kif = T("kif", [128, 64])
    kibf = T("kibf", [128, 128], BF16)
    P.memset(kibf[:], 0.0)
    vvb = T("vvb", [128, 128], BF16)
    kTs = T("kTs", [128, 128], BF16)
    kiTs = T("kiTs", [64, 128], BF16)
    pooldummy = T("pooldummy", [128, 8], BF16)

    def sumsq(src, n, acc):
        if n <= 256:
            P.copy(junkf32b[:, 0:n], src)
            src = junkf32b[:, 0:n]
        P.dot(junkf32[:, 0:n], src, src, acc)

    def rstd_from_ss(dst, ss, n):
        P.act(dst, ss, AF.Sqrt, bias=epsb[:, 0:1], scale=1.0 / n)
        rk, wk_ = P._rw([dst], [dst])
        P.op("dve", lambda e, o=_k(dst)[0]: e.reciprocal(o, o), rk, wk_)

    def sincos(pos_col):
        P.ts(a12[:, 0:24], invf[:], pos_col, None, ALU.mult)
        P.ts(a12[:, 24:48], invf[:], pos_col, 0.5 * PI, ALU.mult, ALU.add)
        P.ts(a12f[:], a12[:], 1.0 / (2 * PI), None, ALU.mult)
        P.copy(a12i[:], a12f[:])
        P.copy(a12f[:], a12i[:])
        P.stt(a12[:], a12f[:], -2 * PI, a12[:], ALU.mult, ALU.add)
        P.ts(a12f[:], a12[:], PI, None, ALU.is_gt)
        P.stt(a12[:], a12f[:], -2 * PI, a12[:], ALU.mult, ALU.add)
        P.ts(a12f[:], a12[:], -PI, None, ALU.is_lt)
        P.stt(a12[:], a12f[:], 2 * PI, a12[:], ALU.mult, ALU.add)
        P.act(sc[:], a12[:], AF.Sin)

    def rope(dst, src, nh, dim, half, off, scv=None):
        if scv is None:
            scv = sc[:]
        sca, sck = _k(scv)
        sinb = (sca[:, off:off + half].unsqueeze(1).to_broadcast([128, nh, half]), sck)
        cosb = (sca[:, 24 + off:24 + off + half].unsqueeze(1).to_broadcast([128, nh, half]), sck)
        sa, sk_ = _k(src)
        da, dk_ = _k(dst)
        x1 = (sa[:, :, 0:half], sk_)
        x2 = (sa[:, :, half:2 * half], sk_)
        t1 = rtmp[:, 0:nh, 0:half]
        t2 = rtmp2[:, 0:nh, 0:half]
        P.tt(t1, x1, cosb, ALU.mult)
        P.tt(t2, x2, sinb, ALU.mult)
        P.tt((da[:, :, 0:half], dk_), t1, t2, ALU.subtract)
        t3 = rtmp[:, 0:nh, 16:16 + half]
        t4 = rtmp2[:, 0:nh, 16:16 + half]
        P.tt(t3, x2, cosb, ALU.mult)
        P.tt(t4, x1, sinb, ALU.mult)
        P.tt((da[:, :, half:2 * half], dk_), t3, t4, ALU.add)
        P.copy((da[:, :, 2 * half:dim], dk_), (sa[:, :, 2 * half:dim], sk_), eng="act")

    def front(xtile, bank):
        sumsq(xtile, D, st[:, 0:1])
        rstd_from_ss(st[:, 1:2], st[:, 0:1], D)
        P.act(xsb[:], xtile, AF.Copy, scale=st[:, 1:2])
        for c in range(8):
            P.transpose(psb(bank)[:, c * 128:(c + 1) * 128], xsb[:, c * 128:(c + 1) * 128], ident[:])
        P.copy(xsT[:].rearrange("p c t -> p (c t)"), psb(bank))

    def sincos_batch(dst, pos_all, t0, t1, A, Af, Ai):
        n = t1 - t0
        Aa, Ak = A
        A3 = Aa.rearrange("p (t c) -> p t c", c=48)
        invb = invf[:].unsqueeze(1).to_broadcast([128, n, 24])
        posb = pos_all[:, t0:t1].unsqueeze(2).to_broadcast([128, n, 24])
        P.tt((A3[:, :, 0:24], Ak), invb, posb, ALU.mult)
        P.ts((A3[:, :, 24:48], Ak), (A3[:, :, 0:24], Ak), 0.5 * PI, None, ALU.add)
        P.ts(Af, A, 1.0 / (2 * PI), None, ALU.mult)
        P.copy(Ai, Af)
        P.copy(Af, Ai)
        P.stt(A, Af, -2 * PI, A, ALU.mult, ALU.add)
        P.ts(Af, A, PI, None, ALU.is_gt)
        P.stt(A, Af, -2 * PI, A, ALU.mult, ALU.add)
        P.ts(Af, A, -PI, None, ALU.is_lt)
        P.stt(A, Af, 2 * PI, A, ALU.mult, ALU.add)
        P.act(dst, A, AF.Sin)


    SC5 = [None]

    def ktile(kt):
        xtile = xt[kt % 2]
        P.dma(xtile[:], x_seq[kt // TPP][(kt % TPP) * 128:(kt % TPP + 1) * 128, :])
        front(xtile[:], 7)
        for c in range(8):
            P.matmul(ps[0][:, 0:320], xsT[:, c, :], wk[:, c, :], start=(c == 0), stop=(c == 7))
        if SC5[0] is not None:
            sckt = SC5[0][:, (kt % 4) * 48:(kt % 4 + 1) * 48]
        else:
            sincos(posk[:, kt:kt + 1])
            sckt = None
        for _d in range(NDUMMY):
            P.act(junkb[:], junkb[:], AF.Copy)
        sumsq(ps[0][:, 0:256], 256, st[:, 2:3])
        rstd_from_ss(st[:, 3:4], st[:, 2:3], 256)
        P.act(cnb[:], ps[0][:, 0:256], AF.Copy, scale=st[:, 3:4])
        P.copy(kif[:], ps[0][:, 256:320])
        rope(kibf[:, 0:64].rearrange("p (h d) -> p h d", h=1), kif[:].rearrange("p (h d) -> p h d", h=1), 1, 64, 8, 16, sckt)
        for c in range(2):
            P.transpose(psb(6)[:, c * 128:(c + 1) * 128], cnb[:, c * 128:(c + 1) * 128], ident[:])
        P.copy(cnT[:].rearrange("p c t -> p (c t)"), psb(6)[:, 0:256])
        for c in range(2):
            P.matmul(ps[1][:, 0:256], cnT[:, c, :], wkv[:, c, :], start=(c == 0), stop=(c == 1))
        if dbg == "K3":
            return
        sumsq(ps[1][:, 0:128], 128, st[:, 4:5])
        rstd_from_ss(st[:, 5:6], st[:, 4:5], 128)
        P.stt(kn[:], ps[1][:, 0:128], st[:, 5:6], kgb[:], ALU.mult, ALU.mult)
        rope(kbf[:].rearrange("p (h d) -> p h d", h=1), kn[:].rearrange("p (h d) -> p h d", h=1), 1, 128, 16, 0, sckt)
        P.copy(vvb[:], ps[1][:, 128:256], eng="act")
        P.transpose(psb(5)[:, 0:128], kbf[:], ident[:])
        P.transpose(psb(5)[:, 128:256], kibf[:], ident[:])
        P.copy(kTs[:], psb(5)[:, 0:128], eng="act")
        P.copy(kiTs[:], psb(5)[0:64, 128:256], eng="act")
        if dbg == "K4":
            return
        if dbg != "K6":
            P.dma(vv_d[kt * 128:(kt + 1) * 128, :], vvb[:])
        if dbg == "K5":
            return
        P.dma(kT_d[kt], kTs[:])
        if dbg == "K6":
            return
        P.dma(kiT_d[kt], kiTs[:])


    if dbg and dbg[0] == "K":
        for kt in range(KT0, NKT if KT1 is None else KT1):
            ktile(kt)

    if dbg and dbg[0] == "K":
        P.dma(out_d[0:128, :], xt[0][:])
        P.emit(es)
        es.close()
        return nc

    wbuf = [T("wbuf%d" % i, [128, 2048], BF16) for i in range(3)]
    wctr = [0]

    def wstream(src, a, b):
        buf = wbuf[wctr[0] % 3]
        wctr[0] += 1
        view = buf[:, 0:a * b].rearrange("p (a b) -> p a b", a=a)
        P.dma(view, src.rearrange("(c p) n -> p c n", p=128))
        return view

    ug = T("ug", [128, 512])
    vg = T("vg", [128, 512])
    gt = T("gt", [128, 1024])
    vnbf = T("vnbf", [128, 512], BF16)
    yab = T("yab", [128, 512], BF16)
    yaT = T("yaT", [128, 4, 128], BF16)
    sg = T("sg", [128, 2048])
    merged = T("merged", [128, D])
    mbf = T("mbf", [128, D], BF16)
    mT = T("mT", [128, 8, 128], BF16)
    qn = T("qn", [128, 4, 128])
    qbf = T("qbf", [128, 4, 128], BF16)
    qT = T("qT", [128, 4, 128], BF16)
    qir = T("qir", [128, 4, 64])
    qibf = T("qibf", [128, 4, 128], BF16)
    qiT = T("qiT", [128, 4, 128], BF16)
    P.memset(qibf[:], 0.0)
    kich = [T("kich%d" % i, [64, 512], BF16) for i in range(2)]
    kch = [T("kch%d" % i, [128, 512], BF16) for i in range(2)]
    vch = [T("vch%d" % i, [128, 4, 128], BF16) for i in range(2)]
    rl = [T("rl%d" % i, [128, 512]) for i in range(4)]
    maskc = T("maskc", [128, 512], BF16)
    maskT = T("maskT", [128, 4, 128], BF16)
    eT = [T("eT%d" % i, [128, 512], BF16) for i in range(2)]
    eTm = [T("eTm%d" % i, [128, 512], BF16) for i in range(2)]
    rz = T("rz", [128, 512])
    ybT = T("ybT", [128, 4, 128], BF16)
    hres = T("hres", [128, D])
    sc5 = T("sc5", [128, 240])

    def sincos5(j):
        A = junkf32[:, 0:240]
        Af = junkf32[:, 256:496]
        Ai = junkf32[:, 512:752].bitcast(I32)
        A3 = A.rearrange("p (t c) -> p t c", c=48)
        P.tt(A3[:, 0:4, 0:24], invf[:].unsqueeze(1).to_broadcast([128, 4, 24]),
             posk[:, 4 * j:4 * j + 4].unsqueeze(2).to_broadcast([128, 4, 24]), ALU.mult)
        P.ts(A3[:, 4, 0:24], invf[:], poso[:, j:j + 1], None, ALU.mult)
        P.ts(A3[:, :, 24:48], A3[:, :, 0:24], 0.5 * PI, None, ALU.add)
        P.ts(Af, A, 1.0 / (2 * PI), None, ALU.mult)
        P.copy(Ai, Af)
        P.copy(Af, Ai)
        P.stt(A, Af, -2 * PI, A, ALU.mult, ALU.add)
        P.ts(Af, A, PI, None, ALU.is_gt)
        P.stt(A, Af, -2 * PI, A, ALU.mult, ALU.add)
        P.ts(Af, A, -PI, None, ALU.is_lt)
        P.stt(A, Af, 2 * PI, A, ALU.mult, ALU.add)
        P.act(sc5[:], A, AF.Sin)

    hsb = T("hsb", [128, D], BF16)
    hsT = T("hsT", [128, 8, 128], BF16)
    qpT = T("qpT", [128, 16, 128], BF16)
    vals = T("vals", [128, 16, 16])
    idxu = T("idxu", [128, 16, 16], U32)
    idxf = T("idxf", [128, 16, 16])
    best = T("best", [128, 8, 16])
    posu = T("posu", [128, 8, 16], U32)
    pa_i = T("pa_i", [128, 8, 16], U32)
    pb_i = T("pb_i", [128, 8, 16], U32)
    pa_f = T("pa_f", [128, 8, 16])
    pb_f = T("pb_f", [128, 8, 16])
    sel1 = T("sel1", [128, 8, 16])
    sel2 = T("sel2", [128, 8, 16])
    eidf = T("eidf", [128, 128])
    eid = T("eid", [128, 128], I32)
    gate = T("gate", [128, 8, 16])
    adot = T("adot", [128, 128])
    cw = T("cw", [128, 128])
    gtmp = T("gtmp", [128, 128])

    def gelu(dst, src, tmp, n):
        P.tt(tmp, src, src, ALU.mult)
        P.ts(tmp, tmp, 0.044715, 1.0, ALU.mult, ALU.add)
        P.tt(tmp, tmp, src, ALU.mult)
        P.act(tmp, tmp, AF.Sigmoid, scale=1.5957691216057308)
        P.tt(dst, src, tmp, ALU.mult)

    def top16(vout, iout, src, tmp):
        va, vk = _k(vout)
        ia, ik = _k(iout)
        rk, wk_ = P._rw([vout], [src])
        P.op("dve", lambda e, o=va[:, 0:8], i=_k(src)[0]: e.max(o, i), rk, wk_)
        rk, wk_ = P._rw([iout], [vout, src])
        P.op("dve", lambda e, o=ia[:, 0:8], m=va[:, 0:8], i=_k(src)[0]: e.max_index(o, m, i), rk, wk_)
        rk, wk_ = P._rw([tmp], [vout, src])
        P.op("dve", lambda e, o=_k(tmp)[0], m=va[:, 0:8], i=_k(src)[0]: e.match_replace(o, m, i, -1e30), rk, wk_)
        rk, wk_ = P._rw([vout], [tmp])
        P.op("dve", lambda e, o=va[:, 8:16], i=_k(tmp)[0]: e.max(o, i), rk, wk_)
        rk, wk_ = P._rw([iout], [vout, tmp])
        P.op("dve", lambda e, o=ia[:, 8:16], m=va[:, 8:16], i=_k(tmp)[0]: e.max_index(o, m, i), rk, wk_)

    proj = av(0, INW)
    pj = proj[0]
    pjk = proj[1]

    for j in range(NOWN):
        nch = (4 * j + 4) * 128 // 512
        sincos5(j)
        SC5[0] = sc5
        for kt in range(4 * j, 4 * j + 4):
            ktile(kt)
        xres = xt[j % 2]
        P.dma(xres[:], x_own[j * 128:(j + 1) * 128, :])
        front(xres[:], 7)
        ncc = (INW + 255) // 256
        for cc in range(ncc):
            c0 = cc * 256
            w = min(256, INW - c0)
            wbf = wstream(winbf[:, c0:c0 + w], 8, w)
            bank = cc % 2
            for c in range(8):
                P.matmul(ps[bank][:, 0:w], xsT[:, c, :], wbf[:, c, :], start=(c == 0), stop=(c == 7))
            P.copy((pj[:, c0:c0 + w], pjk), ps[bank][:, 0:w], eng=("act" if cc % 2 else "dve"))
        gelu(ug[:], (pj[:, 0:512], pjk), gt[:, 0:512], 512)
        gelu(vg[:], (pj[:, 512:1024], pjk), gt[:, 512:1024], 512)
        P.reduce(st[:, 8:9], vg[:], ALU.add)
        P.dot(junkf32[:, 0:512], vg[:], vg[:], st[:, 9:10])
        P.ts(st[:, 10:11], st[:, 8:9], 1.0 / 512, None, ALU.mult)
        P.tt(st[:, 11:12], st[:, 10:11], st[:, 10:11], ALU.mult)
        P.stt(st[:, 12:13], st[:, 9:10], 1.0 / 512, st[:, 11:12], ALU.mult, ALU.subtract)
        rstd_from_ss(st[:, 12:13], st[:, 12:13], 1)
        P.ts(vg[:], vg[:], st[:, 10:11], st[:, 12:13], ALU.subtract, ALU.mult)
        P.tt(vg[:], vg[:], vng[:], ALU.mult)
        P.tt(vnbf[:], vg[:], vnb[:], ALU.add)
        for g in range(4):
            P.matmul(ps[2][:, g * 128:(g + 1) * 128], wsT[:, g, :], vnbf[:, g * 128:(g + 1) * 128])
        for g in range(4):
            P.stt(yab[:, g * 128:(g + 1) * 128], ps[2][:, g * 128:(g + 1) * 128], spb[:, g:g + 1],
                  ug[:, g * 128:(g + 1) * 128], ALU.add, ALU.mult)
        for c in range(4):
            P.transpose(psb(3)[:, c * 128:(c + 1) * 128], yab[:, c * 128:(c + 1) * 128], ident[:])
        P.copy(yaT[:].rearrange("p c t -> p (c t)"), psb(3)[:, 0:512])
        for hf in range(2):
            wv = wstream(wabf[:, hf * 512:(hf + 1) * 512], 4, 512)
            for c in range(4):
                P.matmul(ps[4 + hf][:], yaT[:, c, :], wv[:, c, :], start=(c == 0), stop=(c == 3))
        P.act(sg[:], (pj[:, 2116:4164], pjk), AF.Sigmoid)
        for hf in range(2):
            P.tt(merged[:, hf * 512:(hf + 1) * 512], sg[:, hf * 512:(hf + 1) * 512], ps[4 + hf][:], ALU.mult)
        P.tt(junkf32[:, 0:512], (pj[:, 1024:1536], pjk), (pj[:, 1024:1536], pjk), ALU.mult)
        P.reduce(st[:, 16:20], junkf32[:, 0:512].rearrange("p (h d) -> p h d", h=4), ALU.add)
        rstd_from_ss(st[:, 20:24], st[:, 16:20], 128)
        for h in range(4):
            P.stt(qn[:, h, :], (pj[:, 1024 + h * 128:1024 + (h + 1) * 128], pjk), st[:, 20 + h:21 + h], qgb[:], ALU.mult, ALU.mult)
        rope(qbf[:], qn[:], 4, 128, 16, 0, sc5[:, 192:240])
        for h in range(4):
            P.transpose(psb(6)[:, h * 128:(h + 1) * 128], qbf[:, h, :], ident[:])
        P.copy(qT[:].rearrange("p h t -> p (h t)"), psb(6)[:, 0:512])
        wid = (pj[:, 2112:2116], pjk)
        P.ts(st[:, 28:32], wid, 0.0, 2.0, ALU.is_gt, ALU.mult)
        P.ts(st[:, 28:32], st[:, 28:32], -1.0, None, ALU.add)
        P.stt(st[:, 24:28], wid, 0.0625, st[:, 28:32], ALU.mult, ALU.mult)
        rope(qir[:], (pj[:, 1792:2048].rearrange("p (h d) -> p h d", h=4), pjk), 4, 64, 8, 16, sc5[:, 192:240])
        P.tt(qibf[:, :, 0:64], qir[:], st[:, 24:28].unsqueeze(2).to_broadcast([128, 4, 64]), ALU.mult)
        for h in range(4):
            P.transpose(psb(7)[:, h * 128:(h + 1) * 128], qibf[:, h, :], ident[:])
        P.copy(qiT[:].rearrange("p h t -> p (h t)"), psb(7)[:, 0:512])
        for c in range(nch):
            kb = kich[c % 2]
            P.dma(kb[:].rearrange("p (k t) -> p k t", k=4), kiT_d[c * 4:(c + 1) * 4].rearrange("k p t -> p k t"))
            scv = av(c * 512, (c + 1) * 512)
            for h in range(4):
                P.matmul(ps[h][:], qiT[0:64, h, :], kb[:])
            for h in range(4):
                P.act(rl[h][:], ps[h][:], AF.Relu)
            P.ts(scv, rl[0][:], st[:, 28:29], None, ALU.mult)
            for h in range(1, 4):
                P.stt(scv, rl[h][:], st[:, 28 + h:29 + h], scv, ALU.mult, ALU.add)
            if c == nch - 1:
                P.tt(scv, scv, maskb[:], ALU.add)
        n = nch * 512
        lo, hi, mid, cnt, ge, dd = (st[:, 32:33], st[:, 33:34], st[:, 34:35], st[:, 35:36], st[:, 36:37], st[:, 37:38])
        cnts = st[:, 40:48]
        P.memset(mid, 0.0)
        pieces = [(p0, min(p0 + 2048, n)) for p0 in range(0, n, 2048)]
        hstep = 16.0
        ktop = min(TOPK, S // 4) - 0.5
        for it in range(BIS_ITERS):
            for pi_, (p0, p1) in enumerate(pieces):
                P.ts(junkb[:, 0:p1 - p0], av(p0, p1), mid, None, ALU.is_ge, ALU.add, accum=cnts[:, pi_:pi_ + 1])
            if len(pieces) > 1:
                P.reduce(cnt, cnts[:, 0:len(pieces)], ALU.add)
                cc_ = cnt
            else:
                cc_ = cnts[:, 0:1]
            P.ts(dd, cc_, ktop, hstep, ALU.is_ge, ALU.mult)
            P.stt(mid, dd, -0.5 * hstep, mid, ALU.add, ALU.add)
            hstep *= 0.5
        P.ts(lo, mid, -hstep, None, ALU.add)
        nkb = nch * 4
        for c in range(nch):
            kc = kch[c % 2]
            vc = vch[c % 2]
            P.dma(kc[:].rearrange("p (k t) -> p k t", k=4), kT_d[c * 4:(c + 1) * 4].rearrange("k p t -> p k t"))
            P.dma(vc[:], vv_d[c * 512:(c + 1) * 512, :].rearrange("(kb p) d -> p kb d", p=128))
            P.ts(maskc[:], av(c * 512, (c + 1) * 512), lo, None, ALU.is_ge)
            for b in range(4):
                P.transpose(psb(0)[:, b * 128:(b + 1) * 128], maskc[:, b * 128:(b + 1) * 128], ident[:])
            P.copy(maskT[:].rearrange("p b q -> p (b q)"), psb(0)[:, 0:512])
            for b in range(4):
                gkb = c * 4 + b
                bank = 1 + (gkb % 2)
                P.matmul(ps[bank][:], kc[:, b * 128:(b + 1) * 128], qT[:].rearrange("p h t -> p (h t)"))
                e1 = eT[gkb % 2]
                e2 = eTm[gkb % 2]
                P.act(e1[:], ps[bank][:], AF.Exp, scale=float(128 ** -0.5))
                P.tt(e2[:].rearrange("p (h q) -> p h q", h=4), e1[:].rearrange("p (h q) -> p h q", h=4),
                     maskT[:, b, :].unsqueeze(1).to_broadcast([128, 4, 128]), ALU.mult)
                P.matmul(ps[4][:], vc[:, b, :], e2[:], start=(gkb == 0), stop=(gkb == nkb - 1))
                P.matmul(ps[5][:], ones[:], e2[:], start=(gkb == 0), stop=(gkb == nkb - 1))
        if dbg == "H" and j == NOWN - 1:
            P.dma(d_mT.ap(), maskT[:].rearrange("p b q -> p (b q)"))
            P.dma(d_e1.ap(), eT[(nkb - 1) % 2][:])
            P.dma(d_e2.ap(), eTm[(nkb - 1) % 2][:])
            P.copy(gt[:, 0:512], ps[4][:])
            P.dma(d_num.ap(), gt[:, 0:512])
        P.op("dve", lambda e: e.reciprocal(rz[:], ps[5][:]), ["ps5"], ["rz"])
        if dbg == "H" and j == NOWN - 1:
            P.dma(d_rz.ap(), rz[:])
        P.copy(gt[:, 512:1024], ps[4][:], eng="act")
        P.tt(ybT[:].rearrange("p h t -> p (h t)"), gt[:, 512:1024], rz[:], ALU.mult)
        for hf in range(2):
            wv = wstream(wbbf[:, hf * 512:(hf + 1) * 512], 4, 512)
            for h in range(4):
                P.matmul(ps[6 + hf][:], ybT[:, h, :], wv[:, h, :], start=(h == 0), stop=(h == 3))
        for hf in range(2):
            P.tt(gt[:, hf * 512:(hf + 1) * 512], sg[:, 1024 + hf * 512:1024 + (hf + 1) * 512], ps[6 + hf][:], ALU.mult)
        if dbg == "H" and j == NOWN - 1:
            P.dma(d_yb.ap(), ybT[:].rearrange("p h t -> p (h t)"))
            P.dma(d_gb.ap(), gt[:])
        P.tt(mbf[:], merged[:], gt[:], ALU.add)
        for c in range(8):
            P.transpose(psb(0)[:, c * 128:(c + 1) * 128], mbf[:, c * 128:(c + 1) * 128], ident[:])
        P.copy(mT[:].rearrange("p c t -> p (c t)"), psb(0))
        for qd in range(4):
            wv = wstream(wobf[:, qd * 256:(qd + 1) * 256], 8, 256)
            for c in range(8):
                P.matmul(ps[1 + qd // 2][:, (qd % 2) * 256:(qd % 2 + 1) * 256], mT[:, c, :], wv[:, c, :],
                         start=(c == 0), stop=(c == 7))
        for hf in range(2):
            P.tt(hres[:, hf * 512:(hf + 1) * 512], xres[:, hf * 512:(hf + 1) * 512], ps[1 + hf][:], ALU.add)
        if dbg == "H":
            P.dma(out_d[j * 128:(j + 1) * 128, :], hres[:])
            continue
        hn = av(0, 1024)
        sub = av(1024, 3072)
        sub2 = av(3072, 5120)
        cand = av(5120, 7168)
        oh = av(7168, 9216)
        junkf = av(9216, 10240)
        gbuf = [av(10240 + r * 1024, 10240 + (r + 1) * 1024) for r in range(6)]
        sumsq(hres[:], D, st[:, 48:49])
        rstd_from_ss(st[:, 49:50], st[:, 48:49], D)
        P.act(hsb[:], hres[:], AF.Copy, scale=st[:, 49:50])
        P.stt(hn, hres[:], st[:, 49:50], g2b[:], ALU.mult, ALU.mult)
        for c in range(8):
            P.transpose(psb(2)[:, c * 128:(c + 1) * 128], hsb[:, c * 128:(c + 1) * 128], ident[:])
        P.copy(hsT[:].rearrange("p c t -> p (c t)"), psb(2))
        for gp in range(8):
            wv = wstream(wqbf[:, gp * 256:(gp + 1) * 256], 8, 256)
            for g2_ in range(2):
                g = gp * 2 + g2_
                bank = 3 + g // 4
                for c in range(8):
                    P.matmul(ps[bank][:, (g % 4) * 128:(g % 4 + 1) * 128], wv[:, c, g2_ * 128:(g2_ + 1) * 128], hsT[:, c, :],
                             start=(c == 0), stop=(c == 7))
        for b in range(4):
            P.copy(qpT[:, b * 4:(b + 1) * 4, :].rearrange("p g t -> p (g t)"), ps[3 + b][:], eng=("act" if b % 2 else "dve"))
        sbanks = [7, 0, 1, 2]
        for g in range(16):
            P.matmul(ps[sbanks[g // 4]][:, (g % 4) * 128:(g % 4 + 1) * 128], qpT[:, g, :], skT[:, g, :])
        for b in range(4):
            P.copy((sub[0][:, b * 512:(b + 1) * 512], sub[1]), ps[sbanks[b]][:], eng=("act" if b % 2 else "dve"))
        for g in range(16):
            top16(vals[:, g, :], idxu[:, g, :], (sub[0][:, g * 128:(g + 1) * 128], sub[1]),
                  (sub2[0][:, g * 128:(g + 1) * 128], sub2[1]))
        v4 = vals[:].rearrange("p (h t) k -> p h t k", t=2)
        c4 = (cand[0].rearrange("p (h a b) -> p h a b", h=8, a=16), cand[1])
        P.tt(c4, v4[:, :, 0, :].unsqueeze(3).to_broadcast([128, 8, 16, 16]),
             v4[:, :, 1, :].unsqueeze(2).to_broadcast([128, 8, 16, 16]), ALU.add)
        for h in range(8):
            top16(best[:, h, :], posu[:, h, :], (cand[0][:, h * 256:(h + 1) * 256], cand[1]),
                  (sub2[0][:, h * 256:(h + 1) * 256], sub2[1]))
        P.op("dve", lambda e: e.tensor_single_scalar(pa_i[:], posu[:], 4, ALU.logical_shift_right), ["posu"], ["pa_i"])
        P.op("dve", lambda e: e.tensor_single_scalar(pb_i[:], posu[:], 15, ALU.bitwise_and), ["posu"], ["pb_i"])
        P.copy(pa_f[:], pa_i[:])
        P.copy(pb_f[:], pb_i[:])
        P.copy(idxf[:], idxu[:])
        i4 = idxf[:].rearrange("p (h t) k -> p h t k", t=2)
        o4 = (oh[0].rearrange("p (h k a) -> p h k a", h=8, k=16), oh[1])
        iob = iota16[:].unsqueeze(1).unsqueeze(1).to_broadcast([128, 8, 16, 16])
        for (pf, t_, so) in ((pa_f, 0, sel1), (pb_f, 1, sel2)):
            P.tt(o4, iob, pf[:].unsqueeze(3).to_broadcast([128, 8, 16, 16]), ALU.is_equal)
            P.tt(o4, o4, i4[:, :, t_, :].unsqueeze(2).to_broadcast([128, 8, 16, 16]), ALU.mult)
            P.reduce(so[:], o4, ALU.add)
        P.stt(eidf[:].rearrange("p (h k) -> p h k", h=8), sel1[:], 128.0, sel2[:], ALU.mult, ALU.add)
        P.copy(eid[:], eidf[:])
        P.reduce(st[:, 50:58], best[:], ALU.max)
        P.tt(gate[:], best[:], st[:, 50:58].unsqueeze(2).to_broadcast([128, 8, 16]), ALU.subtract)
        P.act(gate[:], gate[:], AF.Exp)
        P.reduce(rz[:, 0:8], gate[:], ALU.add)
        P.op("dve", lambda e: e.reciprocal(rz[:, 8:16], rz[:, 0:8]), ["rz"], ["rz"])
        P.tt(gate[:], gate[:], rz[:, 8:16].unsqueeze(2).to_broadcast([128, 8, 16]), ALU.mult)
        for s_ in range(128):
            gb = gbuf[s_ % 6]
            P.gather(gb, u_tab.ap(), eid[:, s_:s_ + 1])
            P.dot(junkf, gb, hn, adot[:, s_:s_ + 1])
        gelu(cw[:], adot[:], gtmp[:], 128)
        P.tt(cw[:], cw[:], gate[:].rearrange("p h k -> p (h k)"), ALU.mult)
        for s_ in range(128):
            gb = gbuf[s_ % 6]
            P.gather(gb, v_tab.ap(), eid[:, s_:s_ + 1])
            P.stt(hres[:], gb, cw[:, s_:s_ + 1], hres[:], ALU.mult, ALU.add)
        P.dma(out_d[j * 128:(j + 1) * 128, :], hres[:])

    P.emit(es)
    es.close()
    return nc


def _host_inputs(inp, S, NOWN):
    f32 = np.float32
    B = inp["x"].shape[0]
    x = np.asarray(inp["x"], f32)
    pos = np.asarray(inp["positions"], np.int32)
    NKT = S // 128
    rep = lambda v, n=128: np.ascontiguousarray(np.broadcast_to(np.asarray(v, f32).reshape(1, -1), (n, np.asarray(v).size)))
    pc = lambda v, c: np.ascontiguousarray(np.asarray(v, f32).reshape(c, 128).T)
    half_q = np.arange(16, dtype=f32)
    half_i = np.arange(8, dtype=f32)
    inv_q = np.power(f32(500000.0), -half_q * f32(2.0) / f32(32)).astype(f32)
    inv_i = np.power(f32(500000.0), -half_i * f32(2.0) / f32(16)).astype(f32)
    invf = rep(np.concatenate([inv_q, inv_i]))
    common = {
        "w_in": np.ascontiguousarray(inp["w_in"][0], f32),
        "g1": pc(inp["norm1_g"][0], 8),
        "g2": pc(inp["norm2_g"][0], 8),
        "g2b": rep(inp["norm2_g"][0]),
        "kvg": pc(inp["kv_norm_g"][0], 2),
        "vng": rep(inp["v_norm_g"][0]),
        "vnb": rep(inp["v_norm_b"][0]),
        "spw": np.ascontiguousarray(inp["spatial_w"][0], f32),
        "spb": np.ascontiguousarray(np.asarray(inp["spatial_b"][0], f32).T),
        "wuk": np.ascontiguousarray(inp["w_uk"][0], f32),
        "wuv": np.ascontiguousarray(inp["w_uv"][0], f32),
        "qgb": rep(inp["q_norm_g"][0]),
        "kgb": rep(inp["k_norm_g"][0]),
        "wa": np.ascontiguousarray(inp["w_a_out"][0], f32),
        "wb": np.ascontiguousarray(inp["w_b_out"][0], f32),
        "wo": np.ascontiguousarray(inp["w_o"][0], f32),
        "wq": np.ascontiguousarray(inp["peer_wq"][0], f32),
        "subk": np.ascontiguousarray(np.asarray(inp["peer_subkeys"][0], f32).reshape(16, 128, 128)),
        "peer_u": np.ascontiguousarray(inp["peer_u"][0], f32),
        "peer_v": np.ascontiguousarray(inp["peer_v"][0], f32),
        "ident": np.eye(128, dtype=f32).astype(ml_dtypes.bfloat16),
        "tril": np.tril(np.ones((128, 128), f32)),
        "invf": invf,
        "iota16": rep(np.arange(16, dtype=f32)),
    }
    maps = []
    lanes = 8 // B
    for core in range(8):
        b, c = core // lanes, core % lanes
        tiles = [lanes * j + c for j in range(NOWN)]
        rows = np.concatenate([np.arange(t * 128, (t + 1) * 128) for t in tiles])
        mb = np.zeros((128, 512), f32)
        for r in range(4):
            blk = mb[:, r * 128:(r + 1) * 128]
            if r > c:
                blk[:] = -1e30
            elif r == c:
                blk[:] = np.where(np.arange(128)[None, :] <= np.arange(128)[:, None], 0.0, -1e30)
        m = dict(common)
        for i in range(4):
            m["x_seq%d" % i] = np.ascontiguousarray(x[b][i * (S // 4):(i + 1) * (S // 4)])
        m["x_own"] = np.ascontiguousarray(x[b][rows])
        m["pos_seq"] = np.ascontiguousarray(pos[b].reshape(NKT, 128).T)
        m["pos_own"] = np.ascontiguousarray(pos[b][rows].reshape(NOWN, 128).T)
        m["maskb"] = mb
        maps.append((m, b, rows))
    return maps


_NC_CACHE = {}


def kernel(**inputs):
    x = np.asarray(inputs["x"])
    B, S, _ = x.shape
    lanes = 8 // B
    NOWN = S // 128 // lanes
    key = (S, NOWN)
    if key not in _NC_CACHE:
        _NC_CACHE[key] = build_program(S, NOWN)
    nc = _NC_CACHE[key]
    maps = _host_inputs(inputs, S, NOWN)
    res = run_bass_kernel_spmd(nc, [m for m, _, _ in maps], core_ids=list(range(8)))
    out = np.empty((B, S, D), np.float32)
    for (m, b, rows), r in zip(maps, res.results):
        out[b, rows] = r["out"]
    return out
```

```python
import numpy as np
import ml_dtypes
from contextlib import ExitStack
import concourse.bass as bass
import concourse.mybir as mybir
from concourse.bass_utils import run_bass_kernel_spmd

F32 = mybir.dt.float32
BF16 = mybir.dt.bfloat16
I32 = mybir.dt.int32
U32 = mybir.dt.uint32
ALU = mybir.AluOpType
AF = mybir.ActivationFunctionType
AX = mybir.AxisListType

D = 1024
INW = 4164
EPS = 1e-6
TOPK = 256
NEXP = 16384
PI = float(np.pi)
ENGS = ("pe", "act", "dve", "pool", "sp")
KDMA = 8
BIS_ITERS = 23
KT0 = 0
NDUMMY = 0
KT1 = None


def _k(x):
    if isinstance(x, tuple):
        return x
    return (x, (x.name,))


class Prog:
    def __init__(self, nc):
        self.nc = nc
        self.ops = {e: [] for e in ENGS}
        self.cnt = {e: 0 for e in ENGS}
        self.dcnt = {e: 0 for e in ENGS}
        self.waited = {e: {} for e in ENGS}
        self.res_w = {}
        self.res_r = {}

    def op(self, eng, fn, reads=(), writes=(), dma=False):
        waits = {}

        def addw(t):
            sk, v = t
            if waits.get(sk, 0) < v:
                waits[sk] = v

        for r in reads:
            if r in self.res_w:
                addw(self.res_w[r])
        for w in writes:
            if w in self.res_w:
                addw(self.res_w[w])
            for sk, v in self.res_r.get(w, {}).items():
                addw((sk, v))
        if dma:
            k = self.dcnt[eng]
            self.dcnt[eng] += 1
            slot = k % KDMA
            semkey = ("dma", eng, slot)
            val = 16 * (k // KDMA + 1)
            inc = 16
            if k >= KDMA:
                addw((semkey, val - 16))
        else:
            self.cnt[eng] += 1
            semkey = eng
            val = self.cnt[eng]
            inc = 1
        wl = []
        for sk, v in waits.items():
            if eng == "pe" and sk == "pe":
                continue
            if self.waited[eng].get(sk, 0) >= v:
                continue
            self.waited[eng][sk] = v
            wl.append((sk, v))
        self.ops[eng].append((wl, fn, semkey, inc))
        tok = (semkey, val)
        for r in reads:
            d = self.res_r.setdefault(r, {})
            if d.get(semkey, 0) < val:
                d[semkey] = val
        for w in writes:
            self.res_w[w] = tok
            self.res_r[w] = {}
        return tok

    def _rw(self, outs, ins):
        wk = []
        rk = []
        for o in outs:
            wk += list(_k(o)[1])
        for i in ins:
            if i is None or isinstance(i, (int, float)):
                continue
            rk += list(_k(i)[1])
        return rk, wk

    @staticmethod
    def _a(x):
        if x is None or isinstance(x, (int, float)):
            return x
        return _k(x)[0]

    def dot(self, junk, a, b, acc):
        rk, wk = self._rw([junk, acc], [a, b])
        j, x, y, c = self._a(junk), self._a(a), self._a(b), self._a(acc)
        self.op("dve", lambda e: e.scalar_tensor_tensor(j, x, 1.0, y, ALU.mult, ALU.mult, accum_out=c), rk, wk)

    def matmul(self, out, lhsT, rhs, start=True, stop=True):
        rk, wk = self._rw([out], [lhsT, rhs])
        o, l, r = self._a(out), self._a(lhsT), self._a(rhs)
        self.op("pe", lambda e: e.matmul(o, l, r, start=start, stop=stop), rk, wk)

    def transpose(self, out, in_, ident):
        rk, wk = self._rw([out], [in_])
        o, i, d = self._a(out), self._a(in_), self._a(ident)
        self.op("pe", lambda e: e.transpose(o, i, d), rk, wk)

    def act(self, out, in_, func, bias=None, scale=None, accum=None, eng="act"):
        outs = [out] + ([accum] if accum is not None else [])
        rk, wk = self._rw(outs, [in_, bias, scale])
        o, i, b, s, a = self._a(out), self._a(in_), self._a(bias), self._a(scale), self._a(accum)
        kw = {}
        if b is not None:
            kw["bias"] = b
        if s is not None:
            kw["scale"] = s
        if a is not None:
            kw["accum_out"] = a
        self.op("act", lambda e: e.activation(o, i, func, **kw), rk, wk)

    def ts(self, out, in0, s1, s2, op0, op1=None, accum=None, eng="dve"):
        outs = [out] + ([accum] if accum is not None else [])
        rk, wk = self._rw(outs, [in0, s1, s2])
        o, i, a1, a2, ac = self._a(out), self._a(in0), self._a(s1), self._a(s2), self._a(accum)
        kw = {}
        if op1 is not None:
            kw["op1"] = op1
        if ac is not None:
            kw["accum_out"] = ac
        self.op(eng, lambda e: e.tensor_scalar(o, i, a1, a2, op0, **kw), rk, wk)

    def tt(self, out, in0, in1, op, eng="dve"):
        rk, wk = self._rw([out], [in0, in1])
        o, i0, i1 = self._a(out), self._a(in0), self._a(in1)
        self.op(eng, lambda e: e.tensor_tensor(o, i0, i1, op), rk, wk)

    def stt(self, out, in0, scalar, in1, op0, op1, eng="dve"):
        rk, wk = self._rw([out], [in0, scalar, in1])
        o, i0, s, i1 = self._a(out), self._a(in0), self._a(scalar), self._a(in1)
        self.op(eng, lambda e: e.scalar_tensor_tensor(o, i0, s, i1, op0, op1), rk, wk)

    def copy(self, out, in_, eng="dve"):
        rk, wk = self._rw([out], [in_])
        o, i = self._a(out), self._a(in_)
        if eng == "act":
            self.op("act", lambda e: e.activation(o, i, AF.Copy), rk, wk)
        else:
            self.op(eng, lambda e: e.tensor_copy(o, i), rk, wk)

    def reduce(self, out, in_, op, eng="dve"):
        rk, wk = self._rw([out], [in_])
        o, i = self._a(out), self._a(in_)
        self.op(eng, lambda e: e.tensor_reduce(o, i, AX.X, op), rk, wk)

    def memset(self, out, val, eng="dve"):
        rk, wk = self._rw([out], [])
        o = self._a(out)
        self.op(eng, lambda e: e.memset(o, val), rk, wk)

    def dma(self, out, in_, eng="sp"):
        rk, wk = self._rw([out], [in_])
        o, i = self._a(out), self._a(in_)
        self.op(eng, lambda e: e.dma_start(out=o, in_=i), rk, wk, dma=True)

    def gather(self, out, table, idx):
        rk, wk = self._rw([out], [idx])
        o, t, i = self._a(out), self._a(table), self._a(idx)
        self.op(
            "pool",
            lambda e: e.indirect_dma_start(
                out=o, out_offset=None, in_=t, in_offset=bass.IndirectOffsetOnAxis(ap=i, axis=0)
            ),
            rk,
            wk,
            dma=True,
        )

    def emit(self, es):
        nc = self.nc
        sems = {}
        for e in ENGS:
            sems[e] = es.enter_context(nc.semaphore("s_" + e))
            for s in range(KDMA):
                sems[("dma", e, s)] = es.enter_context(nc.semaphore("d_%s_%d" % (e, s)))
        block = es.enter_context(nc.Block())

        def run(engobj, name):
            for wl, fn, semkey, inc in self.ops[name]:
                for sk, v in wl:
                    engobj.wait_ge(sems[sk], v)
                fn(engobj).then_inc(sems[semkey], inc)
            k = self.dcnt[name]
            for s in range(min(k, KDMA)):
                n = (k - 1 - s) // KDMA + 1
                engobj.wait_ge(sems[("dma", name, s)], 16 * n)

        @block.tensor
        def _(e):
            run(e, "pe")

        @block.scalar
        def _(e):
            run(e, "act")

        @block.vector
        def _(e):
            run(e, "dve")

        @block.gpsimd
        def _(e):
            run(e, "pool")

        @block.sync
        def _(e):
            run(e, "sp")


def build_program(S, NOWN, dbg=None):
    NKT = S // 128
    nc = bass.Bass("TRN2", target_bir_lowering=False)
    es = ExitStack()
    P = Prog(nc)

    def din(name, shape, dt=F32):
        return nc.dram_tensor(name, list(shape), dt, kind="ExternalInput")

    NXP = 4
    TPP = NKT // NXP
    x_seq = [din("x_seq%d" % i, [S // NXP, D]) for i in range(NXP)]
    x_own = din("x_own", [NOWN * 128, D])
    pos_seq = din("pos_seq", [128, NKT], I32)
    pos_own = din("pos_own", [128, NOWN], I32)
    w_in = din("w_in", [D, INW])
    g1_d = din("g1", [128, 8])
    g2_d = din("g2", [128, 8])
    g2b_d = din("g2b", [128, D])
    kvg_d = din("kvg", [128, 2])
    vng_d = din("vng", [128, 512])
    vnb_d = din("vnb", [128, 512])
    spw_d = din("spw", [4, 128, 128])
    spb_d = din("spb", [128, 4])
    wuk_d = din("wuk", [256, 128])
    wuv_d = din("wuv", [256, 128])
    qg_d = din("qgb", [128, 128])
    kg_d = din("kgb", [128, 128])
    wa_d = din("wa", [512, D])
    wb_d = din("wb", [512, D])
    wo_d = din("wo", [D, D])
    wq_d = din("wq", [D, 2048])
    sk_d = din("subk", [16, 128, 128])
    if not dbg:
        u_tab = din("peer_u", [NEXP, D])
        v_tab = din("peer_v", [NEXP, D])
    ident_d = din("ident", [128, 128], BF16)
    tril_d = din("tril", [128, 128])
    maskb_d = din("maskb", [128, 512])
    inv_d = din("invf", [128, 24])
    iota_d = din("iota16", [128, 16])
    out_d = nc.dram_tensor("out", [NOWN * 128, D], F32, kind="ExternalOutput")
    okind = "ExternalOutput" if dbg else "Internal"
    winbf = nc.dram_tensor("winbf", [D, INW], BF16, kind="Internal")
    wabf = nc.dram_tensor("wabf", [512, D], BF16, kind="Internal")
    wbbf = nc.dram_tensor("wbbf", [512, D], BF16, kind="Internal")
    wobf = nc.dram_tensor("wobf", [D, D], BF16, kind="Internal")
    wqbf = nc.dram_tensor("wqbf", [D, 2048], BF16, kind="Internal")
    kiT_d = nc.dram_tensor("kiT_d", [NKT, 64, 128], BF16, kind=okind)
    kT_d = nc.dram_tensor("kT_d", [NKT, 128, 128], BF16, kind=okind)
    vv_d = nc.dram_tensor("vv_d", [S, 128], BF16, kind=okind)

    if dbg == "H":
        d_sc = nc.dram_tensor("d_sc", [128, 512], F32, kind="ExternalOutput")
        d_st = nc.dram_tensor("d_st", [128, 64], F32, kind="ExternalOutput")
        d_yb = nc.dram_tensor("d_yb", [128, 512], BF16, kind="ExternalOutput")
        d_ma = nc.dram_tensor("d_ma", [128, D], F32, kind="ExternalOutput")
        d_gb = nc.dram_tensor("d_gb", [128, D], F32, kind="ExternalOutput")
        d_q = nc.dram_tensor("d_q", [128, 512], BF16, kind="ExternalOutput")
        d_qi = nc.dram_tensor("d_qi", [128, 512], BF16, kind="ExternalOutput")
        d_mT = nc.dram_tensor("d_mT", [128, 512], BF16, kind="ExternalOutput")
        d_e1 = nc.dram_tensor("d_e1", [128, 512], BF16, kind="ExternalOutput")
        d_e2 = nc.dram_tensor("d_e2", [128, 512], BF16, kind="ExternalOutput")
        d_rz = nc.dram_tensor("d_rz", [128, 512], F32, kind="ExternalOutput")
        d_num = nc.dram_tensor("d_num", [128, 512], F32, kind="ExternalOutput")

    def T(name, shape, dt=F32):
        return es.enter_context(nc.sbuf_tensor(name, list(shape), dt))

    AW = 16384
    arena = T("arena", [128, AW])

    def av(lo, hi, dt=F32):
        keys = tuple(("sc", c) for c in range(lo // 512, (hi - 1) // 512 + 1))
        ap = arena[:, lo:hi]
        if dt is not F32:
            ap = ap.bitcast(dt)
        return (ap, keys)

    ident = T("identb", [128, 128], BF16)
    ones = T("onesb", [128, 128], BF16)
    negpi = T("negpi", [128, 1])
    epsb = T("epsb", [128, 1])
    g1 = T("g1s", [128, 8])
    g2 = T("g2s", [128, 8])
    g2b = T("g2bs", [128, D])
    kvg = T("kvgs", [128, 2])
    vng = T("vngs", [128, 512])
    vnb = T("vnbs", [128, 512])
    spb = T("spbs", [128, 4])
    qgb = T("qgbs", [128, 128])
    kgb = T("kgbs", [128, 128])
    trilm = T("trilm", [128, 128])
    maskb = T("maskbs", [128, 512])
    invf = T("invfs", [128, 24])
    iota16 = T("iota16s", [128, 16])
    posk_i = T("posk_i", [128, NKT], I32)
    posk = T("posk", [128, NKT])
    poso_i = T("poso_i", [128, NOWN], I32)
    poso = T("poso", [128, NOWN])
    wk = T("wk", [128, 8, 320], BF16)
    wkv = T("wkv", [128, 2, 256], BF16)
    skT = T("skT", [128, 16, 128], BF16)
    wsT = T("wsT", [128, 4, 128], BF16)

    ps = [es.enter_context(nc.psum_tensor("ps%d" % i, [128, 512], F32)) for i in range(8)]

    def psb(i):
        return ps[i][:].bitcast(BF16)

    for dst, src in ((ident, ident_d), (g1, g1_d), (g2, g2_d), (g2b, g2b_d), (kvg, kvg_d), (vng, vng_d),
                     (vnb, vnb_d), (spb, spb_d), (qgb, qg_d), (kgb, kg_d), (trilm, tril_d), (maskb, maskb_d),
                     (invf, inv_d), (iota16, iota_d), (posk_i, pos_seq), (poso_i, pos_own)):
        P.dma(dst[:], src.ap())
    P.memset(ones[:], 1.0)
    P.memset(negpi[:], -PI)
    P.memset(epsb[:], EPS)
    P.copy(posk[:], posk_i[:])
    P.copy(poso[:], poso_i[:])

    if dbg == "C0":
        P.dma(out_d[0:128, :], g2b[:])
        P.emit(es)
        es.close()
        return nc
    stg = av(0, INW)
    stgb = av(4608, 4608 + INW // 2, BF16)
    for c in range(8):
        P.dma(stg, w_in[c * 128:(c + 1) * 128, :])
        P.act(stgb, stg, AF.Copy, scale=g1[:, c:c + 1])
        P.dma(winbf[c * 128:(c + 1) * 128, :], stgb)
        P.copy(wk[:, c, 0:256], (stgb[0][:, 1536:1792], stgb[1]))
        P.copy(wk[:, c, 256:320], (stgb[0][:, 2048:2112], stgb[1]))
    s2 = av(8192, 8192 + 2048)
    s2b = av(12288, 12288 + 1024, BF16)
    for c in range(8):
        P.dma(s2, wq_d[c * 128:(c + 1) * 128, :])
        P.act(s2b, s2, AF.Copy, scale=g2[:, c:c + 1])
        P.dma(wqbf[c * 128:(c + 1) * 128, :], s2b)
    s3 = av(10240, 10240 + 1024)
    s3b = av(13312, 13312 + 512, BF16)
    for c in range(4):
        P.dma(s3, wa_d[c * 128:(c + 1) * 128, :])
        P.copy(s3b, s3)
        P.dma(wabf[c * 128:(c + 1) * 128, :], s3b)
        P.dma(s3, wb_d[c * 128:(c + 1) * 128, :])
        P.copy(s3b, s3, eng="act")
        P.dma(wbbf[c * 128:(c + 1) * 128, :], s3b)
    for c in range(8):
        P.dma(s3, wo_d[c * 128:(c + 1) * 128, :])
        P.copy(s3b, s3)
        P.dma(wobf[c * 128:(c + 1) * 128, :], s3b)
    s4 = av(11264, 11264 + 128)
    for c in range(2):
        P.dma(s4, wuk_d[c * 128:(c + 1) * 128, :])
        P.act(wkv[:, c, 0:128], s4, AF.Copy, scale=kvg[:, c:c + 1])
        P.dma(s4, wuv_d[c * 128:(c + 1) * 128, :])
        P.act(wkv[:, c, 128:256], s4, AF.Copy, scale=kvg[:, c:c + 1])
    s5 = av(11776, 11776 + 64, BF16)
    for g in range(16):
        P.dma(s4, sk_d[g])
        P.copy(s5, s4)
        P.transpose(psb(7)[:, 0:128], s5, ident[:])
        P.copy(skT[:, g, :], psb(7)[:, 0:128])
    for g in range(4):
        P.dma(s4, spw_d[g])
        P.tt(s5, s4, trilm[:], ALU.mult)
        P.transpose(psb(7)[:, 0:128], s5, ident[:])
        P.copy(wsT[:, g, :], psb(7)[:, 0:128])

    if dbg == "C1":
        P.dma(out_d[0:128, :], g2b[:])
        P.emit(es)
        es.close()
        return nc
    xt = [T("xt%d" % i, [128, D]) for i in range(2)]
    xsb = T("xsb", [128, D], BF16)
    xsT = T("xsT", [128, 8, 128], BF16)
    st = T("stats", [128, 64])
    ang = T("ang", [128, 24])
    a12 = T("a12", [128, 48])
    a12f = T("a12f", [128, 48])
    a12i = T("a12i", [128, 48], I32)
    sc = T("sincos", [128, 48])
    rtmp = T("rtmp", [128, 4, 64])
    rtmp2 = T("rtmp2", [128, 4, 64])
    junkb = T("junkb", [128, 2048], BF16)
    junkf32 = T("junkf32", [128, 1024])
    junkf32b = T("junkf32b", [128, 256])
    cnb = T("cnb", [128, 256], BF16)
    cnT = T("cnT", [128, 2, 128], BF16)
    kn = T("kn", [128, 128])
    kbf = T("kbf", [128, 128], BF16)
    kif = T("kif", [128, 64])
    kibf = T("kibf", [128, 128], BF16)
    P.memset(kibf[:], 0.0)
    vvb = T("vvb", [128, 128], BF16)
    kTs = T("kTs", [128, 128], BF16)
    kiTs = T("kiTs", [64, 128], BF16)
    pooldummy = T("pooldummy", [128, 8], BF16)

    def sumsq(src, n, acc):
        if n <= 256:
            P.copy(junkf32b[:, 0:n], src)
            src = junkf32b[:, 0:n]
        P.dot(junkf32[:, 0:n], src, src, acc)

    def rstd_from_ss(dst, ss, n):
        P.act(dst, ss, AF.Sqrt, bias=epsb[:, 0:1], scale=1.0 / n)
        rk, wk_ = P._rw([dst], [dst])
        P.op("dve", lambda e, o=_k(dst)[0]: e.reciprocal(o, o), rk, wk_)

    def sincos(pos_col):
        P.ts(a12[:, 0:24], invf[:], pos_col, None, ALU.mult)
        P.ts(a12[:, 24:48], invf[:], pos_col, 0.5 * PI, ALU.mult, ALU.add)
        P.ts(a12f[:], a12[:], 1.0 / (2 * PI), None, ALU.mult)
        P.copy(a12i[:], a12f[:])
        P.copy(a12f[:], a12i[:])
        P.stt(a12[:], a12f[:], -2 * PI, a12[:], ALU.mult, ALU.add)
        P.ts(a12f[:], a12[:], PI, None, ALU.is_gt)
        P.stt(a12[:], a12f[:], -2 * PI, a12[:], ALU.mult, ALU.add)
        P.ts(a12f[:], a12[:], -PI, None, ALU.is_lt)
        P.stt(a12[:], a12f[:], 2 * PI, a12[:], ALU.mult, ALU.add)
        P.act(sc[:], a12[:], AF.Sin)

    def rope(dst, src, nh, dim, half, off, scv=None):
        if scv is None:
            scv = sc[:]
        sca, sck = _k(scv)
        sinb = (sca[:, off:off + half].unsqueeze(1).to_broadcast([128, nh, half]), sck)
        cosb = (sca[:, 24 + off:24 + off + half].unsqueeze(1).to_broadcast([128, nh, half]), sck)
        sa, sk_ = _k(src)
        da, dk_ = _k(dst)
        x1 = (sa[:, :, 0:half], sk_)
        x2 = (sa[:, :, half:2 * half], sk_)
        t1 = rtmp[:, 0:nh, 0:half]
        t2 = rtmp2[:, 0:nh, 0:half]
        P.tt(t1, x1, cosb, ALU.mult)
        P.tt(t2, x2, sinb, ALU.mult)
        P.tt((da[:, :, 0:half], dk_), t1, t2, ALU.subtract)
        t3 = rtmp[:, 0:nh, 16:16 + half]
        t4 = rtmp2[:, 0:nh, 16:16 + half]
        P.tt(t3, x2, cosb, ALU.mult)
        P.tt(t4, x1, sinb, ALU.mult)
        P.tt((da[:, :, half:2 * half], dk_), t3, t4, ALU.add)
        P.copy((da[:, :, 2 * half:dim], dk_), (sa[:, :, 2 * half:dim], sk_), eng="act")

    def front(xtile, bank):
        sumsq(xtile, D, st[:, 0:1])
        rstd_from_ss(st[:, 1:2], st[:, 0:1], D)
        P.act(xsb[:], xtile, AF.Copy, scale=st[:, 1:2])
        for c in range(8):
            P.transpose(psb(bank)[:, c * 128:(c + 1) * 128], xsb[:, c * 128:(c + 1) * 128], ident[:])
        P.copy(xsT[:].rearrange("p c t -> p (c t)"), psb(bank))

    def sincos_batch(dst, pos_all, t0, t1, A, Af, Ai):
        n = t1 - t0
        Aa, Ak = A
        A3 = Aa.rearrange("p (t c) -> p t c", c=48)
        invb = invf[:].unsqueeze(1).to_broadcast([128, n, 24])
        posb = pos_all[:, t0:t1].unsqueeze(2).to_broadcast([128, n, 24])
        P.tt((A3[:, :, 0:24], Ak), invb, posb, ALU.mult)
        P.ts((A3[:, :, 24:48], Ak), (A3[:, :, 0:24], Ak), 0.5 * PI, None, ALU.add)
        P.ts(Af, A, 1.0 / (2 * PI), None, ALU.mult)
        P.copy(Ai, Af)
        P.copy(Af, Ai)
        P.stt(A, Af, -2 * PI, A, ALU.mult, ALU.add)
        P.ts(Af, A, PI, None, ALU.is_gt)
        P.stt(A, Af, -2 * PI, A, ALU.mult, ALU.add)
        P.ts(Af, A, -PI, None, ALU.is_lt)
        P.stt(A, Af, 2 * PI, A, ALU.mult, ALU.add)
        P.act(dst, A, AF.Sin)


    SC5 = [None]

    def ktile(kt):
        xtile = xt[kt % 2]
        P.dma(xtile[:], x_seq[kt // TPP][(kt % TPP) * 128:(kt % TPP + 1) * 128, :])
        front(xtile[:], 7)
        for c in range(8):
            P.matmul(ps[0][:, 0:320], xsT[:, c, :], wk[:, c, :], start=(c == 0), stop=(c == 7))
        if SC5[0] is not None:
            sckt = SC5[0][:, (kt % 4) * 48:(kt % 4 + 1) * 48]
        else:
            sincos(posk[:, kt:kt + 1])
            sckt = None
        for _d in range(NDUMMY):
            P.act(junkb[:], junkb[:], AF.Copy)
        sumsq(ps[0][:, 0:256], 256, st[:, 2:3])
        rstd_from_ss(st[:, 3:4], st[:, 2:3], 256)
        P.act(cnb[:], ps[0][:, 0:256], AF.Copy, scale=st[:, 3:4])
        P.copy(kif[:], ps[0][:, 256:320])
        rope(kibf[:, 0:64].rearrange("p (h d) -> p h d", h=1), kif[:].rearrange("p (h d) -> p h d", h=1), 1, 64, 8, 16, sckt)
        for c in range(2):
            P.transpose(psb(6)[:, c * 128:(c + 1) * 128], cnb[:, c * 128:(c + 1) * 128], ident[:])
        P.copy(cnT[:].rearrange("p c t -> p (c t)"), psb(6)[:, 0:256])
        for c in range(2):
            P.matmul(ps[1][:, 0:256], cnT[:, c, :], wkv[:, c, :], start=(c == 0), stop=(c == 1))
        if dbg == "K3":
            return
        sumsq(ps[1][:, 0:128], 128, st[:, 4:5])
        rstd_from_ss(st[:, 5:6], st[:, 4:5], 128)
        P.stt(kn[:], ps[1][:, 0:128], st[:, 5:6], kgb[:], ALU.mult, ALU.mult)
        rope(kbf[:].rearrange("p (h d) -> p h d", h=1), kn[:].rearrange("p (h d) -> p h d", h=1), 1, 128, 16, 0, sckt)
        P.copy(vvb[:], ps[1][:, 128:256], eng="act")
        P.transpose(psb(5)[:, 0:128], kbf[:], ident[:])
        P.transpose(psb(5)[:, 128:256], kibf[:], ident[:])
        P.copy(kTs[:], psb(5)[:, 0:128], eng="act")
        P.copy(kiTs[:], psb(5)[0:64, 128:256], eng="act")
        if dbg == "K4":
            return
        if dbg != "K6":
            P.dma(vv_d[kt * 128:(kt + 1) * 128, :], vvb[:])
        if dbg == "K5":
            return
        P.dma(kT_d[kt], kTs[:])
        if dbg == "K6":
            return
        P.dma(kiT_d[kt], kiTs[:])


    if dbg and dbg[0] == "K":
        for kt in range(KT0, NKT if KT1 is None else KT1):
            ktile(kt)

    if dbg and dbg[0] == "K":
        P.dma(out_d[0:128, :], xt[0][:])
        P.emit(es)
        es.close()
        return nc

    wbuf = [T("wbuf%d" % i, [128, 2048], BF16) for i in range(3)]
    wctr = [0]

    def wstream(src, a, b):
        buf = wbuf[wctr[0] % 3]
        wctr[0] += 1
        view = buf[:, 0:a * b].rearrange("p (a b) -> p a b", a=a)
        P.dma(view, src.rearrange("(c p) n -> p c n", p=128))
        return view

    ug = T("ug", [128, 512])
    vg = T("vg", [128, 512])
    gt = T("gt", [128, 1024])
    vnbf = T("vnbf", [128, 512], BF16)
    yab = T("yab", [128, 512], BF16)
    yaT = T("yaT", [128, 4, 128], BF16)
    sg = T("sg", [128, 2048])
    merged = T("merged", [128, D])
    mbf = T("mbf", [128, D], BF16)
    mT = T("mT", [128, 8, 128], BF16)
    qn = T("qn", [128, 4, 128])
    qbf = T("qbf", [128, 4, 128], BF16)
    qT = T("qT", [128, 4, 128], BF16)
    qir = T("qir", [128, 4, 64])
    qibf = T("qibf", [128, 4, 128], BF16)
    qiT = T("qiT", [128, 4, 128], BF16)
    P.memset(qibf[:], 0.0)
    kich = [T("kich%d" % i, [64, 512], BF16) for i in range(2)]
    kch = [T("kch%d" % i, [128, 512], BF16) for i in range(2)]
    vch = [T("vch%d" % i, [128, 4, 128], BF16) for i in range(2)]
    rl = [T("rl%d" % i, [128, 512]) for i in range(4)]
    maskc = T("maskc", [128, 512], BF16)
    maskT = T("maskT", [128, 4, 128], BF16)
    eT = [T("eT%d" % i, [128, 512], BF16) for i in range(2)]
    eTm = [T("eTm%d" % i, [128, 512], BF16) for i in range(2)]
    rz = T("rz", [128, 512])
    ybT = T("ybT", [128, 4, 128], BF16)
    hres = T("hres", [128, D])
    sc5 = T("sc5", [128, 240])

    def sincos5(j):
        A = junkf32[:, 0:240]
        Af = junkf32[:, 256:496]
        Ai = junkf32[:, 512:752].bitcast(I32)
        A3 = A.rearrange("p (t c) -> p t c", c=48)
        P.tt(A3[:, 0:4, 0:24], invf[:].unsqueeze(1).to_broadcast([128, 4, 24]),
             posk[:, 4 * j:4 * j + 4].unsqueeze(2).to_broadcast([128, 4, 24]), ALU.mult)
        P.ts(A3[:, 4, 0:24], invf[:], poso[:, j:j + 1], None, ALU.mult)
        P.ts(A3[:, :, 24:48], A3[:, :, 0:24], 0.5 * PI, None, ALU.add)
        P.ts(Af, A, 1.0 / (2 * PI), None, ALU.mult)
        P.copy(Ai, Af)
        P.copy(Af, Ai)
        P.stt(A, Af, -2 * PI, A, ALU.mult, ALU.add)
        P.ts(Af, A, PI, None, ALU.is_gt)
        P.stt(A, Af, -2 * PI, A, ALU.mult, ALU.add)
        P.ts(Af, A, -PI, None, ALU.is_lt)
        P.stt(A, Af, 2 * PI, A, ALU.mult, ALU.add)
        P.act(sc5[:], A, AF.Sin)

    hsb = T("hsb", [128, D], BF16)
    hsT = T("hsT", [128, 8, 128], BF16)
    qpT = T("qpT", [128, 16, 128], BF16)
    vals = T("vals", [128, 16, 16])
    idxu = T("idxu", [128, 16, 16], U32)
    idxf = T("idxf", [128, 16, 16])
    best = T("best", [128, 8, 16])
    posu = T("posu", [128, 8, 16], U32)
    pa_i = T("pa_i", [128, 8, 16], U32)
    pb_i = T("pb_i", [128, 8, 16], U32)
    pa_f = T("pa_f", [128, 8, 16])
    pb_f = T("pb_f", [128, 8, 16])
    sel1 = T("sel1", [128, 8, 16])
    sel2 = T("sel2", [128, 8, 16])
    eidf = T("eidf", [128, 128])
    eid = T("eid", [128, 128], I32)
    gate = T("gate", [128, 8, 16])
    adot = T("adot", [128, 128])
    cw = T("cw", [128, 128])
    gtmp = T("gtmp", [128, 128])

    def gelu(dst, src, tmp, n):
        P.tt(tmp, src, src, ALU.mult)
        P.ts(tmp, tmp, 0.044715, 1.0, ALU.mult, ALU.add)
        P.tt(tmp, tmp, src, ALU.mult)
        P.act(tmp, tmp, AF.Sigmoid, scale=1.5957691216057308)
        P.tt(dst, src, tmp, ALU.mult)

    def top16(vout, iout, src, tmp):
        va, vk = _k(vout)
        ia, ik = _k(iout)
        rk, wk_ = P._rw([vout], [src])
        P.op("dve", lambda e, o=va[:, 0:8], i=_k(src)[0]: e.max(o, i), rk, wk_)
        rk, wk_ = P._rw([iout], [vout, src])
        P.op("dve", lambda e, o=ia[:, 0:8], m=va[:, 0:8], i=_k(src)[0]: e.max_index(o, m, i), rk, wk_)
        rk, wk_ = P._rw([tmp], [vout, src])
        P.op("dve", lambda e, o=_k(tmp)[0], m=va[:, 0:8], i=_k(src)[0]: e.match_replace(o, m, i, -1e30), rk, wk_)
        rk, wk_ = P._rw([vout], [tmp])
        P.op("dve", lambda e, o=va[:, 8:16], i=_k(tmp)[0]: e.max(o, i), rk, wk_)
        rk, wk_ = P._rw([iout], [vout, tmp])
        P.op("dve", lambda e, o=ia[:, 8:16], m=va[:, 8:16], i=_k(tmp)[0]: e.max_index(o, m, i), rk, wk_)

    proj = av(0, INW)
    pj = proj[0]
    pjk = proj[1]

    for j in range(NOWN):
        nch = (4 * j + 4) * 128 // 512
        sincos5(j)
        SC5[0] = sc5
        for kt in range(4 * j, 4 * j + 4):
            ktile(kt)
        xres = xt[j % 2]
        P.dma(xres[:], x_own[j * 128:(j + 1) * 128, :])
        front(xres[:], 7)
        ncc = (INW + 255) // 256
        for cc in range(ncc):
            c0 = cc * 256
            w = min(256, INW - c0)
            wbf = wstream(winbf[:, c0:c0 + w], 8, w)
            bank = cc % 2
            for c in range(8):
                P.matmul(ps[bank][:, 0:w], xsT[:, c, :], wbf[:, c, :], start=(c == 0), stop=(c == 7))
            P.copy((pj[:, c0:c0 + w], pjk), ps[bank][:, 0:w], eng=("act" if cc % 2 else "dve"))
        gelu(ug[:], (pj[:, 0:512], pjk), gt[:, 0:512], 512)
        gelu(vg[:], (pj[:, 512:1024], pjk), gt[:, 512:1024], 512)
        P.reduce(st[:, 8:9], vg[:], ALU.add)
        P.dot(junkf32[:, 0:512], vg[:], vg[:], st[:, 9:10])
        P.ts(st[:, 10:11], st[:, 8:9], 1.0 / 512, None, ALU.mult)
        P.tt(st[:, 11:12], st[:, 10:11], st[:, 10:11], ALU.mult)
        P.stt(st[:, 12:13], st[:, 9:10], 1.0 / 512, st[:, 11:12], ALU.mult, ALU.subtract)
        rstd_from_ss(st[:, 12:13], st[:, 12:13], 1)
        P.ts(vg[:], vg[:], st[:, 10:11], st[:, 12:13], ALU.subtract, ALU.mult)
        P.tt(vg[:], vg[:], vng[:], ALU.mult)
        P.tt(vnbf[:], vg[:], vnb[:], ALU.add)
        for g in range(4):
            P.matmul(ps[2][:, g * 128:(g + 1) * 128], wsT[:, g, :], vnbf[:, g * 128:(g + 1) * 128])
        for g in range(4):
            P.stt(yab[:, g * 128:(g + 1) * 128], ps[2][:, g * 128:(g + 1) * 128], spb[:, g:g + 1],
                  ug[:, g * 128:(g + 1) * 128], ALU.add, ALU.mult)
        for c in range(4):
            P.transpose(psb(3)[:, c * 128:(c + 1) * 128], yab[:, c * 128:(c + 1) * 128], ident[:])
        P.copy(yaT[:].rearrange("p c t -> p (c t)"), psb(3)[:, 0:512])
        for hf in range(2):
            wv = wstream(wabf[:, hf * 512:(hf + 1) * 512], 4, 512)
            for c in range(4):
                P.matmul(ps[4 + hf][:], yaT[:, c, :], wv[:, c, :], start=(c == 0), stop=(c == 3))
        P.act(sg[:], (pj[:, 2116:4164], pjk), AF.Sigmoid)
        for hf in range(2):
            P.tt(merged[:, hf * 512:(hf + 1) * 512], sg[:, hf * 512:(hf + 1) * 512], ps[4 + hf][:], ALU.mult)
        P.tt(junkf32[:, 0:512], (pj[:, 1024:1536], pjk), (pj[:, 1024:1536], pjk), ALU.mult)
        P.reduce(st[:, 16:20], junkf32[:, 0:512].rearrange("p (h d) -> p h d", h=4), ALU.add)
        rstd_from_ss(st[:, 20:24], st[:, 16:20], 128)
        for h in range(4):
            P.stt(qn[:, h, :], (pj[:, 1024 + h * 128:1024 + (h + 1) * 128], pjk), st[:, 20 + h:21 + h], qgb[:], ALU.mult, ALU.mult)
        rope(qbf[:], qn[:], 4, 128, 16, 0, sc5[:, 192:240])
        for h in range(4):
            P.transpose(psb(6)[:, h * 128:(h + 1) * 128], qbf[:, h, :], ident[:])
        P.copy(qT[:].rearrange("p h t -> p (h t)"), psb(6)[:, 0:512])
        wid = (pj[:, 2112:2116], pjk)
        P.ts(st[:, 28:32], wid, 0.0, 2.0, ALU.is_gt, ALU.mult)
        P.ts(st[:, 28:32], st[:, 28:32], -1.0, None, ALU.add)
        P.stt(st[:, 24:28], wid, 0.0625, st[:, 28:32], ALU.mult, ALU.mult)
        rope(qir[:], (pj[:, 1792:2048].rearrange("p (h d) -> p h d", h=4), pjk), 4, 64, 8, 16, sc5[:, 192:240])
        P.tt(qibf[:, :, 0:64], qir[:], st[:, 24:28].unsqueeze(2).to_broadcast([128, 4, 64]), ALU.mult)
        for h in range(4):
            P.transpose(psb(7)[:, h * 128:(h + 1) * 128], qibf[:, h, :], ident[:])
        P.copy(qiT[:].rearrange("p h t -> p (h t)"), psb(7)[:, 0:512])
        for c in range(nch):
            kb = kich[c % 2]
            P.dma(kb[:].rearrange("p (k t) -> p k t", k=4), kiT_d[c * 4:(c + 1) * 4].rearrange("k p t -> p k t"))
            scv = av(c * 512, (c + 1) * 512)
            for h in range(4):
                P.matmul(ps[h][:], qiT[0:64, h, :], kb[:])
            for h in range(4):
                P.act(rl[h][:], ps[h][:], AF.Relu)
            P.ts(scv, rl[0][:], st[:, 28:29], None, ALU.mult)
            for h in range(1, 4):
                P.stt(scv, rl[h][:], st[:, 28 + h:29 + h], scv, ALU.mult, ALU.add)
            if c == nch - 1:
                P.tt(scv, scv, maskb[:], ALU.add)
        n = nch * 512
        lo, hi, mid, cnt, ge, dd = (st[:, 32:33], st[:, 33:34], st[:, 34:35], st[:, 35:36], st[:, 36:37], st[:, 37:38])
        cnts = st[:, 40:48]
        P.memset(mid, 0.0)
        pieces = [(p0, min(p0 + 2048, n)) for p0 in range(0, n, 2048)]
        hstep = 8.0
        ktop = min(TOPK, S // 4) - 0.5
        for it in range(BIS_ITERS):
            for pi_, (p0, p1) in enumerate(pieces):
                P.ts(junkb[:, 0:p1 - p0], av(p0, p1), mid, None, ALU.is_ge, ALU.add, accum=cnts[:, pi_:pi_ + 1])
            if len(pieces) > 1:
                P.reduce(cnt, cnts[:, 0:len(pieces)], ALU.add)
                cc_ = cnt
            else:
                cc_ = cnts[:, 0:1]
            P.ts(dd, cc_, ktop, hstep, ALU.is_ge, ALU.mult)
            P.stt(mid, dd, -0.5 * hstep, mid, ALU.add, ALU.add)
            hstep *= 0.5
        P.ts(lo, mid, -hstep, None, ALU.add)
        nkb = nch * 4
        for c in range(nch):
            kc = kch[c % 2]
            vc = vch[c % 2]
            P.dma(kc[:].rearrange("p (k t) -> p k t", k=4), kT_d[c * 4:(c + 1) * 4].rearrange("k p t -> p k t"))
            P.dma(vc[:], vv_d[c * 512:(c + 1) * 512, :].rearrange("(kb p) d -> p kb d", p=128))
            P.ts(maskc[:], av(c * 512, (c + 1) * 512), lo, None, ALU.is_ge)
            for b in range(4):
                P.transpose(psb(0)[:, b * 128:(b + 1) * 128], maskc[:, b * 128:(b + 1) * 128], ident[:])
            P.copy(maskT[:].rearrange("p b q -> p (b q)"), psb(0)[:, 0:512])
            for b in range(4):
                gkb = c * 4 + b
                bank = 1 + (gkb % 2)
                P.matmul(ps[bank][:], kc[:, b * 128:(b + 1) * 128], qT[:].rearrange("p h t -> p (h t)"))
                e1 = eT[gkb % 2]
                e2 = eTm[gkb % 2]
                P.act(e1[:], ps[bank][:], AF.Exp, scale=float(128 ** -0.5))
                P.tt(e2[:].rearrange("p (h q) -> p h q", h=4), e1[:].rearrange("p (h q) -> p h q", h=4),
                     maskT[:, b, :].unsqueeze(1).to_broadcast([128, 4, 128]), ALU.mult)
                P.matmul(ps[4][:], vc[:, b, :], e2[:], start=(gkb == 0), stop=(gkb == nkb - 1))
                P.matmul(ps[5][:], ones[:], e2[:], start=(gkb == 0), stop=(gkb == nkb - 1))
        if dbg == "H" and j == NOWN - 1:
            P.dma(d_mT.ap(), maskT[:].rearrange("p b q -> p (b q)"))
            P.dma(d_e1.ap(), eT[(nkb - 1) % 2][:])
            P.dma(d_e2.ap(), eTm[(nkb - 1) % 2][:])
            P.copy(gt[:, 0:512], ps[4][:])
            P.dma(d_num.ap(), gt[:, 0:512])
        P.op("dve", lambda e: e.reciprocal(rz[:], ps[5][:]), ["ps5"], ["rz"])
        if dbg == "H" and j == NOWN - 1:
            P.dma(d_rz.ap(), rz[:])
        P.copy(gt[:, 512:1024], ps[4][:], eng="act")
        P.tt(ybT[:].rearrange("p h t -> p (h t)"), gt[:, 512:1024], rz[:], ALU.mult)
        for hf in range(2):
            wv = wstream(wbbf[:, hf * 512:(hf + 1) * 512], 4, 512)
            for h in range(4):
                P.matmul(ps[6 + hf][:], ybT[:, h, :], wv[:, h, :], start=(h == 0), stop=(h == 3))
        for hf in range(2):
            P.tt(gt[:, hf * 512:(hf + 1) * 512], sg[:, 1024 + hf * 512:1024 + (hf + 1) * 512], ps[6 + hf][:], ALU.mult)
        if dbg == "H" and j == NOWN - 1:
            P.dma(d_yb.ap(), ybT[:].rearrange("p h t -> p (h t)"))
            P.dma(d_gb.ap(), gt[:])
        P.tt(mbf[:], merged[:], gt[:], ALU.add)
        for c in range(8):
            P.transpose(psb(0)[:, c * 128:(c + 1) * 128], mbf[:, c * 128:(c + 1) * 128], ident[:])
        P.copy(mT[:].rearrange("p c t -> p (c t)"), psb(0))
        for qd in range(4):
            wv = wstream(wobf[:, qd * 256:(qd + 1) * 256], 8, 256)
            for c in range(8):
                P.matmul(ps[1 + qd // 2][:, (qd % 2) * 256:(qd % 2 + 1) * 256], mT[:, c, :], wv[:, c, :],
                         start=(c == 0), stop=(c == 7))
        for hf in range(2):
            P.tt(hres[:, hf * 512:(hf + 1) * 512], xres[:, hf * 512:(hf + 1) * 512], ps[1 + hf][:], ALU.add)
        if dbg == "H":
            P.dma(out_d[j * 128:(j + 1) * 128, :], hres[:])
            continue
        hn = av(0, 1024)
        sub = av(1024, 3072)
        sub2 = av(3072, 5120)
        cand = av(5120, 7168)
        oh = av(7168, 9216)
        junkf = av(9216, 10240)
        gbuf = [av(10240 + r * 1024, 10240 + (r + 1) * 1024) for r in range(6)]
        sumsq(hres[:], D, st[:, 48:49])
        rstd_from_ss(st[:, 49:50], st[:, 48:49], D)
        P.act(hsb[:], hres[:], AF.Copy, scale=st[:, 49:50])
        P.stt(hn, hres[:], st[:, 49:50], g2b[:], ALU.mult, ALU.mult)
        for c in range(8):
            P.transpose(psb(2)[:, c * 128:(c + 1) * 128], hsb[:, c * 128:(c + 1) * 128], ident[:])
        P.copy(hsT[:].rearrange("p c t -> p (c t)"), psb(2))
        for gp in range(8):
            wv = wstream(wqbf[:, gp * 256:(gp + 1) * 256], 8, 256)
            for g2_ in range(2):
                g = gp * 2 + g2_
                bank = 3 + g // 4
                for c in range(8):
                    P.matmul(ps[bank][:, (g % 4) * 128:(g % 4 + 1) * 128], wv[:, c, g2_ * 128:(g2_ + 1) * 128], hsT[:, c, :],
                             start=(c == 0), stop=(c == 7))
        for b in range(4):
            P.copy(qpT[:, b * 4:(b + 1) * 4, :].rearrange("p g t -> p (g t)"), ps[3 + b][:], eng=("act" if b % 2 else "dve"))
        sbanks = [7, 0, 1, 2]
        for g in range(16):
            P.matmul(ps[sbanks[g // 4]][:, (g % 4) * 128:(g % 4 + 1) * 128], qpT[:, g, :], skT[:, g, :])
        for b in range(4):
            P.copy((sub[0][:, b * 512:(b + 1) * 512], sub[1]), ps[sbanks[b]][:], eng=("act" if b % 2 else "dve"))
        for g in range(16):
            top16(vals[:, g, :], idxu[:, g, :], (sub[0][:, g * 128:(g + 1) * 128], sub[1]),
                  (sub2[0][:, g * 128:(g + 1) * 128], sub2[1]))
        v4 = vals[:].rearrange("p (h t) k -> p h t k", t=2)
        c4 = (cand[0].rearrange("p (h a b) -> p h a b", h=8, a=16), cand[1])
        P.tt(c4, v4[:, :, 0, :].unsqueeze(3).to_broadcast([128, 8, 16, 16]),
             v4[:, :, 1, :].unsqueeze(2).to_broadcast([128, 8, 16, 16]), ALU.add)
        for h in range(8):
            top16(best[:, h, :], posu[:, h, :], (cand[0][:, h * 256:(h + 1) * 256], cand[1]),
                  (sub2[0][:, h * 256:(h + 1) * 256], sub2[1]))
        P.op("dve", lambda e: e.tensor_single_scalar(pa_i[:], posu[:], 4, ALU.logical_shift_right), ["posu"], ["pa_i"])
        P.op("dve", lambda e: e.tensor_single_scalar(pb_i[:], posu[:], 15, ALU.bitwise_and), ["posu"], ["pb_i"])
        P.copy(pa_f[:], pa_i[:])
        P.copy(pb_f[:], pb_i[:])
        P.copy(idxf[:], idxu[:])
        i4 = idxf[:].rearrange("p (h t) k -> p h t k", t=2)
        o4 = (oh[0].rearrange("p (h k a) -> p h k a", h=8, k=16), oh[1])
        iob = iota16[:].unsqueeze(1).unsqueeze(1).to_broadcast([128, 8, 16, 16])
        for (pf, t_, so) in ((pa_f, 0, sel1), (pb_f, 1, sel2)):
            P.tt(o4, iob, pf[:].unsqueeze(3).to_broadcast([128, 8, 16, 16]), ALU.is_equal)
            P.tt(o4, o4, i4[:, :, t_, :].unsqueeze(2).to_broadcast([128, 8, 16, 16]), ALU.mult)
            P.reduce(so[:], o4, ALU.add)
        P.stt(eidf[:].rearrange("p (h k) -> p h k", h=8), sel1[:], 128.0, sel2[:], ALU.mult, ALU.add)
        P.copy(eid[:], eidf[:])
        P.reduce(st[:, 50:58], best[:], ALU.max)
        P.tt(gate[:], best[:], st[:, 50:58].unsqueeze(2).to_broadcast([128, 8, 16]), ALU.subtract)
        P.act(gate[:], gate[:], AF.Exp)
        P.reduce(rz[:, 0:8], gate[:], ALU.add)
        P.op("dve", lambda e: e.reciprocal(rz[:, 8:16], rz[:, 0:8]), ["rz"], ["rz"])
        P.tt(gate[:], gate[:], rz[:, 8:16].unsqueeze(2).to_broadcast([128, 8, 16]), ALU.mult)
        for s_ in range(128):
            gb = gbuf[s_ % 6]
            P.gather(gb, u_tab.ap(), eid[:, s_:s_ + 1])
            P.dot(junkf, gb, hn, adot[:, s_:s_ + 1])
        gelu(cw[:], adot[:], gtmp[:], 128)
        P.tt(cw[:], cw[:], gate[:].rearrange("p h k -> p (h k)"), ALU.mult)
        for s_ in range(128):
            gb = gbuf[s_ % 6]
            P.gather(gb, v_tab.ap(), eid[:, s_:s_ + 1])
            P.stt(hres[:], gb, cw[:, s_:s_ + 1], hres[:], ALU.mult, ALU.add)
        P.dma(out_d[j * 128:(j + 1) * 128, :], hres[:])

    P.emit(es)
    es.close()
    return nc


def _host_inputs(inp, S, NOWN):
    f32 = np.float32
    B = inp["x"].shape[0]
    x = np.asarray(inp["x"], f32)
    pos = np.asarray(inp["positions"], np.int32)
    NKT = S // 128
    rep = lambda v, n=128: np.ascontiguousarray(np.broadcast_to(np.asarray(v, f32).reshape(1, -1), (n, np.asarray(v).size)))
    pc = lambda v, c: np.ascontiguousarray(np.asarray(v, f32).reshape(c, 128).T)
    half_q = np.arange(16, dtype=f32)
    half_i = np.arange(8, dtype=f32)
    inv_q = np.power(f32(500000.0), -half_q * f32(2.0) / f32(32)).astype(f32)
    inv_i = np.power(f32(500000.0), -half_i * f32(2.0) / f32(16)).astype(f32)
    invf = rep(np.concatenate([inv_q, inv_i]))
    common = {
        "w_in": np.ascontiguousarray(inp["w_in"][0], f32),
        "g1": pc(inp["norm1_g"][0], 8),
        "g2": pc(inp["norm2_g"][0], 8),
        "g2b": rep(inp["norm2_g"][0]),
        "kvg": pc(inp["kv_norm_g"][0], 2),
        "vng": rep(inp["v_norm_g"][0]),
        "vnb": rep(inp["v_norm_b"][0]),
        "spw": np.ascontiguousarray(inp["spatial_w"][0], f32),
        "spb": np.ascontiguousarray(np.asarray(inp["spatial_b"][0], f32).T),
        "wuk": np.ascontiguousarray(inp["w_uk"][0], f32),
        "wuv": np.ascontiguousarray(inp["w_uv"][0], f32),
        "qgb": rep(inp["q_norm_g"][0]),
        "kgb": rep(inp["k_norm_g"][0]),
        "wa": np.ascontiguousarray(inp["w_a_out"][0], f32),
        "wb": np.ascontiguousarray(inp["w_b_out"][0], f32),
        "wo": np.ascontiguousarray(inp["w_o"][0], f32),
        "wq": np.ascontiguousarray(inp["peer_wq"][0], f32),
        "subk": np.ascontiguousarray(np.asarray(inp["peer_subkeys"][0], f32).reshape(16, 128, 128)),
        "peer_u": np.ascontiguousarray(inp["peer_u"][0], f32),
        "peer_v": np.ascontiguousarray(inp["peer_v"][0], f32),
        "ident": np.eye(128, dtype=f32).astype(ml_dtypes.bfloat16),
        "tril": np.tril(np.ones((128, 128), f32)),
        "invf": invf,
        "iota16": rep(np.arange(16, dtype=f32)),
    }
    maps = []
    lanes = 8 // B
    for core in range(8):
        b, c = core // lanes, core % lanes
        tiles = [lanes * j + c for j in range(NOWN)]
        rows = np.concatenate([np.arange(t * 128, (t + 1) * 128) for t in tiles])
        mb = np.zeros((128, 512), f32)
        for r in range(4):
            blk = mb[:, r * 128:(r + 1) * 128]
            if r > c:
                blk[:] = -1e30
            elif r == c:
                blk[:] = np.where(np.arange(128)[None, :] <= np.arange(128)[:, None], 0.0, -1e30)
        m = dict(common)
        for i in range(4):
            m["x_seq%d" % i] = np.ascontiguousarray(x[b][i * (S // 4):(i + 1) * (S // 4)])
        m["x_own"] = np.ascontiguousarray(x[b][rows])
        m["pos_seq"] = np.ascontiguousarray(pos[b].reshape(NKT, 128).T)
        m["pos_own"] = np.ascontiguousarray(pos[b][rows].reshape(NOWN, 128).T)
        m["maskb"] = mb
        maps.append((m, b, rows))
    return maps


_NC_CACHE = {}


def kernel(**inputs):
    x = np.asarray(inputs["x"])
    B, S, _ = x.shape
    lanes = 8 // B
    NOWN = S // 128 // lanes
    key = (S, NOWN)
    if key not in _NC_CACHE:
        _NC_CACHE[key] = build_program(S, NOWN)
    nc = _NC_CACHE[key]
    maps = _host_inputs(inputs, S, NOWN)
    res = run_bass_kernel_spmd(nc, [m for m, _, _ in maps], core_ids=list(range(8)))
    out = np.empty((B, S, D), np.float32)
    for (m, b, rows), r in zip(maps, res.results):
        out[b, rows] = r["out"]
    return out
```

```python
import numpy as np
import ml_dtypes
from contextlib import ExitStack
import concourse.bass as bass
import concourse.mybir as mybir
from concourse.bass_utils import run_bass_kernel_spmd

F32 = mybir.dt.float32
BF16 = mybir.dt.bfloat16
I32 = mybir.dt.int32
U32 = mybir.dt.uint32
ALU = mybir.AluOpType
AF = mybir.ActivationFunctionType
AX = mybir.AxisListType

D = 1024
INW = 4164
EPS = 1e-6
TOPK = 256
NEXP = 16384
PI = float(np.pi)
ENGS = ("pe", "act", "dve", "pool", "sp")
KDMA = 8
BIS_ITERS = 23
KT0 = 0
NDUMMY = 0
KT1 = None


def _k(x):
    if isinstance(x, tuple):
        return x
    return (x, (x.name,))


class Prog:
    def __init__(self, nc):
        self.nc = nc
        self.ops = {e: [] for e in ENGS}
        self.cnt = {e: 0 for e in ENGS}
        self.dcnt = {e: 0 for e in ENGS}
        self.waited = {e: {} for e in ENGS}
        self.res_w = {}
        self.res_r = {}

    def op(self, eng, fn, reads=(), writes=(), dma=False):
        waits = {}

        def addw(t):
            sk, v = t
            if waits.get(sk, 0) < v:
                waits[sk] = v

        for r in reads:
            if r in self.res_w:
                addw(self.res_w[r])
        for w in writes:
            if w in self.res_w:
                addw(self.res_w[w])
            for sk, v in self.res_r.get(w, {}).items():
                addw((sk, v))
        if dma:
            k = self.dcnt[eng]
            self.dcnt[eng] += 1
            slot = k % KDMA
            semkey = ("dma", eng, slot)
            val = 16 * (k // KDMA + 1)
            inc = 16
            if k >= KDMA:
                addw((semkey, val - 16))
        else:
            self.cnt[eng] += 1
            semkey = eng
            val = self.cnt[eng]
            inc = 1
        wl = []
        for sk, v in waits.items():
            if eng == "pe" and sk == "pe":
                continue
            if self.waited[eng].get(sk, 0) >= v:
                continue
            self.waited[eng][sk] = v
            wl.append((sk, v))
        self.ops[eng].append((wl, fn, semkey, inc))
        tok = (semkey, val)
        for r in reads:
            d = self.res_r.setdefault(r, {})
            if d.get(semkey, 0) < val:
                d[semkey] = val
        for w in writes:
            self.res_w[w] = tok
            self.res_r[w] = {}
        return tok

    def _rw(self, outs, ins):
        wk = []
        rk = []
        for o in outs:
            wk += list(_k(o)[1])
        for i in ins:
            if i is None or isinstance(i, (int, float)):
                continue
            rk += list(_k(i)[1])
        return rk, wk

    @staticmethod
    def _a(x):
        if x is None or isinstance(x, (int, float)):
            return x
        return _k(x)[0]

    def dot(self, junk, a, b, acc):
        rk, wk = self._rw([junk, acc], [a, b])
        j, x, y, c = self._a(junk), self._a(a), self._a(b), self._a(acc)
        self.op("dve", lambda e: e.scalar_tensor_tensor(j, x, 1.0, y, ALU.mult, ALU.mult, accum_out=c), rk, wk)

    def matmul(self, out, lhsT, rhs, start=True, stop=True):
        rk, wk = self._rw([out], [lhsT, rhs])
        o, l, r = self._a(out), self._a(lhsT), self._a(rhs)
        self.op("pe", lambda e: e.matmul(o, l, r, start=start, stop=stop), rk, wk)

    def transpose(self, out, in_, ident):
        rk, wk = self._rw([out], [in_])
        o, i, d = self._a(out), self._a(in_), self._a(ident)
        self.op("pe", lambda e: e.transpose(o, i, d), rk, wk)

    def act(self, out, in_, func, bias=None, scale=None, accum=None, eng="act"):
        outs = [out] + ([accum] if accum is not None else [])
        rk, wk = self._rw(outs, [in_, bias, scale])
        o, i, b, s, a = self._a(out), self._a(in_), self._a(bias), self._a(scale), self._a(accum)
        kw = {}
        if b is not None:
            kw["bias"] = b
        if s is not None:
            kw["scale"] = s
        if a is not None:
            kw["accum_out"] = a
        self.op("act", lambda e: e.activation(o, i, func, **kw), rk, wk)

    def ts(self, out, in0, s1, s2, op0, op1=None, accum=None, eng="dve"):
        outs = [out] + ([accum] if accum is not None else [])
        rk, wk = self._rw(outs, [in0, s1, s2])
        o, i, a1, a2, ac = self._a(out), self._a(in0), self._a(s1), self._a(s2), self._a(accum)
        kw = {}
        if op1 is not None:
            kw["op1"] = op1
        if ac is not None:
            kw["accum_out"] = ac
        self.op(eng, lambda e: e.tensor_scalar(o, i, a1, a2, op0, **kw), rk, wk)

    def tt(self, out, in0, in1, op, eng="dve"):
        rk, wk = self._rw([out], [in0, in1])
        o, i0, i1 = self._a(out), self._a(in0), self._a(in1)
        self.op(eng, lambda e: e.tensor_tensor(o, i0, i1, op), rk, wk)

    def stt(self, out, in0, scalar, in1, op0, op1, eng="dve"):
        rk, wk = self._rw([out], [in0, scalar, in1])
        o, i0, s, i1 = self._a(out), self._a(in0), self._a(scalar), self._a(in1)
        self.op(eng, lambda e: e.scalar_tensor_tensor(o, i0, s, i1, op0, op1), rk, wk)

    def copy(self, out, in_, eng="dve"):
        rk, wk = self._rw([out], [in_])
        o, i = self._a(out), self._a(in_)
        if eng == "act":
            self.op("act", lambda e: e.activation(o, i, AF.Copy), rk, wk)
        else:
            self.op(eng, lambda e: e.tensor_copy(o, i), rk, wk)

    def reduce(self, out, in_, op, eng="dve"):
        rk, wk = self._rw([out], [in_])
        o, i = self._a(out), self._a(in_)
        self.op(eng, lambda e: e.tensor_reduce(o, i, AX.X, op), rk, wk)

    def memset(self, out, val, eng="dve"):
        rk, wk = self._rw([out], [])
        o = self._a(out)
        self.op(eng, lambda e: e.memset(o, val), rk, wk)

    def dma(self, out, in_, eng="sp"):
        rk, wk = self._rw([out], [in_])
        o, i = self._a(out), self._a(in_)
        self.op(eng, lambda e: e.dma_start(out=o, in_=i), rk, wk, dma=True)

    def gather(self, out, table, idx):
        rk, wk = self._rw([out], [idx])
        o, t, i = self._a(out), self._a(table), self._a(idx)
        self.op(
            "pool",
            lambda e: e.indirect_dma_start(
                out=o, out_offset=None, in_=t, in_offset=bass.IndirectOffsetOnAxis(ap=i, axis=0)
            ),
            rk,
            wk,
            dma=True,
        )

    def emit(self, es):
        nc = self.nc
        sems = {}
        for e in ENGS:
            sems[e] = es.enter_context(nc.semaphore("s_" + e))
            for s in range(KDMA):
                sems[("dma", e, s)] = es.enter_context(nc.semaphore("d_%s_%d" % (e, s)))
        block = es.enter_context(nc.Block())

        def run(engobj, name):
            for wl, fn, semkey, inc in self.ops[name]:
                for sk, v in wl:
                    engobj.wait_ge(sems[sk], v)
                fn(engobj).then_inc(sems[semkey], inc)
            k = self.dcnt[name]
            for s in range(min(k, KDMA)):
                n = (k - 1 - s) // KDMA + 1
                engobj.wait_ge(sems[("dma", name, s)], 16 * n)

        @block.tensor
        def _(e):
            run(e, "pe")

        @block.scalar
        def _(e):
            run(e, "act")

        @block.vector
        def _(e):
            run(e, "dve")

        @block.gpsimd
        def _(e):
            run(e, "pool")

        @block.sync
        def _(e):
            run(e, "sp")


def build_program(S, NOWN, dbg=None):
    NKT = S // 128
    nc = bass.Bass("TRN2", target_bir_lowering=False)
    es = ExitStack()
    P = Prog(nc)

    def din(name, shape, dt=F32):
        return nc.dram_tensor(name, list(shape), dt, kind="ExternalInput")

    NXP = 4
    TPP = NKT // NXP
    x_seq = [din("x_seq%d" % i, [S // NXP, D]) for i in range(NXP)]
    x_own = din("x_own", [NOWN * 128, D])
    pos_seq = din("pos_seq", [128, NKT], I32)
    pos_own = din("pos_own", [128, NOWN], I32)
    w_in = din("w_in", [D, INW])
    g1_d = din("g1", [128, 8])
    g2_d = din("g2", [128, 8])
    g2b_d = din("g2b", [128, D])
    kvg_d = din("kvg", [128, 2])
    vng_d = din("vng", [128, 512])
    vnb_d = din("vnb", [128, 512])
    spw_d = din("spw", [4, 128, 128])
    spb_d = din("spb", [128, 4])
    wuk_d = din("wuk", [256, 128])
    wuv_d = din("wuv", [256, 128])
    qg_d = din("qgb", [128, 128])
    kg_d = din("kgb", [128, 128])
    wa_d = din("wa", [512, D])
    wb_d = din("wb", [512, D])
    wo_d = din("wo", [D, D])
    wq_d = din("wq", [D, 2048])
    sk_d = din("subk", [16, 128, 128])
    if not dbg:
        u_tab = din("peer_u", [NEXP, D])
        v_tab = din("peer_v", [NEXP, D])
    ident_d = din("ident", [128, 128], BF16)
    tril_d = din("tril", [128, 128])
    maskb_d = din("maskb", [128, 512])
    inv_d = din("invf", [128, 24])
    iota_d = din("iota16", [128, 16])
    out_d = nc.dram_tensor("out", [NOWN * 128, D], F32, kind="ExternalOutput")
    okind = "ExternalOutput" if dbg else "Internal"
    winbf = nc.dram_tensor("winbf", [D, INW], BF16, kind="Internal")
    wabf = nc.dram_tensor("wabf", [512, D], BF16, kind="Internal")
    wbbf = nc.dram_tensor("wbbf", [512, D], BF16, kind="Internal")
    wobf = nc.dram_tensor("wobf", [D, D], BF16, kind="Internal")
    wqbf = nc.dram_tensor("wqbf", [D, 2048], BF16, kind="Internal")
    kiT_d = nc.dram_tensor("kiT_d", [NKT, 64, 128], BF16, kind=okind)
    kT_d = nc.dram_tensor("kT_d", [NKT, 128, 128], BF16, kind=okind)
    vv_d = nc.dram_tensor("vv_d", [S, 128], BF16, kind=okind)

    if dbg == "H":
        d_sc = nc.dram_tensor("d_sc", [128, 512], F32, kind="ExternalOutput")
        d_st = nc.dram_tensor("d_st", [128, 64], F32, kind="ExternalOutput")
        d_yb = nc.dram_tensor("d_yb", [128, 512], BF16, kind="ExternalOutput")
        d_ma = nc.dram_tensor("d_ma", [128, D], F32, kind="ExternalOutput")
        d_gb = nc.dram_tensor("d_gb", [128, D], F32, kind="ExternalOutput")
        d_q = nc.dram_tensor("d_q", [128, 512], BF16, kind="ExternalOutput")
        d_qi = nc.dram_tensor("d_qi", [128, 512], BF16, kind="ExternalOutput")
        d_mT = nc.dram_tensor("d_mT", [128, 512], BF16, kind="ExternalOutput")
        d_e1 = nc.dram_tensor("d_e1", [128, 512], BF16, kind="ExternalOutput")
        d_e2 = nc.dram_tensor("d_e2", [128, 512], BF16, kind="ExternalOutput")
        d_rz = nc.dram_tensor("d_rz", [128, 512], F32, kind="ExternalOutput")
        d_num = nc.dram_tensor("d_num", [128, 512], F32, kind="ExternalOutput")

    def T(name, shape, dt=F32):
        return es.enter_context(nc.sbuf_tensor(name, list(shape), dt))

    AW = 16384
    arena = T("arena", [128, AW])

    def av(lo, hi, dt=F32):
        keys = tuple(("sc", c) for c in range(lo // 512, (hi - 1) // 512 + 1))
        ap = arena[:, lo:hi]
        if dt is not F32:
            ap = ap.bitcast(dt)
        return (ap, keys)

    ident = T("identb", [128, 128], BF16)
    ones = T("onesb", [128, 128], BF16)
    negpi = T("negpi", [128, 1])
    epsb = T("epsb", [128, 1])
    g1 = T("g1s", [128, 8])
    g2 = T("g2s", [128, 8])
    g2b = T("g2bs", [128, D])
    kvg = T("kvgs", [128, 2])
    vng = T("vngs", [128, 512])
    vnb = T("vnbs", [128, 512])
    spb = T("spbs", [128, 4])
    qgb = T("qgbs", [128, 128])
    kgb = T("kgbs", [128, 128])
    trilm = T("trilm", [128, 128])
    maskb = T("maskbs", [128, 512])
    invf = T("invfs", [128, 24])
    iota16 = T("iota16s", [128, 16])
    posk_i = T("posk_i", [128, NKT], I32)
    posk = T("posk", [128, NKT])
    poso_i = T("poso_i", [128, NOWN], I32)
    poso = T("poso", [128, NOWN])
    wk = T("wk", [128, 8, 320], BF16)
    wkv = T("wkv", [128, 2, 256], BF16)
    skT = T("skT", [128, 16, 128], BF16)
    wsT = T("wsT", [128, 4, 128], BF16)

    ps = [es.enter_context(nc.psum_tensor("ps%d" % i, [128, 512], F32)) for i in range(8)]

    def psb(i):
        return ps[i][:].bitcast(BF16)

    for dst, src in ((ident, ident_d), (g1, g1_d), (g2, g2_d), (g2b, g2b_d), (kvg, kvg_d), (vng, vng_d),
                     (vnb, vnb_d), (spb, spb_d), (qgb, qg_d), (kgb, kg_d), (trilm, tril_d), (maskb, maskb_d),
                     (invf, inv_d), (iota16, iota_d), (posk_i, pos_seq), (poso_i, pos_own)):
        P.dma(dst[:], src.ap())
    P.memset(ones[:], 1.0)
    P.memset(negpi[:], -PI)
    P.memset(epsb[:], EPS)
    P.copy(posk[:], posk_i[:])
    P.copy(poso[:], poso_i[:])

    if dbg == "C0":
        P.dma(out_d[0:128, :], g2b[:])
        P.emit(es)
        es.close()
        return nc
    stg = av(0, INW)
    stgb = av(4608, 4608 + INW // 2, BF16)
    for c in range(8):
        P.dma(stg, w_in[c * 128:(c + 1) * 128, :])
        P.act(stgb, stg, AF.Copy, scale=g1[:, c:c + 1])
        P.dma(winbf[c * 128:(c + 1) * 128, :], stgb)
        P.copy(wk[:, c, 0:256], (stgb[0][:, 1536:1792], stgb[1]))
        P.copy(wk[:, c, 256:320], (stgb[0][:, 2048:2112], stgb[1]))
    s2 = av(8192, 8192 + 2048)
    s2b = av(12288, 12288 + 1024, BF16)
    for c in range(8):
        P.dma(s2, wq_d[c * 128:(c + 1) * 128, :])
        P.act(s2b, s2, AF.Copy, scale=g2[:, c:c + 1])
        P.dma(wqbf[c * 128:(c + 1) * 128, :], s2b)
    s3 = av(10240, 10240 + 1024)
    s3b = av(13312, 13312 + 512, BF16)
    for c in range(4):
        P.dma(s3, wa_d[c * 128:(c + 1) * 128, :])
        P.copy(s3b, s3)
        P.dma(wabf[c * 128:(c + 1) * 128, :], s3b)
        P.dma(s3, wb_d[c * 128:(c + 1) * 128, :])
        P.copy(s3b, s3, eng="act")
        P.dma(wbbf[c * 128:(c + 1) * 128, :], s3b)
    for c in range(8):
        P.dma(s3, wo_d[c * 128:(c + 1) * 128, :])
        P.copy(s3b, s3)
        P.dma(wobf[c * 128:(c + 1) * 128, :], s3b)
    s4 = av(11264, 11264 + 128)
    for c in range(2):
        P.dma(s4, wuk_d[c * 128:(c + 1) * 128, :])
        P.act(wkv[:, c, 0:128], s4, AF.Copy, scale=kvg[:, c:c + 1])
        P.dma(s4, wuv_d[c * 128:(c + 1) * 128, :])
        P.act(wkv[:, c, 128:256], s4, AF.Copy, scale=kvg[:, c:c + 1])
    s5 = av(11776, 11776 + 64, BF16)
    for g in range(16):
        P.dma(s4, sk_d[g])
        P.copy(s5, s4)
        P.transpose(psb(7)[:, 0:128], s5, ident[:])
        P.copy(skT[:, g, :], psb(7)[:, 0:128])
    for g in range(4):
        P.dma(s4, spw_d[g])
        P.tt(s5, s4, trilm[:], ALU.mult)
        P.transpose(psb(7)[:, 0:128], s5, ident[:])
        P.copy(wsT[:, g, :], psb(7)[:, 0:128])

    if dbg == "C1":
        P.dma(out_d[0:128, :], g2b[:])
        P.emit(es)
        es.close()
        return nc
    xt = [T("xt%d" % i, [128, D]) for i in range(2)]
    xsb = T("xsb", [128, D], BF16)
    xsT = T("xsT", [128, 8, 128], BF16)
    st = T("stats", [128, 64])
    ang = T("ang", [128, 24])
    a12 = T("a12", [128, 48])
    a12f = T("a12f", [128, 48])
    a12i = T("a12i", [128, 48], I32)
    sc = T("sincos", [128, 48])
    rtmp = T("rtmp", [128, 4, 64])
    rtmp2 = T("rtmp2", [128, 4, 64])
    junkb = T("junkb", [128, 2048], BF16)
    junkf32 = T("junkf32", [128, 1024])
    junkf32b = T("junkf32b", [128, 256])
    cnb = T("cnb", [128, 256], BF16)
    cnT = T("cnT", [128, 2, 128], BF16)
    kn = T("kn", [128, 128])
    kbf = T("kbf", [128, 128], BF16)
    kif = T("kif", [128, 64])
    kibf = T("kibf", [128, 128], BF16)
    P.memset(kibf[:], 0.0)
    vvb = T("vvb", [128, 128], BF16)
    kTs = T("kTs", [128, 128], BF16)
    kiTs = T("kiTs", [64, 128], BF16)
    pooldummy = T("pooldummy", [128, 8], BF16)

    def sumsq(src, n, acc):
        if n <= 256:
            P.copy(junkf32b[:, 0:n], src)
            src = junkf32b[:, 0:n]
        P.dot(junkf32[:, 0:n], src, src, acc)

    def rstd_from_ss(dst, ss, n):
        P.act(dst, ss, AF.Sqrt, bias=epsb[:, 0:1], scale=1.0 / n)
        rk, wk_ = P._rw([dst], [dst])
        P.op("dve", lambda e, o=_k(dst)[0]: e.reciprocal(o, o), rk, wk_)

    def sincos(pos_col):
        P.ts(a12[:, 0:24], invf[:], pos_col, None, ALU.mult)
        P.ts(a12[:, 24:48], invf[:], pos_col, 0.5 * PI, ALU.mult, ALU.add)
        P.ts(a12f[:], a12[:], 1.0 / (2 * PI), None, ALU.mult)
        P.copy(a12i[:], a12f[:])
        P.copy(a12f[:], a12i[:])
        P.stt(a12[:], a12f[:], -2 * PI, a12[:], ALU.mult, ALU.add)
        P.ts(a12f[:], a12[:], PI, None, ALU.is_gt)
        P.stt(a12[:], a12f[:], -2 * PI, a12[:], ALU.mult, ALU.add)
        P.ts(a12f[:], a12[:], -PI, None, ALU.is_lt)
        P.stt(a12[:], a12f[:], 2 * PI, a12[:], ALU.mult, ALU.add)
        P.act(sc[:], a12[:], AF.Sin)

    def rope(dst, src, nh, dim, half, off, scv=None):
        if scv is None:
            scv = sc[:]
        sca, sck = _k(scv)
        sinb = (sca[:, off:off + half].unsqueeze(1).to_broadcast([128, nh, half]), sck)
        cosb = (sca[:, 24 + off:24 + off + half].unsqueeze(1).to_broadcast([128, nh, half]), sck)
        sa, sk_ = _k(src)
        da, dk_ = _k(dst)
        x1 = (sa[:, :, 0:half], sk_)
        x2 = (sa[:, :, half:2 * half], sk_)
        t1 = rtmp[:, 0:nh, 0:half]
        t2 = rtmp2[:, 0:nh, 0:half]
        P.tt(t1, x1, cosb, ALU.mult)
        P.tt(t2, x2, sinb, ALU.mult)
        P.tt((da[:, :, 0:half], dk_), t1, t2, ALU.subtract)
        t3 = rtmp[:, 0:nh, 16:16 + half]
        t4 = rtmp2[:, 0:nh, 16:16 + half]
        P.tt(t3, x2, cosb, ALU.mult)
        P.tt(t4, x1, sinb, ALU.mult)
        P.tt((da[:, :, half:2 * half], dk_), t3, t4, ALU.add)
        P.copy((da[:, :, 2 * half:dim], dk_), (sa[:, :, 2 * half:dim], sk_), eng="act")

    def front(xtile, bank):
        sumsq(xtile, D, st[:, 0:1])
        rstd_from_ss(st[:, 1:2], st[:, 0:1], D)
        P.act(xsb[:], xtile, AF.Copy, scale=st[:, 1:2])
        for c in range(8):
            P.transpose(psb(bank)[:, c * 128:(c + 1) * 128], xsb[:, c * 128:(c + 1) * 128], ident[:])
        P.copy(xsT[:].rearrange("p c t -> p (c t)"), psb(bank))

    def sincos_batch(dst, pos_all, t0, t1, A, Af, Ai):
        n = t1 - t0
        Aa, Ak = A
        A3 = Aa.rearrange("p (t c) -> p t c", c=48)
        invb = invf[:].unsqueeze(1).to_broadcast([128, n, 24])
        posb = pos_all[:, t0:t1].unsqueeze(2).to_broadcast([128, n, 24])
        P.tt((A3[:, :, 0:24], Ak), invb, posb, ALU.mult)
        P.ts((A3[:, :, 24:48], Ak), (A3[:, :, 0:24], Ak), 0.5 * PI, None, ALU.add)
        P.ts(Af, A, 1.0 / (2 * PI), None, ALU.mult)
        P.copy(Ai, Af)
        P.copy(Af, Ai)
        P.stt(A, Af, -2 * PI, A, ALU.mult, ALU.add)
        P.ts(Af, A, PI, None, ALU.is_gt)
        P.stt(A, Af, -2 * PI, A, ALU.mult, ALU.add)
        P.ts(Af, A, -PI, None, ALU.is_lt)
        P.stt(A, Af, 2 * PI, A, ALU.mult, ALU.add)
        P.act(dst, A, AF.Sin)


    SC5 = [None]

    def ktile(kt):
        xtile = xt[kt % 2]
        P.dma(xtile[:], x_seq[kt // TPP][(kt % TPP) * 128:(kt % TPP + 1) * 128, :])
        front(xtile[:], 7)
        for c in range(8):
            P.matmul(ps[0][:, 0:320], xsT[:, c, :], wk[:, c, :], start=(c == 0), stop=(c == 7))
        if SC5[0] is not None:
            sckt = SC5[0][:, (kt % 4) * 48:(kt % 4 + 1) * 48]
        else:
            sincos(posk[:, kt:kt + 1])
            sckt = None
        for _d in range(NDUMMY):
            P.act(junkb[:], junkb[:], AF.Copy)
        sumsq(ps[0][:, 0:256], 256, st[:, 2:3])
        rstd_from_ss(st[:, 3:4], st[:, 2:3], 256)
        P.act(cnb[:], ps[0][:, 0:256], AF.Copy, scale=st[:, 3:4])
        P.copy(kif[:], ps[0][:, 256:320])
        rope(kibf[:, 0:64].rearrange("p (h d) -> p h d", h=1), kif[:].rearrange("p (h d) -> p h d", h=1), 1, 64, 8, 16, sckt)
        for c in range(2):
            P.transpose(psb(6)[:, c * 128:(c + 1) * 128], cnb[:, c * 128:(c + 1) * 128], ident[:])
        P.copy(cnT[:].rearrange("p c t -> p (c t)"), psb(6)[:, 0:256])
        for c in range(2):
            P.matmul(ps[1][:, 0:256], cnT[:, c, :], wkv[:, c, :], start=(c == 0), stop=(c == 1))
        if dbg == "K3":
            return
        sumsq(ps[1][:, 0:128], 128, st[:, 4:5])
        rstd_from_ss(st[:, 5:6], st[:, 4:5], 128)
        P.stt(kn[:], ps[1][:, 0:128], st[:, 5:6], kgb[:], ALU.mult, ALU.mult)
        rope(kbf[:].rearrange("p (h d) -> p h d", h=1), kn[:].rearrange("p (h d) -> p h d", h=1), 1, 128, 16, 0, sckt)
        P.copy(vvb[:], ps[1][:, 128:256], eng="act")
        P.transpose(psb(5)[:, 0:128], kbf[:], ident[:])
        P.transpose(psb(5)[:, 128:256], kibf[:], ident[:])
        P.copy(kTs[:], psb(5)[:, 0:128], eng="act")
        P.copy(kiTs[:], psb(5)[0:64, 128:256], eng="act")
        if dbg == "K4":
            return
        if dbg != "K6":
            P.dma(vv_d[kt * 128:(kt + 1) * 128, :], vvb[:])
        if dbg == "K5":
            return
        P.dma(kT_d[kt], kTs[:])
        if dbg == "K6":
            return
        P.dma(kiT_d[kt], kiTs[:])


    if dbg and dbg[0] == "K":
        for kt in range(KT0, NKT if KT1 is None else KT1):
            ktile(kt)

    if dbg and dbg[0] == "K":
        P.dma(out_d[0:128, :], xt[0][:])
        P.emit(es)
        es.close()
        return nc

    wbuf = [T("wbuf%d" % i, [128, 2048], BF16) for i in range(3)]
    wctr = [0]

    def wstream(src, a, b):
        buf = wbuf[wctr[0] % 3]
        wctr[0] += 1
        view = buf[:, 0:a * b].rearrange("p (a b) -> p a b", a=a)
        P.dma(view, src.rearrange("(c p) n -> p c n", p=128))
        return view

    ug = T("ug", [128, 512])
    vg = T("vg", [128, 512])
    gt = T("gt", [128, 1024])
    vnbf = T("vnbf", [128, 512], BF16)
    yab = T("yab", [128, 512], BF16)
    yaT = T("yaT", [128, 4, 128], BF16)
    sg = T("sg", [128, 2048])
    merged = T("merged", [128, D])
    mbf = T("mbf", [128, D], BF16)
    mT = T("mT", [128, 8, 128], BF16)
    qn = T("qn", [128, 4, 128])
    qbf = T("qbf", [128, 4, 128], BF16)
    qT = T("qT", [128, 4, 128], BF16)
    qir = T("qir", [128, 4, 64])
    qibf = T("qibf", [128, 4, 128], BF16)
    qiT = T("qiT", [128, 4, 128], BF16)
    P.memset(qibf[:], 0.0)
    kich = [T("kich%d" % i, [64, 512], BF16) for i in range(2)]
    kch = [T("kch%d" % i, [128, 512], BF16) for i in range(2)]
    vch = [T("vch%d" % i, [128, 4, 128], BF16) for i in range(2)]
    rl = [T("rl%d" % i, [128, 512]) for i in range(4)]
    maskc = T("maskc", [128, 512], BF16)
    maskT = T("maskT", [128, 4, 128], BF16)
    eT = [T("eT%d" % i, [128, 512], BF16) for i in range(2)]
    eTm = [T("eTm%d" % i, [128, 512], BF16) for i in range(2)]
    rz = T("rz", [128, 512])
    ybT = T("ybT", [128, 4, 128], BF16)
    hres = T("hres", [128, D])
    sc5 = T("sc5", [128, 240])

    def sincos5(j):
        A = junkf32[:, 0:240]
        Af = junkf32[:, 256:496]
        Ai = junkf32[:, 512:752].bitcast(I32)
        A3 = A.rearrange("p (t c) -> p t c", c=48)
        P.tt(A3[:, 0:4, 0:24], invf[:].unsqueeze(1).to_broadcast([128, 4, 24]),
             posk[:, 4 * j:4 * j + 4].unsqueeze(2).to_broadcast([128, 4, 24]), ALU.mult)
        P.ts(A3[:, 4, 0:24], invf[:], poso[:, j:j + 1], None, ALU.mult)
        P.ts(A3[:, :, 24:48], A3[:, :, 0:24], 0.5 * PI, None, ALU.add)
        P.ts(Af, A, 1.0 / (2 * PI), None, ALU.mult)
        P.copy(Ai, Af)
        P.copy(Af, Ai)
        P.stt(A, Af, -2 * PI, A, ALU.mult, ALU.add)
        P.ts(Af, A, PI, None, ALU.is_gt)
        P.stt(A, Af, -2 * PI, A, ALU.mult, ALU.add)
        P.ts(Af, A, -PI, None, ALU.is_lt)
        P.stt(A, Af, 2 * PI, A, ALU.mult, ALU.add)
        P.act(sc5[:], A, AF.Sin)

    hsb = T("hsb", [128, D], BF16)
    hsT = T("hsT", [128, 8, 128], BF16)
    qpT = T("qpT", [128, 16, 128], BF16)
    vals = T("vals", [128, 16, 16])
    idxu = T("idxu", [128, 16, 16], U32)
    idxf = T("idxf", [128, 16, 16])
    best = T("best", [128, 8, 16])
    posu = T("posu", [128, 8, 16], U32)
    pa_i = T("pa_i", [128, 8, 16], U32)
    pb_i = T("pb_i", [128, 8, 16], U32)
    pa_f = T("pa_f", [128, 8, 16])
    pb_f = T("pb_f", [128, 8, 16])
    sel1 = T("sel1", [128, 8, 16])
    sel2 = T("sel2", [128, 8, 16])
    eidf = T("eidf", [128, 128])
    eid = T("eid", [128, 128], I32)
    gate = T("gate", [128, 8, 16])
    adot = T("adot", [128, 128])
    cw = T("cw", [128, 128])
    gtmp = T("gtmp", [128, 128])

    def gelu(dst, src, tmp, n):
        P.tt(tmp, src, src, ALU.mult)
        P.ts(tmp, tmp, 0.044715, 1.0, ALU.mult, ALU.add)
        P.tt(tmp, tmp, src, ALU.mult)
        P.act(tmp, tmp, AF.Sigmoid, scale=1.5957691216057308)
        P.tt(dst, src, tmp, ALU.mult)

    def top16(vout, iout, src, tmp):
        va, vk = _k(vout)
        ia, ik = _k(iout)
        rk, wk_ = P._rw([vout], [src])
        P.op("dve", lambda e, o=va[:, 0:8], i=_k(src)[0]: e.max(o, i), rk, wk_)
        rk, wk_ = P._rw([iout], [vout, src])
        P.op("dve", lambda e, o=ia[:, 0:8], m=va[:, 0:8], i=_k(src)[0]: e.max_index(o, m, i), rk, wk_)
        rk, wk_ = P._rw([tmp], [vout, src])
        P.op("dve", lambda e, o=_k(tmp)[0], m=va[:, 0:8], i=_k(src)[0]: e.match_replace(o, m, i, -1e30), rk, wk_)
        rk, wk_ = P._rw([vout], [tmp])
        P.op("dve", lambda e, o=va[:, 8:16], i=_k(tmp)[0]: e.max(o, i), rk, wk_)
        rk, wk_ = P._rw([iout], [vout, tmp])
        P.op("dve", lambda e, o=ia[:, 8:16], m=va[:, 8:16], i=_k(tmp)[0]: e.max_index(o, m, i), rk, wk_)

    proj = av(0, INW)
    pj = proj[0]
    pjk = proj[1]

    for j in range(NOWN):
        nch = (4 * j + 4) * 128 // 512
        sincos5(j)
        SC5[0] = sc5
        for kt in range(4 * j, 4 * j + 4):
            ktile(kt)
        xres = xt[j % 2]
        P.dma(xres[:], x_own[j * 128:(j + 1) * 128, :])
        front(xres[:], 7)
        ncc = (INW + 255) // 256
        for cc in range(ncc):
            c0 = cc * 256
            w = min(256, INW - c0)
            wbf = wstream(winbf[:, c0:c0 + w], 8, w)
            bank = cc % 2
            for c in range(8):
                P.matmul(ps[bank][:, 0:w], xsT[:, c, :], wbf[:, c, :], start=(c == 0), stop=(c == 7))
            P.copy((pj[:, c0:c0 + w], pjk), ps[bank][:, 0:w], eng=("act" if cc % 2 else "dve"))
        gelu(ug[:], (pj[:, 0:512], pjk), gt[:, 0:512], 512)
        gelu(vg[:], (pj[:, 512:1024], pjk), gt[:, 512:1024], 512)
        P.reduce(st[:, 8:9], vg[:], ALU.add)
        P.dot(junkf32[:, 0:512], vg[:], vg[:], st[:, 9:10])
        P.ts(st[:, 10:11], st[:, 8:9], 1.0 / 512, None, ALU.mult)
        P.tt(st[:, 11:12], st[:, 10:11], st[:, 10:11], ALU.mult)
        P.stt(st[:, 12:13], st[:, 9:10], 1.0 / 512, st[:, 11:12], ALU.mult, ALU.subtract)
        rstd_from_ss(st[:, 12:13], st[:, 12:13], 1)
        P.ts(vg[:], vg[:], st[:, 10:11], st[:, 12:13], ALU.subtract, ALU.mult)
        P.tt(vg[:], vg[:], vng[:], ALU.mult)
        P.tt(vnbf[:], vg[:], vnb[:], ALU.add)
        for g in range(4):
            P.matmul(ps[2][:, g * 128:(g + 1) * 128], wsT[:, g, :], vnbf[:, g * 128:(g + 1) * 128])
        for g in range(4):
            P.stt(yab[:, g * 128:(g + 1) * 128], ps[2][:, g * 128:(g + 1) * 128], spb[:, g:g + 1],
                  ug[:, g * 128:(g + 1) * 128], ALU.add, ALU.mult)
        for c in range(4):
            P.transpose(psb(3)[:, c * 128:(c + 1) * 128], yab[:, c * 128:(c + 1) * 128], ident[:])
        P.copy(yaT[:].rearrange("p c t -> p (c t)"), psb(3)[:, 0:512])
        for hf in range(2):
            wv = wstream(wabf[:, hf * 512:(hf + 1) * 512], 4, 512)
            for c in range(4):
                P.matmul(ps[4 + hf][:], yaT[:, c, :], wv[:, c, :], start=(c == 0), stop=(c == 3))
        P.act(sg[:], (pj[:, 2116:4164], pjk), AF.Sigmoid)
        for hf in range(2):
            P.tt(merged[:, hf * 512:(hf + 1) * 512], sg[:, hf * 512:(hf + 1) * 512], ps[4 + hf][:], ALU.mult)
        P.tt(junkf32[:, 0:512], (pj[:, 1024:1536], pjk), (pj[:, 1024:1536], pjk), ALU.mult)
        P.reduce(st[:, 16:20], junkf32[:, 0:512].rearrange("p (h d) -> p h d", h=4), ALU.add)
        rstd_from_ss(st[:, 20:24], st[:, 16:20], 128)
        for h in range(4):
            P.stt(qn[:, h, :], (pj[:, 1024 + h * 128:1024 + (h + 1) * 128], pjk), st[:, 20 + h:21 + h], qgb[:], ALU.mult, ALU.mult)
        rope(qbf[:], qn[:], 4, 128, 16, 0, sc5[:, 192:240])
        for h in range(4):
            P.transpose(psb(6)[:, h * 128:(h + 1) * 128], qbf[:, h, :], ident[:])
        P.copy(qT[:].rearrange("p h t -> p (h t)"), psb(6)[:, 0:512])
        wid = (pj[:, 2112:2116], pjk)
        P.ts(st[:, 28:32], wid, 0.0, 2.0, ALU.is_gt, ALU.mult)
        P.ts(st[:, 28:32], st[:, 28:32], -1.0, None, ALU.add)
        P.stt(st[:, 24:28], wid, 0.0625, st[:, 28:32], ALU.mult, ALU.mult)
        rope(qir[:], (pj[:, 1792:2048].rearrange("p (h d) -> p h d", h=4), pjk), 4, 64, 8, 16, sc5[:, 192:240])
        P.tt(qibf[:, :, 0:64], qir[:], st[:, 24:28].unsqueeze(2).to_broadcast([128, 4, 64]), ALU.mult)
        for h in range(4):
            P.transpose(psb(7)[:, h * 128:(h + 1) * 128], qibf[:, h, :], ident[:])
        P.copy(qiT[:].rearrange("p h t -> p (h t)"), psb(7)[:, 0:512])
        for c in range(nch):
            kb = kich[c % 2]
            P.dma(kb[:].rearrange("p (k t) -> p k t", k=4), kiT_d[c * 4:(c + 1) * 4].rearrange("k p t -> p k t"))
            scv = av(c * 512, (c + 1) * 512)
            for h in range(4):
                P.matmul(ps[h][:], qiT[0:64, h, :], kb[:])
            for h in range(4):
                P.act(rl[h][:], ps[h][:], AF.Relu)
            P.ts(scv, rl[0][:], st[:, 28:29], None, ALU.mult)
            for h in range(1, 4):
                P.stt(scv, rl[h][:], st[:, 28 + h:29 + h], scv, ALU.mult, ALU.add)
            if c == nch - 1:
                P.tt(scv, scv, maskb[:], ALU.add)
        n = nch * 512
        lo, hi, mid, cnt, ge, dd = (st[:, 32:33], st[:, 33:34], st[:, 34:35], st[:, 35:36], st[:, 36:37], st[:, 37:38])
        cnts = st[:, 40:48]
        P.memset(mid, 0.0)
        pieces = [(p0, min(p0 + 2048, n)) for p0 in range(0, n, 2048)]
        hstep = 8.0
        ktop = min(TOPK, S // 4) - 0.5
        for it in range(BIS_ITERS):
            jk_ = [junkb[:, 0:2048], junkf32[:].bitcast(BF16)]
            for pi_, (p0, p1) in enumerate(pieces):
                P.ts(jk_[pi_ % 2][:, 0:p1 - p0], av(p0, p1), mid, None, ALU.is_ge, ALU.add,
                     accum=(cnts[:, pi_:pi_ + 1], (("cnt", pi_),)))
            if len(pieces) > 1:
                P.reduce(cnt, (cnts[:, 0:len(pieces)], tuple(("cnt", p_) for p_ in range(len(pieces)))), ALU.add)
                cc_ = cnt
            else:
                cc_ = (cnts[:, 0:1], (("cnt", 0),))
            P.ts(dd, cc_, ktop, hstep, ALU.is_ge, ALU.mult)
            P.stt(mid, dd, -0.5 * hstep, mid, ALU.add, ALU.add)
            hstep *= 0.5
        P.ts(lo, mid, -hstep, None, ALU.add)
        nkb = nch * 4
        for c in range(nch):
            kc = kch[c % 2]
            vc = vch[c % 2]
            P.dma(kc[:].rearrange("p (k t) -> p k t", k=4), kT_d[c * 4:(c + 1) * 4].rearrange("k p t -> p k t"))
            P.dma(vc[:], vv_d[c * 512:(c + 1) * 512, :].rearrange("(kb p) d -> p kb d", p=128))
            P.ts(maskc[:], av(c * 512, (c + 1) * 512), lo, None, ALU.is_ge)
            for b in range(4):
                P.transpose(psb(0)[:, b * 128:(b + 1) * 128], maskc[:, b * 128:(b + 1) * 128], ident[:])
            P.copy(maskT[:].rearrange("p b q -> p (b q)"), psb(0)[:, 0:512])
            for b in range(4):
                gkb = c * 4 + b
                bank = 1 + (gkb % 2)
                P.matmul(ps[bank][:], kc[:, b * 128:(b + 1) * 128], qT[:].rearrange("p h t -> p (h t)"))
                e1 = eT[gkb % 2]
                e2 = eTm[gkb % 2]
                P.act(e1[:], ps[bank][:], AF.Exp, scale=float(128 ** -0.5))
                P.tt(e2[:].rearrange("p (h q) -> p h q", h=4), e1[:].rearrange("p (h q) -> p h q", h=4),
                     maskT[:, b, :].unsqueeze(1).to_broadcast([128, 4, 128]), ALU.mult)
                P.matmul(ps[4][:], vc[:, b, :], e2[:], start=(gkb == 0), stop=(gkb == nkb - 1))
                P.matmul(ps[5][:], ones[:], e2[:], start=(gkb == 0), stop=(gkb == nkb - 1))
        if dbg == "H" and j == NOWN - 1:
            P.dma(d_mT.ap(), maskT[:].rearrange("p b q -> p (b q)"))
            P.dma(d_e1.ap(), eT[(nkb - 1) % 2][:])
            P.dma(d_e2.ap(), eTm[(nkb - 1) % 2][:])
            P.copy(gt[:, 0:512], ps[4][:])
            P.dma(d_num.ap(), gt[:, 0:512])
        P.op("dve", lambda e: e.reciprocal(rz[:], ps[5][:]), ["ps5"], ["rz"])
        if dbg == "H" and j == NOWN - 1:
            P.dma(d_rz.ap(), rz[:])
        P.copy(gt[:, 512:1024], ps[4][:], eng="act")
        P.tt(ybT[:].rearrange("p h t -> p (h t)"), gt[:, 512:1024], rz[:], ALU.mult)
        for hf in range(2):
            wv = wstream(wbbf[:, hf * 512:(hf + 1) * 512], 4, 512)
            for h in range(4):
                P.matmul(ps[6 + hf][:], ybT[:, h, :], wv[:, h, :], start=(h == 0), stop=(h == 3))
        for hf in range(2):
            P.tt(gt[:, hf * 512:(hf + 1) * 512], sg[:, 1024 + hf * 512:1024 + (hf + 1) * 512], ps[6 + hf][:], ALU.mult)
        if dbg == "H" and j == NOWN - 1:
            P.dma(d_yb.ap(), ybT[:].rearrange("p h t -> p (h t)"))
            P.dma(d_gb.ap(), gt[:])
        P.tt(mbf[:], merged[:], gt[:], ALU.add)
        for c in range(8):
            P.transpose(psb(0)[:, c * 128:(c + 1) * 128], mbf[:, c * 128:(c + 1) * 128], ident[:])
        P.copy(mT[:].rearrange("p c t -> p (c t)"), psb(0))
        for qd in range(4):
            wv = wstream(wobf[:, qd * 256:(qd + 1) * 256], 8, 256)
            for c in range(8):
                P.matmul(ps[1 + qd // 2][:, (qd % 2) * 256:(qd % 2 + 1) * 256], mT[:, c, :], wv[:, c, :],
                         start=(c == 0), stop=(c == 7))
        for hf in range(2):
            P.tt(hres[:, hf * 512:(hf + 1) * 512], xres[:, hf * 512:(hf + 1) * 512], ps[1 + hf][:], ALU.add)
        if dbg == "H":
            P.dma(out_d[j * 128:(j + 1) * 128, :], hres[:])
            continue
        hn = av(0, 1024)
        sub = av(1024, 3072)
        sub2 = av(3072, 5120)
        cand = av(5120, 7168)
        oh = av(7168, 9216)
        junkf = av(9216, 10240)
        gbuf = [av(10240 + r * 1024, 10240 + (r + 1) * 1024) for r in range(6)]
        sumsq(hres[:], D, st[:, 48:49])
        rstd_from_ss(st[:, 49:50], st[:, 48:49], D)
        P.act(hsb[:], hres[:], AF.Copy, scale=st[:, 49:50])
        P.stt(hn, hres[:], st[:, 49:50], g2b[:], ALU.mult, ALU.mult)
        for c in range(8):
            P.transpose(psb(2)[:, c * 128:(c + 1) * 128], hsb[:, c * 128:(c + 1) * 128], ident[:])
        P.copy(hsT[:].rearrange("p c t -> p (c t)"), psb(2))
        for gp in range(8):
            wv = wstream(wqbf[:, gp * 256:(gp + 1) * 256], 8, 256)
            for g2_ in range(2):
                g = gp * 2 + g2_
                bank = 3 + g // 4
                for c in range(8):
                    P.matmul(ps[bank][:, (g % 4) * 128:(g % 4 + 1) * 128], wv[:, c, g2_ * 128:(g2_ + 1) * 128], hsT[:, c, :],
                             start=(c == 0), stop=(c == 7))
        for b in range(4):
            P.copy(qpT[:, b * 4:(b + 1) * 4, :].rearrange("p g t -> p (g t)"), ps[3 + b][:], eng=("act" if b % 2 else "dve"))
        sbanks = [7, 0, 1, 2]
        for g in range(16):
            P.matmul(ps[sbanks[g // 4]][:, (g % 4) * 128:(g % 4 + 1) * 128], qpT[:, g, :], skT[:, g, :])
        for b in range(4):
            P.copy((sub[0][:, b * 512:(b + 1) * 512], sub[1]), ps[sbanks[b]][:], eng=("act" if b % 2 else "dve"))
        for g in range(16):
            top16(vals[:, g, :], idxu[:, g, :], (sub[0][:, g * 128:(g + 1) * 128], sub[1]),
                  (sub2[0][:, g * 128:(g + 1) * 128], sub2[1]))
        v4 = vals[:].rearrange("p (h t) k -> p h t k", t=2)
        c4 = (cand[0].rearrange("p (h a b) -> p h a b", h=8, a=16), cand[1])
        P.tt(c4, v4[:, :, 0, :].unsqueeze(3).to_broadcast([128, 8, 16, 16]),
             v4[:, :, 1, :].unsqueeze(2).to_broadcast([128, 8, 16, 16]), ALU.add)
        for h in range(8):
            top16(best[:, h, :], posu[:, h, :], (cand[0][:, h * 256:(h + 1) * 256], cand[1]),
                  (sub2[0][:, h * 256:(h + 1) * 256], sub2[1]))
        P.op("dve", lambda e: e.tensor_single_scalar(pa_i[:], posu[:], 4, ALU.logical_shift_right), ["posu"], ["pa_i"])
        P.op("dve", lambda e: e.tensor_single_scalar(pb_i[:], posu[:], 15, ALU.bitwise_and), ["posu"], ["pb_i"])
        P.copy(pa_f[:], pa_i[:])
        P.copy(pb_f[:], pb_i[:])
        P.copy(idxf[:], idxu[:])
        i4 = idxf[:].rearrange("p (h t) k -> p h t k", t=2)
        o4 = (oh[0].rearrange("p (h k a) -> p h k a", h=8, k=16), oh[1])
        iob = iota16[:].unsqueeze(1).unsqueeze(1).to_broadcast([128, 8, 16, 16])
        for (pf, t_, so) in ((pa_f, 0, sel1), (pb_f, 1, sel2)):
            P.tt(o4, iob, pf[:].unsqueeze(3).to_broadcast([128, 8, 16, 16]), ALU.is_equal)
            P.tt(o4, o4, i4[:, :, t_, :].unsqueeze(2).to_broadcast([128, 8, 16, 16]), ALU.mult)
            P.reduce(so[:], o4, ALU.add)
        P.stt(eidf[:].rearrange("p (h k) -> p h k", h=8), sel1[:], 128.0, sel2[:], ALU.mult, ALU.add)
        P.copy(eid[:], eidf[:])
        P.reduce(st[:, 50:58], best[:], ALU.max)
        P.tt(gate[:], best[:], st[:, 50:58].unsqueeze(2).to_broadcast([128, 8, 16]), ALU.subtract)
        P.act(gate[:], gate[:], AF.Exp)
        P.reduce(rz[:, 0:8], gate[:], ALU.add)
        P.op("dve", lambda e: e.reciprocal(rz[:, 8:16], rz[:, 0:8]), ["rz"], ["rz"])
        P.tt(gate[:], gate[:], rz[:, 8:16].unsqueeze(2).to_broadcast([128, 8, 16]), ALU.mult)
        for s_ in range(128):
            gb = gbuf[s_ % 6]
            P.gather(gb, u_tab.ap(), eid[:, s_:s_ + 1])
            P.dot(junkf, gb, hn, adot[:, s_:s_ + 1])
        gelu(cw[:], adot[:], gtmp[:], 128)
        P.tt(cw[:], cw[:], gate[:].rearrange("p h k -> p (h k)"), ALU.mult)
        for s_ in range(128):
            gb = gbuf[s_ % 6]
            P.gather(gb, v_tab.ap(), eid[:, s_:s_ + 1])
            P.stt(hres[:], gb, cw[:, s_:s_ + 1], hres[:], ALU.mult, ALU.add)
        P.dma(out_d[j * 128:(j + 1) * 128, :], hres[:])

    P.emit(es)
    es.close()
    return nc


def _host_inputs(inp, S, NOWN):
    f32 = np.float32
    B = inp["x"].shape[0]
    x = np.asarray(inp["x"], f32)
    pos = np.asarray(inp["positions"], np.int32)
    NKT = S // 128
    rep = lambda v, n=128: np.ascontiguousarray(np.broadcast_to(np.asarray(v, f32).reshape(1, -1), (n, np.asarray(v).size)))
    pc = lambda v, c: np.ascontiguousarray(np.asarray(v, f32).reshape(c, 128).T)
    half_q = np.arange(16, dtype=f32)
    half_i = np.arange(8, dtype=f32)
    inv_q = np.power(f32(500000.0), -half_q * f32(2.0) / f32(32)).astype(f32)
    inv_i = np.power(f32(500000.0), -half_i * f32(2.0) / f32(16)).astype(f32)
    invf = rep(np.concatenate([inv_q, inv_i]))
    common = {
        "w_in": np.ascontiguousarray(inp["w_in"][0], f32),
        "g1": pc(inp["norm1_g"][0], 8),
        "g2": pc(inp["norm2_g"][0], 8),
        "g2b": rep(inp["norm2_g"][0]),
        "kvg": pc(inp["kv_norm_g"][0], 2),
        "vng": rep(inp["v_norm_g"][0]),
        "vnb": rep(inp["v_norm_b"][0]),
        "spw": np.ascontiguousarray(inp["spatial_w"][0], f32),
        "spb": np.ascontiguousarray(np.asarray(inp["spatial_b"][0], f32).T),
        "wuk": np.ascontiguousarray(inp["w_uk"][0], f32),
        "wuv": np.ascontiguousarray(inp["w_uv"][0], f32),
        "qgb": rep(inp["q_norm_g"][0]),
        "kgb": rep(inp["k_norm_g"][0]),
        "wa": np.ascontiguousarray(inp["w_a_out"][0], f32),
        "wb": np.ascontiguousarray(inp["w_b_out"][0], f32),
        "wo": np.ascontiguousarray(inp["w_o"][0], f32),
        "wq": np.ascontiguousarray(inp["peer_wq"][0], f32),
        "subk": np.ascontiguousarray(np.asarray(inp["peer_subkeys"][0], f32).reshape(16, 128, 128)),
        "peer_u": np.ascontiguousarray(inp["peer_u"][0], f32),
        "peer_v": np.ascontiguousarray(inp["peer_v"][0], f32),
        "ident": np.eye(128, dtype=f32).astype(ml_dtypes.bfloat16),
        "tril": np.tril(np.ones((128, 128), f32)),
        "invf": invf,
        "iota16": rep(np.arange(16, dtype=f32)),
    }
    maps = []
    lanes = 8 // B
    for core in range(8):
        b, c = core // lanes, core % lanes
        tiles = [lanes * j + c for j in range(NOWN)]
        rows = np.concatenate([np.arange(t * 128, (t + 1) * 128) for t in tiles])
        mb = np.zeros((128, 512), f32)
        for r in range(4):
            blk = mb[:, r * 128:(r + 1) * 128]
            if r > c:
                blk[:] = -1e30
            elif r == c:
                blk[:] = np.where(np.arange(128)[None, :] <= np.arange(128)[:, None], 0.0, -1e30)
        m = dict(common)
        for i in range(4):
            m["x_seq%d" % i] = np.ascontiguousarray(x[b][i * (S // 4):(i + 1) * (S // 4)])
        m["x_own"] = np.ascontiguousarray(x[b][rows])
        m["pos_seq"] = np.ascontiguousarray(pos[b].reshape(NKT, 128).T)
        m["pos_own"] = np.ascontiguousarray(pos[b][rows].reshape(NOWN, 128).T)
        m["maskb"] = mb
        maps.append((m, b, rows))
    return maps


_NC_CACHE = {}


def kernel(**inputs):
    x = np.asarray(inputs["x"])
    B, S, _ = x.shape
    lanes = 8 // B
    NOWN = S // 128 // lanes
    key = (S, NOWN)
    if key not in _NC_CACHE:
        _NC_CACHE[key] = build_program(S, NOWN)
    nc = _NC_CACHE[key]
    maps = _host_inputs(inputs, S, NOWN)
    res = run_bass_kernel_spmd(nc, [m for m, _, _ in maps], core_ids=list(range(8)))
    out = np.empty((B, S, D), np.float32)
    for (m, b, rows), r in zip(maps, res.results):
        out[b, rows] = r["out"]
    return out
```
